# Optimizing a Trainium2 kernel written in Bass

```python
import jax
import jax.numpy as jnp
from jax import lax
import numpy as np

D_MODEL = 1024
BATCH = 8
SEQ = 2048
DEPTH = 4

GRID_W = 64
CTX_LEN = 256
N_BRANCH = 4
BRANCH_W = D_MODEL // 2
RET_HEADS = 4
RET_DK = BRANCH_W // RET_HEADS
HG_HEADS = 4
HG_DK = BRANCH_W // HG_HEADS
NA_HEADS = 8
NA_HD = BRANCH_W // NA_HEADS
NA_KH = 8
NA_KW = 16
GQA_HEADS = 8
GQA_KV_HEADS = 2
GQA_HD = BRANCH_W // GQA_HEADS
GQA_KV_W = GQA_KV_HEADS * GQA_HD
GQA_BLOCK = 128
SCAN_CHUNK = 64
ROPE_BASE = 10000.0
N_EXPERTS = 32
TOP_K = 4
D_FF_EXPERT = D_MODEL
SWIGLU_LIMIT = 7.0
SWIGLU_ALPHA = 1.702
MOE_BLOCK = 256
LN_EPS = 1e-5
NORM_EPS = 1e-6
NEG_INF = -1e30
DN_ALPHA = (2 * DEPTH) ** 0.25
DN_BETA = (8 * DEPTH) ** -0.25
PROJ_WIDTHS = (BRANCH_W,) * 4 + (BRANCH_W,) * 5 + (BRANCH_W,) * 3 + (BRANCH_W, GQA_KV_W, GQA_KV_W, N_BRANCH * D_MODEL)
PROJ_TOTAL = sum(PROJ_WIDTHS)

kernel_name = 'hybrid_gated_ret_hgrn2_na_gqa_moe_dit'


def _heads(a, n):
    b, t, w = a.shape
    return a.reshape(b, t, n, w // n).transpose(0, 2, 1, 3)


def _merge_heads(a):
    b, n, t, d = a.shape
    return a.transpose(0, 2, 1, 3).reshape(b, t, n * d)


def _layernorm(x, g, b):
    xf = x.astype(jnp.float32)
    mu = jnp.mean(xf, -1, keepdims=True)
    var = jnp.mean(jnp.square(xf - mu), -1, keepdims=True)
    return ((xf - mu) * lax.rsqrt(var + LN_EPS) * g + b).astype(x.dtype)


def _rmsnorm(x, g):
    xf = x.astype(jnp.float32)
    return (xf * lax.rsqrt(jnp.mean(jnp.square(xf), -1, keepdims=True) + NORM_EPS) * g).astype(x.dtype)


def _grid_pos(t):
    idx = jnp.arange(t, dtype=jnp.int32)
    return (idx // GRID_W).astype(jnp.float32), (idx % GRID_W).astype(jnp.float32)


def _rotate(x, pos):
    n = x.shape[-1]
    inv = ROPE_BASE ** (-jnp.arange(0, n, 2, dtype=jnp.float32) / n)
    ang = pos[:, None] * inv[None, :]
    cos, sin = jnp.cos(ang), jnp.sin(ang)
    x1 = x[..., : n // 2].astype(jnp.float32)
    x2 = x[..., n // 2:].astype(jnp.float32)
    return jnp.concatenate([x1 * cos - x2 * sin, x1 * sin + x2 * cos], -1).astype(x.dtype)


def _rope2d(x, row, col):
    d = x.shape[-1]
    return jnp.concatenate([_rotate(x[..., : d // 2], row), _rotate(x[..., d // 2:], col)], -1)


def _attend(q, k, v):
    s = jnp.einsum('bhqd,bhkd->bhqk', q, k).astype(jnp.float32) * (q.shape[-1] ** -0.5)
    p = jax.nn.softmax(s, axis=-1)
    return jnp.einsum('bhqk,bhkd->bhqd', p.astype(v.dtype), v)


def _chunk_scan(q, k, v, logf, s0, include_diag):
    out_dtype = v.dtype
    b_, h_, t_, _ = q.shape
    dv = v.shape[-1]
    n = t_ // SCAN_CHUNK
    scalar_decay = logf.shape[-1] == 1
    mask = jnp.tril(jnp.ones((SCAN_CHUNK, SCAN_CHUNK), bool), 0 if include_diag else -1)

    def to_chunks(a):
        a = a.astype(jnp.float32)
        return jnp.moveaxis(a.reshape(b_, h_, n, SCAN_CHUNK, a.shape[-1]), 2, 0)

    def step(s, inp):
        qc, kc, vc, lf = inp
        cum = jnp.cumsum(lf, axis=2)
        seg = cum[:, :, :, None, :] - cum[:, :, None, :, :]
        dec = jnp.exp(jnp.where(mask[:, :, None], seg, -jnp.inf))
        if scalar_decay:
            a = jnp.einsum('bhid,bhjd->bhij', qc, kc) * dec[..., 0]
        else:
            a = jnp.einsum('bhid,bhjd,bhijd->bhij', qc, kc, dec)
        o = jnp.einsum('bhij,bhje->bhie', a, vc) + jnp.einsum('bhid,bhde->bhie', qc * jnp.exp(cum), s)
        last = cum[:, :, -1:, :]
        s_new = jnp.exp(last[:, :, 0, :])[..., None] * s + jnp.einsum('bhjd,bhje->bhde', kc * jnp.exp(last - cum), vc)
        return s_new, o

    s_fin, o = lax.scan(step, s0, (to_chunks(q), to_chunks(k), to_chunks(v), to_chunks(logf)))
    o = jnp.moveaxis(o, 0, 2).reshape(b_, h_, t_, dv)
    return o.astype(out_dtype), s_fin


def _bidir_prefix(lat_dirs, ctx_dirs, diags):
    outs_l, outs_c = [], []
    for d in range(2):
        lat, cx = lat_dirs[d], ctx_dirs[d]
        if d == 1:
            lat = tuple(jnp.flip(a, axis=2) for a in lat)
            cx = tuple(jnp.flip(a, axis=2) for a in cx)
        b_, h_, _, dk = cx[0].shape
        dv = cx[2].shape[-1]
        s0 = jnp.zeros((b_, h_, dk, dv), jnp.float32)
        o_c, s_c = _chunk_scan(cx[0], cx[1], cx[2], cx[3], s0, diags[d])
        o_l, _ = _chunk_scan(lat[0], lat[1], lat[2], lat[3], s_c, diags[d])
        if d == 1:
            o_c, o_l = jnp.flip(o_c, axis=2), jnp.flip(o_l, axis=2)
        outs_l.append(o_l)
        outs_c.append(o_c)
    return outs_l[0] + outs_l[1], outs_c[0] + outs_c[1]


def _retention(parts, parts_c, decay_logit, gn_g, gn_b, row, col):
    q, k, v, g = parts
    qc, kc, vc, gc = parts_c
    b_, t_, _ = q.shape
    l_ = qc.shape[1]
    log_gamma = jax.nn.log_sigmoid(decay_logit.astype(jnp.float32))

    def heads(q_, k_, v_):
        return _heads(q_, RET_HEADS), _heads(k_, RET_HEADS) * (RET_DK ** -0.5), _heads(v_, RET_HEADS)

    ql, kl, vl = heads(q, k, v)
    ql, kl = _rope2d(ql, row, col), _rope2d(kl, row, col)
    qx, kx, vx = heads(qc, kc, vc)

    def decay(d, t):
        return jnp.broadcast_to(log_gamma[d][None, :, None, None], (b_, RET_HEADS, t, 1))

    lat_dirs = [(ql, kl, vl, decay(d, t_)) for d in range(2)]
    ctx_dirs = [(qx, kx, vx, decay(d, l_)) for d in range(2)]
    o_l, o_c = _bidir_prefix(lat_dirs, ctx_dirs, (True, False))

    def out(o, gate):
        of = o.astype(jnp.float32)
        mu = jnp.mean(of, -1, keepdims=True)
        var = jnp.mean(jnp.square(of - mu), -1, keepdims=True)
        y = _merge_heads((of - mu) * lax.rsqrt(var + LN_EPS)) * gn_g + gn_b
        return (y * jax.nn.silu(gate.astype(jnp.float32))).astype(gate.dtype)

    return out(o_l, g), out(o_c, gc)


def _hgrn2(parts, parts_c, lower, norm_g):
    def prep(q, f_fwd, f_bwd, i):
        qh = _heads(jax.nn.silu(q), HG_HEADS)
        vh = _heads(i, HG_HEADS)
        dirs = []
        for d, f_raw in enumerate((f_fwd, f_bwd)):
            z = _heads(f_raw, HG_HEADS).astype(jnp.float32)
            lb = lower[d].reshape(1, HG_HEADS, 1, HG_DK)
            f = lb + (1.0 - lb) * jax.nn.sigmoid(z)
            key = (1.0 - lb) * jax.nn.sigmoid(-z)
            dirs.append((qh, key, vh, jnp.log(f)))
        return dirs

    o_l, o_c = _bidir_prefix(prep(*parts[:4]), prep(*parts_c[:4]), (True, True))

    def out(o, gate):
        of = o.astype(jnp.float32)
        on = of * lax.rsqrt(jnp.mean(jnp.square(of), -1, keepdims=True) + NORM_EPS)
        y = _merge_heads(on) * norm_g
        return (y * jax.nn.silu(gate.astype(jnp.float32))).astype(gate.dtype)

    return out(o_l, parts[4]), out(o_c, parts_c[4])


def _neighbourhood_attn(ql, kl, vl, qc, kc, vc, rpb):
    b_, t_, _ = ql.shape
    rows = t_ // GRID_W
    kh = min(NA_KH, rows)
    n_cb = GRID_W // NA_KW
    band = 2 * NA_KW
    scale = NA_HD ** -0.5

    def grid(a):
        return _heads(a, NA_HEADS).reshape(b_, NA_HEADS, rows, GRID_W, NA_HD)

    qg, kg, vg = grid(ql), grid(kl), grid(vl)
    r = jnp.arange(rows, dtype=jnp.int32)
    row_idx = jnp.clip(r - kh // 2, 0, rows - kh)[:, None] + jnp.arange(kh, dtype=jnp.int32)[None, :]
    cidx = jnp.arange(GRID_W, dtype=jnp.int32)
    col0 = jnp.clip(cidx - NA_KW // 2, 0, GRID_W - NA_KW)
    band_idx = (jnp.clip(jnp.arange(n_cb, dtype=jnp.int32) * NA_KW - NA_KW // 2, 0, GRID_W - band)[:, None]
                + jnp.arange(band, dtype=jnp.int32)[None, :])

    k_blk = kg[:, :, row_idx][:, :, :, :, band_idx]
    v_blk = vg[:, :, row_idx][:, :, :, :, band_idx]
    q_blk = qg.reshape(b_, NA_HEADS, rows, n_cb, NA_KW, NA_HD)
    s_loc = jnp.einsum('bhrjqd,bhrajkd->bhrjqak', q_blk, k_blk).astype(jnp.float32) * scale

    key_col = band_idx[:, None, :]
    q_col = cidx.reshape(n_cb, NA_KW)[:, :, None]
    q_col0 = col0.reshape(n_cb, NA_KW)[:, :, None]
    valid = (key_col >= q_col0) & (key_col < q_col0 + NA_KW)
    dr = row_idx - r[:, None] + (NA_KH - 1)
    dc = jnp.clip(key_col - q_col + (NA_KW - 1), 0, 2 * NA_KW - 2)
    bias = rpb[:, dr[:, None, None, :, None], dc[None, :, :, None, :]]
    s_loc = jnp.where(valid[:, :, None, :], s_loc + bias.astype(jnp.float32), NEG_INF)

    qx, kx, vx = _heads(qc, NA_HEADS), _heads(kc, NA_HEADS), _heads(vc, NA_HEADS)
    s_ctx = jnp.einsum('bhrjqd,bhld->bhrjql', q_blk, kx).astype(jnp.float32) * scale
    n_loc = kh * band
    s_all = jnp.concatenate([s_loc.reshape(b_, NA_HEADS, rows, n_cb, NA_KW, n_loc), s_ctx], -1)
    p = jax.nn.softmax(s_all, axis=-1).astype(vl.dtype)
    p_loc = p[..., :n_loc].reshape(b_, NA_HEADS, rows, n_cb, NA_KW, kh, band)
    o = (jnp.einsum('bhrjqak,bhrajkd->bhrjqd', p_loc, v_blk)
         + jnp.einsum('bhrjql,bhld->bhrjqd', p[..., n_loc:], vx))
    o_lat = _merge_heads(o.reshape(b_, NA_HEADS, t_, NA_HD))
    o_ctx = _merge_heads(_attend(qx, kx, vx))
    return o_lat, o_ctx


def _gqa_attend(q, k, v):
    s = jnp.einsum('bhgqd,bhkd->bhgqk', q, k).astype(jnp.float32) * (q.shape[-1] ** -0.5)
    p = jax.nn.softmax(s, axis=-1)
    return jnp.einsum('bhgqk,bhkd->bhgqd', p.astype(v.dtype), v)


def _gqa(ql, kl, vl, qc, kc, vc, qn_g, kn_g, row, col):
    b_, t_, _ = ql.shape
    grp = GQA_HEADS // GQA_KV_HEADS

    def q_heads(a):
        return a.reshape(b_, a.shape[1], GQA_KV_HEADS, grp, GQA_HD).transpose(0, 2, 3, 1, 4)

    qlh = _rope2d(_rmsnorm(q_heads(ql), qn_g), row, col)
    klh = _rope2d(_rmsnorm(_heads(kl, GQA_KV_HEADS), kn_g), row, col)
    qch = _rmsnorm(q_heads(qc), qn_g)
    kch = _rmsnorm(_heads(kc, GQA_KV_HEADS), kn_g)
    vlh, vch = _heads(vl, GQA_KV_HEADS), _heads(vc, GQA_KV_HEADS)
    k_all = jnp.concatenate([klh, kch], axis=2)
    v_all = jnp.concatenate([vlh, vch], axis=2)
    nb = t_ // GQA_BLOCK
    q_blocks = jnp.moveaxis(qlh.reshape(b_, GQA_KV_HEADS, grp, nb, GQA_BLOCK, GQA_HD), 3, 0)
    o = lax.map(lambda qb: _gqa_attend(qb, k_all, v_all), q_blocks)
    o = jnp.moveaxis(o, 0, 3).reshape(b_, GQA_KV_HEADS, grp, t_, GQA_HD)
    o_lat = o.transpose(0, 3, 1, 2, 4).reshape(b_, t_, GQA_HEADS * GQA_HD)
    oc = _gqa_attend(qch, kch, vch)
    o_ctx = oc.transpose(0, 3, 1, 2, 4).reshape(b_, qc.shape[1], GQA_HEADS * GQA_HD)
    return o_lat, o_ctx


def _merge_branches(outs, gate_raw, w_branch, w_out):
    b_, t_, _ = gate_raw.shape
    yb = jnp.einsum('btnw,nwd->btnd', jnp.stack(outs, axis=2), w_branch)
    g = jax.nn.sigmoid(gate_raw.reshape(b_, t_, N_BRANCH, D_MODEL))
    return jnp.sum(g * yb, axis=2) @ w_out


def _moe(h, w_router, b_router, w_gu, b_gu, w_down, b_down):
    n_tok, d = h.shape
    logits = (h @ w_router).astype(jnp.float32) + b_router.astype(jnp.float32)
    top_val, top_idx = lax.top_k(logits, TOP_K)
    gate = jax.nn.softmax(top_val, axis=-1)
    nk = n_tok * TOP_K
    flat_e = top_idx.reshape(nk).astype(jnp.int32)
    flat_t = jnp.arange(nk, dtype=jnp.int32) // TOP_K
    order = jnp.argsort(flat_e)
    e_s, t_s, g_s = flat_e[order], flat_t[order], gate.reshape(nk)[order]
    counts = jnp.zeros((N_EXPERTS,), jnp.int32).at[flat_e].add(1)
    padded = (counts + MOE_BLOCK - 1) // MOE_BLOCK * MOE_BLOCK
    start_s = jnp.cumsum(counts) - counts
    ends_p = jnp.cumsum(padded)
    start_p = ends_p - padded
    dest = start_p[e_s] + (jnp.arange(nk, dtype=jnp.int32) - start_s[e_s])
    n_blocks = (nk + N_EXPERTS * (MOE_BLOCK - 1) + MOE_BLOCK - 1) // MOE_BLOCK
    n_slots = n_blocks * MOE_BLOCK
    slot_tok = jnp.full((n_slots,), n_tok, jnp.int32).at[dest].set(t_s)
    slot_gate = jnp.zeros((n_slots,), jnp.float32).at[dest].set(g_s)
    block_start = jnp.arange(n_blocks, dtype=jnp.int32) * MOE_BLOCK
    block_e = jnp.minimum(jnp.sum(ends_p[None, :] <= block_start[:, None], axis=1), N_EXPERTS - 1)
    h_pad = jnp.concatenate([h, jnp.zeros((1, d), h.dtype)], axis=0)
    xb = h_pad[slot_tok].reshape(n_blocks, MOE_BLOCK, d)

    def expert_block(args):
        xe, e = args
        gu = xe @ w_gu[e] + b_gu[e]
        g_, u_ = gu[:, :D_FF_EXPERT], gu[:, D_FF_EXPERT:]
        g_ = jnp.minimum(g_, SWIGLU_LIMIT)
        u_ = jnp.clip(u_, -SWIGLU_LIMIT, SWIGLU_LIMIT)
        act = g_ * jax.nn.sigmoid(SWIGLU_ALPHA * g_) * (u_ + 1.0)
        return act @ w_down[e] + b_down[e]

    yb = lax.map(expert_block, (xb, block_e))
    y = yb.reshape(n_slots, d) * slot_gate[:, None].astype(yb.dtype)
    return jnp.zeros((n_tok + 1, d), y.dtype).at[slot_tok].add(y)[:n_tok]


def setup_inputs(seed: int = 0) -> dict:
    key = jax.random.key(seed)
    ks = jax.random.split(key, 26)

    def nrm(k, shape, scale):
        return jax.random.normal(k, shape, jnp.float32) * scale

    d = D_MODEL
    p = 5.0 + jnp.arange(RET_HEADS, dtype=jnp.float32)
    ret_base = jnp.log(2.0 ** p - 1.0)
    return {
        'x': nrm(ks[0], (BATCH, SEQ, d), 1.0),
        'c': nrm(ks[1], (BATCH, d), 1.0),
        'ctx': nrm(ks[2], (BATCH, CTX_LEN, d), 1.0),
        'c_ctx': nrm(ks[3], (d,), 1.0),
        'w_mod': nrm(ks[4], (DEPTH, d, 6 * d), d ** -0.5),
        'b_mod': nrm(ks[5], (DEPTH, 6 * d), 0.02),
        'w_in': nrm(ks[6], (DEPTH, d, PROJ_TOTAL), d ** -0.5),
        'ret_decay': ret_base[None, None, :] + nrm(ks[7], (DEPTH, 2, RET_HEADS), 0.05),
        'ret_gn_g': 1.0 + nrm(ks[8], (DEPTH, BRANCH_W), 0.02),
        'ret_gn_b': nrm(ks[9], (DEPTH, BRANCH_W), 0.02),
        'hg_lb': 1.0 + nrm(ks[10], (2, DEPTH, BRANCH_W), 0.1),
        'hg_norm_g': 1.0 + nrm(ks[11], (DEPTH, BRANCH_W), 0.02),
        'na_rpb': nrm(ks[12], (DEPTH, NA_HEADS, 2 * NA_KH - 1, 2 * NA_KW - 1), 0.1),
        'gq_qn_g': 1.0 + nrm(ks[13], (DEPTH, GQA_HD), 0.02),
        'gq_kn_g': 1.0 + nrm(ks[14], (DEPTH, GQA_HD), 0.02),
        'w_branch': nrm(ks[15], (DEPTH, N_BRANCH, BRANCH_W, d), BRANCH_W ** -0.5),
        'w_out': nrm(ks[16], (DEPTH, d, d), d ** -0.5 * DN_BETA),
        'ln_g': 1.0 + nrm(ks[17], (DEPTH, 2, d), 0.02),
        'ln_b': nrm(ks[18], (DEPTH, 2, d), 0.02),
        'w_router': nrm(ks[19], (DEPTH, d, N_EXPERTS), d ** -0.5),
        'b_router': nrm(ks[20], (DEPTH, N_EXPERTS), 0.01),
        'w_gu': nrm(ks[21], (DEPTH, N_EXPERTS, d, 2 * D_FF_EXPERT), d ** -0.5),
        'b_gu': nrm(ks[22], (DEPTH, N_EXPERTS, 2 * D_FF_EXPERT), 0.02),
        'w_down': nrm(ks[23], (DEPTH, N_EXPERTS, D_FF_EXPERT, d), D_FF_EXPERT ** -0.5 * DN_BETA),
        'b_down': nrm(ks[24], (DEPTH, N_EXPERTS, d), 0.02),
    }


def reference(x, c, ctx, c_ctx, w_mod, b_mod, w_in, ret_decay, ret_gn_g, ret_gn_b, hg_lb, hg_norm_g,
              na_rpb, gq_qn_g, gq_kn_g, w_branch, w_out, ln_g, ln_b, w_router, b_router, w_gu, b_gu,
              w_down, b_down):
    b_, t_, d = x.shape
    row, col = _grid_pos(t_)
    sm = jax.nn.softmax(hg_lb.astype(jnp.float32), axis=1)
    lower = jnp.cumsum(sm, axis=1) - sm[:, :1]
    split_at = np.cumsum(PROJ_WIDTHS)[:-1].tolist()
    silu_c = jax.nn.silu(c)
    silu_cc = jax.nn.silu(c_ctx)
    xc = ctx
    for l in range(DEPTH):
        last = l == DEPTH - 1
        sh1, sc1, g1, sh2, sc2, g2 = jnp.split((silu_c @ w_mod[l] + b_mod[l])[:, None, :], 6, axis=-1)
        csh1, csc1, cg1, csh2, csc2, cg2 = jnp.split(silu_cc @ w_mod[l] + b_mod[l], 6, axis=-1)
        h = x * (1.0 + sc1) + sh1
        hc = xc * (1.0 + csc1) + csh1
        z = jnp.split(h @ w_in[l], split_at, axis=-1)
        zc = jnp.split(hc @ w_in[l], split_at, axis=-1)
        ret_l, ret_c = _retention(z[0:4], zc[0:4], ret_decay[l], ret_gn_g[l], ret_gn_b[l], row, col)
        hg_l, hg_c = _hgrn2(z[4:9], zc[4:9], lower[:, l], hg_norm_g[l])
        na_l, na_c = _neighbourhood_attn(z[9], z[10], z[11], zc[9], zc[10], zc[11], na_rpb[l])
        gq_l, gq_c = _gqa(z[12], z[13], z[14], zc[12], zc[13], zc[14], gq_qn_g[l], gq_kn_g[l], row, col)
        y = _merge_branches((ret_l, hg_l, na_l, gq_l), z[15], w_branch[l], w_out[l])
        x = _layernorm(DN_ALPHA * x + g1 * y, ln_g[l, 0], ln_b[l, 0])
        h2 = (x * (1.0 + sc2) + sh2).reshape(b_ * t_, d)
        if last:
            y2 = _moe(h2, w_router[l], b_router[l], w_gu[l], b_gu[l], w_down[l], b_down[l]).reshape(b_, t_, d)
        else:
            yc = _merge_branches((ret_c, hg_c, na_c, gq_c), zc[15], w_branch[l], w_out[l])
            xc = _layernorm(DN_ALPHA * xc + cg1 * yc, ln_g[l, 0], ln_b[l, 0])
            h2c = (xc * (1.0 + csc2) + csh2).reshape(-1, d)
            y_all = _moe(jnp.concatenate([h2, h2c], axis=0), w_router[l], b_router[l], w_gu[l], b_gu[l],
                         w_down[l], b_down[l])
            y2 = y_all[: b_ * t_].reshape(b_, t_, d)
            y2c = y_all[b_ * t_:].reshape(xc.shape)
            xc = _layernorm(DN_ALPHA * xc + cg2 * y2c, ln_g[l, 1], ln_b[l, 1])
        x = _layernorm(DN_ALPHA * x + g2 * y2, ln_g[l, 1], ln_b[l, 1])
    return x
```

```python
import numpy as np
import concourse.bass as bass
import concourse.mybir as mybir
from concourse.bass_utils import run_bass_kernel_spmd

F32 = mybir.dt.float32
BF16 = mybir.dt.bfloat16
AF = mybir.ActivationFunctionType
ALU = mybir.AluOpType
AX = mybir.AxisListType

D = 1024; T = 2048; LC = 256; NT = 2304; DEPTH = 4
NCORES = 4
NB = 8 // NCORES
TBS = [(0, 512), (512, 512), (1024, 512), (1536, 512), (2048, 256)]
WIN_COLS = 12672
C_RQ, C_RK, C_RV, C_RG = 0, 512, 1024, 1536
C_HQ, C_HF, C_HB, C_HI, C_HG = 2048, 2560, 3072, 3584, 4096
C_NQ, C_NK, C_NV = 4608, 5120, 5632
C_GQ, C_GK, C_GV = 6144, 6656, 6784
C_GATE = 6912
C_RQS, C_RKS, C_GQS, C_GKS = 11008, 11520, 12032, 12544
DN_ALPHA = (2 * DEPTH) ** 0.25
LN_EPS = 1e-5
NORM_EPS = 1e-6


class Buf:
    def __init__(self, ap, name):
        self.ap = ap; self.name = name
        self.writes = {}; self.reads = {}; self.dsem = None; self.dcnt = 0
        self.is_psum = name.startswith("ps")

    def __getitem__(self, idx):
        return self.ap[idx]


class Ring:
    def __init__(self, tiles):
        self.tiles = tiles; self.i = 0

    def next(self):
        t = self.tiles[self.i % len(self.tiles)]; self.i += 1
        return t


class Sched:
    def __init__(self, nc):
        self.nc = nc
        self.E = {}
        for n, e in (("pe", nc.tensor), ("act", nc.scalar), ("dve", nc.vector),
                     ("pool", nc.gpsimd), ("sp", nc.sync)):
            self.E[n] = dict(e=e, sem=nc.alloc_semaphore("s_" + n), cnt=0, seen={})
        self.bsem = nc.alloc_semaphore("s_bar"); self.bcnt = 0
        self.dsems = []; self.free_dsems = []
        self.nwait = 0; self.nins = 0; self.uid = 0

    def sb(self, name, shape, dt):
        self.uid += 1
        return Buf(self.nc.alloc_sbuf_tensor("%s_%d" % (name, self.uid), list(shape), dt).ap(), name)

    def dram(self, name, shape, dt, kind="Internal"):
        return Buf(self.nc.dram_tensor(name, list(shape), dt, kind=kind).ap(), name)

    def _wait(self, en, events):
        E = self.E[en]
        for sem, val in events.items():
            if E["seen"].get(sem, 0) >= val:
                continue
            E["e"].wait_ge(sem, val)
            E["seen"][sem] = val
            self.nwait += 1

    @staticmethod
    def _merge(dst, src):
        for s, v in src.items():
            if v > dst.get(s, 0):
                dst[s] = v

    def op(self, en, fn, reads=(), writes=(), inc=True):
        E = self.E[en]
        ev = {}
        for t in reads:
            self._merge(ev, t.writes)
            if t.is_psum and en != "pe":
                self._merge(ev, t.reads)
        for t in writes:
            self._merge(ev, t.writes); self._merge(ev, t.reads)
        if ev.get(E["sem"], 0) > E["cnt"]:
            del ev[E["sem"]]
        self._wait(en, ev)
        ins = fn(E["e"])
        self.nins += 1
        nxt = E["cnt"] + 1
        if inc:
            ins.then_inc(E["sem"], 1); E["cnt"] = nxt
        me = {E["sem"]: nxt}
        for t in reads:
            self._merge(t.reads, me)
        for t in writes:
            t.writes = dict(me); t.reads = {}
        return ins

    def dma(self, en, out_t, out_ap, in_t, in_ap, join=False, **kw):
        E = self.E[en]
        if out_t.dsem is None:
            out_t.dsem = self.nc.alloc_semaphore("d%d_%s" % (len(self.dsems), out_t.name))
            self.dsems.append(out_t)
        ev = {}
        self._merge(ev, in_t.writes)
        w = dict(out_t.writes)
        if join:
            w.pop(out_t.dsem, None)
        self._merge(ev, w); self._merge(ev, out_t.reads)
        self._wait(en, ev)
        ins = E["e"].dma_start(out=out_ap, in_=in_ap, **kw)
        self.nins += 1
        ins.then_inc(out_t.dsem, 16)
        out_t.dcnt += 16
        me = {out_t.dsem: out_t.dcnt}
        self._merge(in_t.reads, me)
        out_t.writes = dict(me); out_t.reads = {}
        return ins

    def barrier(self):
        ev = {}
        for n, E in self.E.items():
            if n != "sp" and E["cnt"] > 0:
                ev[E["sem"]] = E["cnt"]
        for t in self.dsems:
            ev[t.dsem] = t.dcnt
        self._wait("sp", ev)
        self.bcnt += 1
        self.nc.sync.sem_inc(self.bsem, 1)
        for n in self.E:
            if n != "sp":
                self._wait(n, {self.bsem: self.bcnt})


from contextlib import ExitStack, contextmanager


class SchedX(Sched):
    def __init__(self, nc):
        super().__init__(nc)
        self.es = None
        self.scope_tiles = None
        self.dsem_cnt = {}
        self.free_sems = []

    @contextmanager
    def scope(self):
        assert self.es is None
        with ExitStack() as es:
            self.es = es; self.scope_tiles = []
            yield
            self.barrier()
            for t in self.scope_tiles:
                if t.dsem is not None:
                    self.free_sems.append(t.dsem); t.dsem = None
            self.es = None; self.scope_tiles = None

    def sb(self, name, shape, dt, persist=False):
        self.uid += 1
        nm = "%s_%d" % (name, self.uid)
        if self.es is None or persist:
            h = self.nc.alloc_sbuf_tensor(nm, list(shape), dt)
            return Buf(h.ap(), name)
        h = self.es.enter_context(self.nc.sbuf_tensor(nm, list(shape), dt))
        t = Buf(h.ap(), name)
        self.scope_tiles.append(t)
        return t

    def _get_dsem(self, t):
        if t.dsem is None:
            if self.free_sems:
                t.dsem = self.free_sems.pop()
            else:
                t.dsem = self.nc.alloc_semaphore("d%d" % len(self.dsem_cnt))
                self.dsem_cnt[t.dsem] = 0
        return t.dsem

    def dma(self, en, out_t, out_ap, in_t, in_ap, join=False, cc=None, **kw):
        E = self.E[en]
        sem = self._get_dsem(out_t)
        ev = {}
        self._merge(ev, in_t.writes)
        w = dict(out_t.writes)
        if join:
            w.pop(sem, None)
        self._merge(ev, w); self._merge(ev, out_t.reads)
        self._wait(en, ev)
        if cc is None:
            ins = E["e"].dma_start(out=out_ap, in_=in_ap, **kw)
        else:
            ins = E["e"].collective_compute(cc, ALU.bypass, replica_groups=[list(range(NCORES))],
                                            ins=[in_ap], outs=[out_ap])
        self.nins += 1
        ins.then_inc(sem, 16)
        self.dsem_cnt[sem] += 16
        me = {sem: self.dsem_cnt[sem]}
        self._merge(in_t.reads, me)
        out_t.writes = dict(me); out_t.reads = {}
        return ins

    def barrier(self):
        ev = {}
        for n, E in self.E.items():
            if n != "sp" and E["cnt"] > 0:
                ev[E["sem"]] = E["cnt"]
        for sem, c in self.dsem_cnt.items():
            if c > 0:
                ev[sem] = c
        self._wait("sp", ev)
        self.bcnt += 1
        self.nc.sync.sem_inc(self.bsem, 1)
        for n in self.E:
            if n != "sp":
                self._wait(n, {self.bsem: self.bcnt})


def _bcast_mid(ap, n):
    return ap.unsqueeze(1).to_broadcast([ap.shape[0], n, ap.shape[1]])


def _bcast_last(ap, n):
    return ap.unsqueeze(2).to_broadcast([ap.shape[0], ap.shape[1], n])


class Builder:
    def __init__(self, nc, layers=range(DEPTH), gather=False, n_experts=32, dbg=(), full=False):
        self.nc = nc
        self.S = S = SchedX(nc)
        self.layers = list(layers); self.n_experts = n_experts
        self.dbg = set(dbg)
        ein = lambda n, s, dt=F32: S.dram(n, s, dt, kind="ExternalInput")
        self.x_in = ein("x_in", [NB, T, D]); self.ctx_in = ein("ctx_in", [NB, LC, D]); self.cT_in = ein("cT", [NB, 128, 8, 2])
        R = NCORES if gather else 1
        nlw = DEPTH if full else 1
        ne = 32 if full else n_experts
        self.wshapes = dict(w_in=(nlw * 1024, WIN_COLS), w_mod=(nlw * 1024, 6144), w_br=(nlw * 2048, 1024),
                            w_out=(nlw * 1024, 1024), w_gu=(nlw * ne * 1024, 2048), w_dn=(nlw * ne * 1024, 1024))
        self.ne_w = ne
        self.W = {}
        for k, (r, c) in self.wshapes.items():
            if gather:
                sh = ein(k + "_s", [r // NCORES, c])
                full = S.dram(k + "_f", [r, c], F32)
                S.dma("pool", full, full[:], sh, sh[:], cc="AllGather")
                self.W[k] = full
            else:
                self.W[k] = ein(k + "_s", [r, c])
        self.b_modT = ein("b_modT", [4, 128, 48]); self.decb = ein("decb", [4, 128, 8])
        self.gn_gT = ein("gn_gT", [4, 128, 4]); self.gn_bT = ein("gn_bT", [4, 128, 4])
        self.hg_lbT = ein("hg_lbT", [128, 2, 4, 4]); self.hg_ngT = ein("hg_ngT", [4, 128, 4])
        self.na_T = ein("na_T", [4, 8, 64, 15, 64])
        self.gq_qg = ein("gq_qg", [4, 64, 2]); self.gq_kg = ein("gq_kg", [4, 64, 2])
        self.ln_gT = ein("ln_gT", [4, 2, 128, 8]); self.ln_bT = ein("ln_bT", [4, 2, 128, 8])
        self.w_router = ein("w_router", [4, 1024, 32]); self.b_router_b = ein("b_router_b", [4, 128, 32])
        self.b_guT = ein("b_guT", [4, 128, 32, 16]); self.b_dn = ein("b_dn", [4, 32, 1024])
        self.c_ident = ein("ident", [128, 128])
        self.c_rcos = ein("ret_cos", [128, NT]); self.c_rsin = ein("ret_sin", [128, NT])
        self.c_gcos = ein("gq_cos", [64, NT]); self.c_gsin = ein("gq_sin", [64, NT])
        self.c_delta = ein("delta", [128, 512]); self.c_cvals = ein("cvals", [128, 21]); self.c_m0 = ein("m0", [128, 4, 512])
        self.c_rmask = ein("rmask", [128, NT]); self.c_tril = ein("tril", [32, 2, 32])
        self.c_validC2 = ein("validC2", [128, 64]); self.c_sel = ein("sel", [32, 32, 128])
        self.out = S.dram("out", [NB, T, D], F32, kind="ExternalOutput")
        self.bi = 0
        self.XT = S.dram("XT", [8, 128, NT], F32); self.HT = S.dram("HT", [8, 128, NT], BF16)
        self.H2T = S.dram("H2T", [8, 128, NT], BF16)
        self.OB = [S.dram("OB0", [4, 128, NT], BF16), S.dram("OB1", [4, 128, NT], BF16),
                   S.dram("OB2", [8, 64, NT], BF16), S.dram("OB3", [8, 64, NT], BF16)]
        self.dbg_out = {}
        self.ident = S.sb("ident", [128, 128], F32)
        S.dma("sp", self.ident, self.ident[:], self.c_ident, self.c_ident[:])
        self.onesb = S.sb("onesb", [128, 128], BF16)
        S.op("dve", lambda e: e.memset(self.onesb[:], 1.0), writes=[self.onesb])
        self.modT = S.sb("modT", [128, 48, 2], F32)
        self.ps = [Buf(nc.alloc_psum_tensor("ps%d" % i, [128, 512], F32).ap(), "ps%d" % i) for i in range(8)]
        self.psr = Ring(self.ps)
        self.epsc = {}
        for eps in (LN_EPS, NORM_EPS):
            et = S.sb("epsc", [128, 1], F32)
            S.op("dve", lambda e, et=et, eps=eps: e.memset(et[:], eps), writes=[et])
            self.epsc[eps] = et

    def dbg_dump(self, name, src_t, src_ap, shape, dt=F32):
        if name not in self.dbg:
            return
        o = self.S.dram("dbg_" + name, list(shape), dt, kind="ExternalOutput")
        self.S.dma("sp", o, o[:], src_t, src_ap)
        self.dbg_out[name] = o

    def mm(self, pt, pap, lt, lap, rt, rap, start=True, stop=True):
        self.S.op("pe", lambda e: e.matmul(pap, lhsT=lap, rhs=rap, start=start, stop=stop),
                  reads=[lt, rt], writes=[pt], inc=stop)

    def load_w(self, dst_t, dst_ap, key, row0, nk, col0, ncols, en="pool", join=True):
        w = self.W[key]
        src = w[row0:row0 + nk * 128, col0:col0 + ncols].rearrange("(k p) n -> p k n", p=128)
        self.S.dma(en, dst_t, dst_ap, w, src, join=join)

    def act(self, ot, oap, it, iap, func, bias=None, scale=1.0, rd=()):
        kw = {}
        if bias is not None:
            kw["bias"] = bias
        self.S.op("act", lambda e: e.activation(out=oap, in_=iap, func=func, scale=scale, **kw),
                  reads=[it] + list(rd), writes=[ot])

    def tt(self, en, ot, oap, at, aap, bt, bap, op):
        self.S.op(en, lambda e: e.tensor_tensor(out=oap, in0=aap, in1=bap, op=op), reads=[at, bt], writes=[ot])

    def ts(self, en, ot, oap, it, iap, s1, s2, op0, op1=None, rd=()):
        if op1 is None:
            self.S.op(en, lambda e: e.tensor_scalar(out=oap, in0=iap, scalar1=s1, scalar2=None, op0=op0),
                      reads=[it] + list(rd), writes=[ot])
        else:
            self.S.op(en, lambda e: e.tensor_scalar(out=oap, in0=iap, scalar1=s1, scalar2=s2, op0=op0, op1=op1),
                      reads=[it] + list(rd), writes=[ot])

    def stt(self, en, ot, oap, at, aap, scalar, bt, bap, op0, op1, rd=()):
        self.S.op(en, lambda e: e.scalar_tensor_tensor(out=oap, in0=aap, scalar=scalar, in1=bap, op0=op0, op1=op1),
                  reads=[at, bt] + list(rd), writes=[ot])

    def rsqrt_eps(self, t, ap, eps, mult=1.0):
        if self.epsc is None:
            self.epsc = {}
        if eps not in self.epsc:
            et = self.S.sb("epsc", [128, 1], F32, persist=True)
            self.S.op("dve", lambda e: e.memset(et[:], eps), writes=[et])
            self.epsc[eps] = et
        et = self.epsc[eps]
        self.act(t, ap, t, ap, AF.Ln, bias=et[0:ap.shape[0], 0:1], scale=mult, rd=[et])
        self.act(t, ap, t, ap, AF.Exp, scale=-0.5)

    def phase_init(self):
        S = self.S
        with S.scope():
            xin = Ring([S.sb("xin", [128, D], F32) for _ in range(3)])
            xtb = Ring([S.sb("xtb", [128, 8, 512], F32) for _ in range(2)])
            for (t0, n) in TBS:
                ob = xtb.next()
                tiles = []
                for j in range(n // 128):
                    xt = xin.next()
                    if t0 < T:
                        S.dma("sp", xt, xt[:], self.x_in, self.x_in[self.bi, t0 + j * 128:t0 + (j + 1) * 128, :])
                    else:
                        S.dma("sp", xt, xt[:], self.ctx_in, self.ctx_in[self.bi, t0 - T + j * 128:t0 - T + (j + 1) * 128, :])
                    tiles.append(xt)
                    if len(tiles) == 2 or j == n // 128 - 1:
                        j0 = j + 1 - len(tiles)
                        for c in range(8):
                            p = self.psr.next()
                            for jj, xt_ in enumerate(tiles):
                                S.op("pe", lambda e, p=p, jj=jj, xt_=xt_, c=c: e.transpose(
                                    out=p[:, jj * 128:(jj + 1) * 128], in_=xt_[:, c * 128:(c + 1) * 128],
                                    identity=self.ident[:]), reads=[xt_, self.ident], writes=[p])
                            w = len(tiles) * 128
                            S.op("act" if c % 2 else "dve",
                                 (lambda e, p=p, c=c, j0=j0, w=w, ob=ob: e.copy(out=ob[:, c, j0 * 128:j0 * 128 + w], in_=p[:, 0:w]))
                                 if c % 2 else
                                 (lambda e, p=p, c=c, j0=j0, w=w, ob=ob: e.tensor_copy(out=ob[:, c, j0 * 128:j0 * 128 + w], in_=p[:, 0:w])),
                                 reads=[p], writes=[ob])
                        tiles = []
                S.dma("sp", self.XT, self.XT[:, :, t0:t0 + n].rearrange("c p t -> p c t"), ob, ob[:, :, 0:n])

    def phase_final(self):
        S = self.S
        with S.scope():
            xtb = Ring([S.sb("xtb", [128, 8, 512], F32) for _ in range(2)])
            ot = Ring([S.sb("otile", [128, D], F32) for _ in range(3)])
            for (t0, n) in TBS[:4]:
                xb = xtb.next()
                S.dma("sp", xb, xb[:], self.XT, self.XT[:, :, t0:t0 + n].rearrange("c p t -> p c t"))
                for j in range(4):
                    o = ot.next()
                    for g in range(2):
                        p = self.psr.next()
                        for cc in range(4):
                            c = g * 4 + cc
                            S.op("pe", lambda e, p=p, cc=cc, c=c, j=j, xb=xb: e.transpose(
                                out=p[:, cc * 128:(cc + 1) * 128], in_=xb[:, c, j * 128:(j + 1) * 128],
                                identity=self.ident[:]), reads=[xb, self.ident], writes=[p])
                        if g:
                            S.op("act", lambda e, p=p, o=o, g=g: e.copy(out=o[:, g * 512:(g + 1) * 512], in_=p[:]), reads=[p], writes=[o])
                        else:
                            S.op("dve", lambda e, p=p, o=o, g=g: e.tensor_copy(out=o[:, g * 512:(g + 1) * 512], in_=p[:]), reads=[p], writes=[o])
                    S.dma("sp", self.out, self.out[self.bi, t0 + j * 128:t0 + (j + 1) * 128, :], o, o[:], join=True)

    def finish(self):
        S = self.S
        ev = {}
        S._merge(ev, self.out.writes)
        for o in self.dbg_out.values():
            S._merge(ev, o.writes)
        S._wait("sp", ev)
        S.barrier()


class Builder2(Builder):
    def phase_mod(self, l, lw):
        S = self.S
        with S.scope():
            cT = S.sb("cT", [128, 8, 2], F32); scT = S.sb("scT", [128, 8, 2], BF16)
            bm = S.sb("bm", [128, 48], F32)
            S.dma("sp", cT, cT[:], self.cT_in, self.cT_in[self.bi])
            S.dma("sp", bm, bm[:], self.b_modT, self.b_modT[l])
            self.act(scT, scT[:], cT, cT[:], AF.Silu)
            wr = Ring([S.sb("wmod", [128, 8, 1024], BF16) for _ in range(2)])
            pm = self.psr.next()
            for piece in range(6):
                wt = wr.next()
                self.load_w(wt, wt[:], "w_mod", lw * 1024, 8, piece * 1024, 1024, join=False)
                for j in range(8):
                    jj = piece * 8 + j
                    for k in range(8):
                        self.mm(pm, pm[:, jj * 2:jj * 2 + 2], wt, wt[:, k, j * 128:(j + 1) * 128], scT, scT[:, k, :],
                                start=(k == 0), stop=(k == 7))
            mt = self.modT
            self.tt("dve", mt, mt[:], pm, pm[:, 0:96].rearrange("p (j s) -> p j s", s=2), bm, _bcast_last(bm[:], 2), ALU.add)
            for w in (1, 4):
                self.ts("dve", mt, mt[:, w * 8:(w + 1) * 8, :], mt, mt[:, w * 8:(w + 1) * 8, :], 1.0, None, ALU.add)
            self.dbg_dump("modT", mt, mt[:], [128, 48, 2])

    def phase_hT(self, which, src, dst):
        S = self.S
        mt = self.modT
        with S.scope():
            xb_r = Ring([S.sb("xb", [128, 8, 512], F32) for _ in range(2)])
            hb_r = Ring([S.sb("hb", [128, 8, 512], BF16) for _ in range(2)])
            for (t0, n) in TBS:
                s = 0 if t0 < T else 1
                xb = xb_r.next(); hb = hb_r.next()
                S.dma("sp", xb, xb[:, :, 0:n], src, src[:, :, t0:t0 + n].rearrange("c p t -> p c t"))
                for c in range(8):
                    self.act(hb, hb[:, c, 0:n], xb, xb[:, c, 0:n], AF.Identity,
                             bias=mt[:, which * 24 + c, s:s + 1], scale=mt[:, which * 24 + 8 + c, s:s + 1], rd=[mt])
                S.dma("sp", dst, dst[:, :, t0:t0 + n].rearrange("c p t -> p c t"), hb, hb[:, :, 0:n])

    def ln_block(self, xn, n, l, i, g_t, b_t, sq, onesf, rstd, mean_sb):
        S = self.S
        pmean = self.psr.next(); pex2 = self.psr.next()
        for c in range(8):
            self.act(sq, sq[:, c, 0:n], xn, xn[:, c, 0:n], AF.Square)
        for c in range(8):
            self.mm(pmean, pmean[:, 0:n], onesf, onesf[:], xn, xn[:, c, 0:n], start=(c == 0), stop=(c == 7))
        for c in range(8):
            self.mm(pex2, pex2[:, 0:n], onesf, onesf[:], sq, sq[:, c, 0:n], start=(c == 0), stop=(c == 7))
        self.act(mean_sb, mean_sb[:, 0:n], pmean, pmean[:, 0:n], AF.Identity)
        self.tt("dve", rstd, rstd[:, 0:n], mean_sb, mean_sb[:, 0:n], mean_sb, mean_sb[:, 0:n], ALU.mult)
        self.tt("dve", rstd, rstd[:, 0:n], pex2, pex2[:, 0:n], rstd, rstd[:, 0:n], ALU.subtract)
        self.rsqrt_eps(rstd, rstd[:, 0:n], LN_EPS)
        for c in range(8):
            self.tt("dve", xn, xn[:, c, 0:n], xn, xn[:, c, 0:n], mean_sb, mean_sb[:, 0:n], ALU.subtract)
            self.tt("pool", xn, xn[:, c, 0:n], xn, xn[:, c, 0:n], rstd, rstd[:, 0:n], ALU.mult)
            self.act(xn, xn[:, c, 0:n], xn, xn[:, c, 0:n], AF.Identity, bias=b_t[:, c:c + 1], scale=g_t[:, c:c + 1], rd=[g_t, b_t])

    def phase_merge(self, l, lw):
        S = self.S
        mt = self.modT
        with S.scope():
            wg = S.sb("wg", [128, 8, 4096], BF16)
            wb01 = S.sb("wb01", [128, 2, 4, 1024], BF16); wb23 = S.sb("wb23", [64, 2, 8, 1024], BF16)
            wo = S.sb("wo", [128, 8, 1024], BF16)
            for n4 in range(4):
                self.load_w(wg, wg[:, :, n4 * 1024:(n4 + 1) * 1024], "w_in", lw * 1024, 8, C_GATE + n4 * 1024, 1024)
            wbr = self.W["w_br"]
            for n in range(2):
                S.dma("pool", wb01, wb01[:, n], wbr, wbr[lw * 2048 + n * 512: lw * 2048 + (n + 1) * 512, :].rearrange("(k p) n -> p k n", p=128), join=True)
            for n in range(2):
                S.dma("pool", wb23, wb23[:, n], wbr, wbr[lw * 2048 + (n + 2) * 512: lw * 2048 + (n + 3) * 512, :].rearrange("(k p) n -> p k n", p=64), join=True)
            self.load_w(wo, wo[:], "w_out", lw * 1024, 8, 0, 1024)
            g_t = S.sb("lng", [128, 8], F32); b_t = S.sb("lnb", [128, 8], F32)
            S.dma("sp", g_t, g_t[:], self.ln_gT, self.ln_gT[l, 0]); S.dma("sp", b_t, b_t[:], self.ln_bT, self.ln_bT[l, 0])
            onesf = S.sb("onesf", [128, 128], F32)
            S.op("dve", lambda e: e.memset(onesf[:], 1.0 / D), writes=[onesf])
            hb_r = Ring([S.sb("hb", [128, 8, 512], BF16) for _ in range(2)])
            o01_r = Ring([S.sb("o01", [128, 2, 4, 512], BF16) for _ in range(2)])
            o23_r = Ring([S.sb("o23", [64, 2, 8, 512], BF16) for _ in range(2)])
            xb_r = Ring([S.sb("xb", [128, 8, 512], F32) for _ in range(2)])
            mb = S.sb("mb", [128, 8, 512], BF16)
            sg_r = Ring([S.sb("sg", [128, 512], F32) for _ in range(3)])
            macc_r = Ring([S.sb("macc", [128, 512], F32) for _ in range(2)])
            tmp_r = Ring([S.sb("tmpm", [128, 512], F32) for _ in range(2)])
            sq = S.sb("sq", [128, 8, 512], F32)
            rstd = S.sb("rstd", [128, 512], F32); mean_sb = S.sb("mean_sb", [128, 512], F32)
            h2b_r = Ring([S.sb("h2b", [128, 8, 512], BF16) for _ in range(2)])
            for (t0, n) in TBS:
                s = 0 if t0 < T else 1
                hb = hb_r.next(); o01 = o01_r.next(); o23 = o23_r.next(); xb = xb_r.next()
                S.dma("sp", hb, hb[:, :, 0:n], self.HT, self.HT[:, :, t0:t0 + n].rearrange("c p t -> p c t"))
                for nb in range(2):
                    S.dma("sp", o01, o01[:, nb, :, 0:n], self.OB[nb], self.OB[nb][:, :, t0:t0 + n].rearrange("c p t -> p c t"), join=True)
                    S.dma("sp", o23, o23[:, nb, :, 0:n], self.OB[2 + nb], self.OB[2 + nb][:, :, t0:t0 + n].rearrange("c p t -> p c t"), join=True)
                S.dma("sp", xb, xb[:, :, 0:n], self.XT, self.XT[:, :, t0:t0 + n].rearrange("c p t -> p c t"))
                for oc in range(8):
                    macc = macc_r.next()
                    for nb in range(4):
                        pg = self.psr.next(); py = self.psr.next()
                        for k in range(8):
                            self.mm(pg, pg[:, 0:n], wg, wg[:, k, nb * 1024 + oc * 128: nb * 1024 + (oc + 1) * 128], hb, hb[:, k, 0:n],
                                    start=(k == 0), stop=(k == 7))
                        sg = sg_r.next()
                        self.act(sg, sg[:, 0:n], pg, pg[:, 0:n], AF.Sigmoid)
                        if nb < 2:
                            for c in range(4):
                                self.mm(py, py[:, 0:n], wb01, wb01[:, nb, c, oc * 128:(oc + 1) * 128], o01, o01[:, nb, c, 0:n],
                                        start=(c == 0), stop=(c == 3))
                        else:
                            for c in range(8):
                                self.mm(py, py[:, 0:n], wb23, wb23[:, nb - 2, c, oc * 128:(oc + 1) * 128], o23, o23[:, nb - 2, c, 0:n],
                                        start=(c == 0), stop=(c == 7))
                        if nb == 0:
                            self.tt("dve", macc, macc[:, 0:n], sg, sg[:, 0:n], py, py[:, 0:n], ALU.mult)
                        else:
                            tmp = tmp_r.next()
                            self.tt("dve", tmp, tmp[:, 0:n], sg, sg[:, 0:n], py, py[:, 0:n], ALU.mult)
                            if nb < 3:
                                self.tt("pool", macc, macc[:, 0:n], macc, macc[:, 0:n], tmp, tmp[:, 0:n], ALU.add)
                            else:
                                self.tt("pool", mb, mb[:, oc, 0:n], macc, macc[:, 0:n], tmp, tmp[:, 0:n], ALU.add)
                for oc2 in range(8):
                    py = self.psr.next()
                    for k in range(8):
                        self.mm(py, py[:, 0:n], wo, wo[:, k, oc2 * 128:(oc2 + 1) * 128], mb, mb[:, k, 0:n], start=(k == 0), stop=(k == 7))
                    self.act(xb, xb[:, oc2, 0:n], xb, xb[:, oc2, 0:n], AF.Identity, scale=DN_ALPHA)
                    self.stt("dve", xb, xb[:, oc2, 0:n], py, py[:, 0:n], mt[:, 16 + oc2, s:s + 1], xb, xb[:, oc2, 0:n], ALU.mult, ALU.add, rd=[mt])
                self.ln_block(xb, n, l, 0, g_t, b_t, sq, onesf, rstd, mean_sb)
                S.dma("sp", self.XT, self.XT[:, :, t0:t0 + n].rearrange("c p t -> p c t"), xb, xb[:, :, 0:n])
                h2b = h2b_r.next()
                for c in range(8):
                    self.act(h2b, h2b[:, c, 0:n], xb, xb[:, c, 0:n], AF.Identity,
                             bias=mt[:, 24 + c, s:s + 1], scale=mt[:, 32 + c, s:s + 1], rd=[mt])
                S.dma("sp", self.H2T, self.H2T[:, :, t0:t0 + n].rearrange("c p t -> p c t"), h2b, h2b[:, :, 0:n])


class Builder3(Builder2):
    def __init__(self, *a, **k):
        super().__init__(*a, **k)
        self.OB[2] = self.S.dram("OB2b", [4, 128, NT], BF16); self.OB[3] = self.S.dram("OB3b", [4, 128, NT], BF16)

    def phase_merge(self, l, lw):
        S = self.S
        mt = self.modT
        with S.scope():
            wgr = Ring([S.sb("wg", [128, 8, 1024], BF16) for _ in range(2)])
            wb = S.sb("wb", [128, 4, 4, 1024], BF16)
            wo = S.sb("wo", [128, 8, 1024], BF16)
            wbr = self.W["w_br"]
            for n in range(4):
                S.dma("pool", wb, wb[:, n], wbr, wbr[lw * 2048 + n * 512: lw * 2048 + (n + 1) * 512, :].rearrange("(k p) n -> p k n", p=128), join=True)
            self.load_w(wo, wo[:], "w_out", lw * 1024, 8, 0, 1024)
            g_t = S.sb("lng", [128, 8], F32); b_t = S.sb("lnb", [128, 8], F32)
            S.dma("sp", g_t, g_t[:], self.ln_gT, self.ln_gT[l, 0]); S.dma("sp", b_t, b_t[:], self.ln_bT, self.ln_bT[l, 0])
            onesf = S.sb("onesf", [128, 128], F32)
            S.op("dve", lambda e: e.memset(onesf[:], 1.0 / D), writes=[onesf])
            hb = S.sb("hb", [128, 8, 512], BF16)
            oall = S.sb("oall", [128, 4, 4, 512], BF16)
            xb = S.sb("xb", [128, 8, 512], F32)
            mb = S.sb("mb", [128, 8, 512], BF16)
            macc8 = S.sb("macc8", [128, 8, 512], F32)
            sg_r = Ring([S.sb("sg", [128, 512], F32) for _ in range(3)])
            tmp_r = Ring([S.sb("tmpm", [128, 512], F32) for _ in range(2)])
            rstd = S.sb("rstd", [128, 512], F32); mean_sb = S.sb("mean_sb", [128, 512], F32)
            h2b = S.sb("h2b", [128, 8, 512], BF16)
            for (t0, n) in TBS:
                s = 0 if t0 < T else 1
                S.dma("sp", hb, hb[:, :, 0:n], self.HT, self.HT[:, :, t0:t0 + n].rearrange("c p t -> p c t"))
                for nb in range(4):
                    S.dma("sp", oall, oall[:, nb, :, 0:n], self.OB[nb], self.OB[nb][:, :, t0:t0 + n].rearrange("c p t -> p c t"), join=True)
                S.dma("sp", xb, xb[:, :, 0:n], self.XT, self.XT[:, :, t0:t0 + n].rearrange("c p t -> p c t"))
                for nb in range(4):
                    wg = wgr.next()
                    self.load_w(wg, wg[:], "w_in", lw * 1024, 8, C_GATE + nb * 1024, 1024, join=False)
                    for oc in range(8):
                        pg = self.psr.next(); py = self.psr.next()
                        for k in range(8):
                            self.mm(pg, pg[:, 0:n], wg, wg[:, k, oc * 128:(oc + 1) * 128], hb, hb[:, k, 0:n], start=(k == 0), stop=(k == 7))
                        sg = sg_r.next()
                        self.act(sg, sg[:, 0:n], pg, pg[:, 0:n], AF.Sigmoid)
                        for c in range(4):
                            self.mm(py, py[:, 0:n], wb, wb[:, nb, c, oc * 128:(oc + 1) * 128], oall, oall[:, nb, c, 0:n], start=(c == 0), stop=(c == 3))
                        if nb == 0:
                            self.tt("dve", macc8, macc8[:, oc, 0:n], sg, sg[:, 0:n], py, py[:, 0:n], ALU.mult)
                        else:
                            tmp = tmp_r.next()
                            self.tt("dve", tmp, tmp[:, 0:n], sg, sg[:, 0:n], py, py[:, 0:n], ALU.mult)
                            if nb < 3:
                                self.tt("pool", macc8, macc8[:, oc, 0:n], macc8, macc8[:, oc, 0:n], tmp, tmp[:, 0:n], ALU.add)
                            else:
                                self.tt("pool", mb, mb[:, oc, 0:n], macc8, macc8[:, oc, 0:n], tmp, tmp[:, 0:n], ALU.add)
                for oc2 in range(8):
                    py = self.psr.next()
                    for k in range(8):
                        self.mm(py, py[:, 0:n], wo, wo[:, k, oc2 * 128:(oc2 + 1) * 128], mb, mb[:, k, 0:n], start=(k == 0), stop=(k == 7))
                    self.act(xb, xb[:, oc2, 0:n], xb, xb[:, oc2, 0:n], AF.Identity, scale=DN_ALPHA)
                    self.stt("dve", xb, xb[:, oc2, 0:n], py, py[:, 0:n], mt[:, 16 + oc2, s:s + 1], xb, xb[:, oc2, 0:n], ALU.mult, ALU.add, rd=[mt])
                self.ln_block(xb, n, l, 0, g_t, b_t, macc8, onesf, rstd, mean_sb)
                S.dma("sp", self.XT, self.XT[:, :, t0:t0 + n].rearrange("c p t -> p c t"), xb, xb[:, :, 0:n])
                for c in range(8):
                    self.act(h2b, h2b[:, c, 0:n], xb, xb[:, c, 0:n], AF.Identity,
                             bias=mt[:, 24 + c, s:s + 1], scale=mt[:, 32 + c, s:s + 1], rd=[mt])
                S.dma("sp", self.H2T, self.H2T[:, :, t0:t0 + n].rearrange("c p t -> p c t"), h2b, h2b[:, :, 0:n])


class Builder4(Builder3):
    def __init__(self, *a, **k):
        super().__init__(*a, **k)
        self.GW = self.S.dram("GW", [32, NT], F32)

    def phase_moe(self, l, lw):
        S = self.S
        mt = self.modT
        ne = self.n_experts
        with S.scope():
            xacc = S.sb("xacc", [128, 8, NT], F32)
            for c in range(8):
                S.dma("sp", xacc, xacc[:, c, :], self.XT, self.XT[c], join=True)
            wr = S.sb("wr", [128, 8, 32], F32); brb = S.sb("brb", [128, 32], F32)
            S.dma("sp", wr, wr[:], self.w_router, self.w_router[l].rearrange("(k p) e -> p k e", p=128))
            S.dma("sp", brb, brb[:], self.b_router_b, self.b_router_b[l])
            bgu = S.sb("bgu", [128, 32, 16], F32); bdn = S.sb("bdn", [32, 1024], F32)
            S.dma("sp", bgu, bgu[:], self.b_guT, self.b_guT[l]); S.dma("sp", bdn, bdn[:], self.b_dn, self.b_dn[l])
            h2f_r = Ring([S.sb("h2f", [128, 8, 128], F32) for _ in range(2)])
            sm_r = Ring([S.sb("rsm", [128, 4, 32], F32) for _ in range(2)])
            sc_r = Ring([S.sb("rsc", [128, 16], F32) for _ in range(2)])
            gwt_r = Ring([S.sb("gwt", [32, 128], F32) for _ in range(2)])
            for j in range(NT // 128):
                s = 0 if j < 16 else 1
                h2f = h2f_r.next(); sm = sm_r.next(); sc = sc_r.next()
                for c in range(8):
                    self.act(h2f, h2f[:, c, :], xacc, xacc[:, c, j * 128:(j + 1) * 128], AF.Identity,
                             bias=mt[:, 24 + c, s:s + 1], scale=mt[:, 32 + c, s:s + 1], rd=[mt])
                pl = self.psr.next()
                for c in range(8):
                    self.mm(pl, pl[:, 0:32], h2f, h2f[:, c, :], wr, wr[:, c, :], start=(c == 0), stop=(c == 7))
                lg = sm[:, 0, :]; ex = sm[:, 1, :]; mk = sm[:, 2, :]; gw = sm[:, 3, :]
                self.tt("dve", sm, lg, pl, pl[:, 0:32], brb, brb[:], ALU.add)
                S.op("dve", lambda e, sc=sc, lg=lg: e.max(out=sc[:, 0:8], in_=lg), reads=[sm], writes=[sc])
                self.ts("dve", sc, sc[:, 8:9], sc, sc[:, 0:1], -1.0, None, ALU.mult)
                self.act(sm, ex, sm, lg, AF.Exp, bias=sc[:, 8:9], rd=[sc])
                self.ts("dve", sm, mk, sm, lg, sc[:, 3:4], None, ALU.is_ge, rd=[sc])
                self.tt("dve", sm, ex, sm, ex, sm, mk, ALU.mult)
                S.op("dve", lambda e, sc=sc, ex=ex: e.reduce_sum(out=sc[:, 9:10], in_=ex, axis=AX.X), reads=[sm], writes=[sc])
                S.op("dve", lambda e, sc=sc: e.reciprocal(out=sc[:, 10:11], in_=sc[:, 9:10]), reads=[sc], writes=[sc])
                self.ts("dve", sm, gw, sm, ex, sc[:, 10:11], None, ALU.mult, rd=[sc])
                pT = self.psr.next()
                S.op("pe", lambda e, pT=pT, gw=gw: e.transpose(out=pT[0:32, 0:128], in_=gw, identity=self.ident[:]),
                     reads=[sm, self.ident], writes=[pT])
                gwt = gwt_r.next()
                self.act(gwt, gwt[:], pT, pT[0:32, 0:128], AF.Identity)
                S.dma("sp", self.GW, self.GW[:, j * 128:(j + 1) * 128], gwt, gwt[:], join=True)
            self.dbg_dump("GW", self.GW, self.GW[:], [32, NT])
            for c in range(8):
                self.act(xacc, xacc[:, c, :], xacc, xacc[:, c, :], AF.Identity, scale=DN_ALPHA)
            wu_r = Ring([S.sb("wu", [128, 8, 512], BF16) for _ in range(6)])
            h2b_r = Ring([S.sb("h2b", [128, 8, 512], BF16) for _ in range(2)])
            act_r = Ring([S.sb("actT", [128, 8, 512], BF16) for _ in range(2)])
            gwb_r = Ring([S.sb("gwb", [128, NT], F32) for _ in range(2)])
            gwblk = S.sb("gwblk", [32, 512], F32)
            tg_r = Ring([S.sb("tg", [128, 512], F32) for _ in range(2)])
            tsg_r = Ring([S.sb("tsg", [128, 512], F32) for _ in range(2)])
            tu_r = Ring([S.sb("tu", [128, 512], F32) for _ in range(2)])
            for e_ in range(ne):
                gwb = gwb_r.next()
                S.dma("sp", gwb, gwb[:], self.GW, self.GW[e_:e_ + 1, :].partition_broadcast(128))
                row0 = (lw * self.ne_w + e_) * 1024
                units = []
                for q in range(4):
                    u = wu_r.next(); self.load_w(u, u[:], "w_gu", row0, 8, q * 512, 512, join=False); units.append(u)
                dunits = []
                for q in range(2):
                    u = wu_r.next(); self.load_w(u, u[:], "w_dn", row0, 8, q * 512, 512, join=False); dunits.append(u)
                for (t0, n) in TBS:
                    s = 0 if t0 < T else 1
                    h2b = h2b_r.next()
                    S.dma("sp", h2b, h2b[:, :, 0:n], self.H2T, self.H2T[:, :, t0:t0 + n].rearrange("c p t -> p c t"))
                    if e_ == 0:
                        S.dma("sp", gwblk, gwblk[:, 0:n], self.GW, self.GW[:, t0:t0 + n])
                    aT = act_r.next()
                    for fc in range(8):
                        ug = units[fc // 4]; uu = units[2 + fc // 4]; co = (fc % 4) * 128
                        pg = self.psr.next(); pu = self.psr.next()
                        for k in range(8):
                            self.mm(pg, pg[:, 0:n], ug, ug[:, k, co:co + 128], h2b, h2b[:, k, 0:n], start=(k == 0), stop=(k == 7))
                        for k in range(8):
                            self.mm(pu, pu[:, 0:n], uu, uu[:, k, co:co + 128], h2b, h2b[:, k, 0:n], start=(k == 0), stop=(k == 7))
                        tg = tg_r.next(); tsg = tsg_r.next(); tu = tu_r.next()
                        self.ts("dve", tg, tg[:, 0:n], pg, pg[:, 0:n], bgu[:, e_, fc:fc + 1], 7.0, ALU.add, ALU.min, rd=[bgu])
                        self.act(tsg, tsg[:, 0:n], tg, tg[:, 0:n], AF.Sigmoid, scale=1.702)
                        self.ts("dve", tu, tu[:, 0:n], pu, pu[:, 0:n], bgu[:, e_, 8 + fc:9 + fc], 7.0, ALU.add, ALU.min, rd=[bgu])
                        self.ts("pool", tu, tu[:, 0:n], tu, tu[:, 0:n], -7.0, 1.0, ALU.max, ALU.add)
                        self.tt("pool", tg, tg[:, 0:n], tg, tg[:, 0:n], tsg, tsg[:, 0:n], ALU.mult)
                        self.tt("pool", tg, tg[:, 0:n], tg, tg[:, 0:n], tu, tu[:, 0:n], ALU.mult)
                        self.tt("dve", aT, aT[:, fc, 0:n], tg, tg[:, 0:n], gwb, gwb[:, t0:t0 + n], ALU.mult)
                    for oc in range(8):
                        ud = dunits[oc // 4]; co = (oc % 4) * 128
                        py = self.psr.next()
                        for fc in range(8):
                            self.mm(py, py[:, 0:n], ud, ud[:, fc, co:co + 128], aT, aT[:, fc, 0:n], start=(fc == 0), stop=(fc == 7))
                        if e_ == 0:
                            pyb = self.psr.next()
                            self.mm(pyb, pyb[:, 0:n], bdn, bdn[:, oc * 128:(oc + 1) * 128], gwblk, gwblk[:, 0:n], start=True, stop=True)
                            self.stt("dve", xacc, xacc[:, oc, t0:t0 + n], pyb, pyb[:, 0:n], mt[:, 40 + oc, s:s + 1],
                                     xacc, xacc[:, oc, t0:t0 + n], ALU.mult, ALU.add, rd=[mt])
                        self.stt("dve", xacc, xacc[:, oc, t0:t0 + n], py, py[:, 0:n], mt[:, 40 + oc, s:s + 1],
                                 xacc, xacc[:, oc, t0:t0 + n], ALU.mult, ALU.add, rd=[mt])
            for c in range(8):
                S.dma("sp", self.XT, self.XT[c], xacc, xacc[:, c, :], join=True)

    def phase_ln2(self, l):
        S = self.S
        with S.scope():
            g_t = S.sb("lng", [128, 8], F32); b_t = S.sb("lnb", [128, 8], F32)
            S.dma("sp", g_t, g_t[:], self.ln_gT, self.ln_gT[l, 1]); S.dma("sp", b_t, b_t[:], self.ln_bT, self.ln_bT[l, 1])
            onesf = S.sb("onesf", [128, 128], F32)
            S.op("dve", lambda e: e.memset(onesf[:], 1.0 / D), writes=[onesf])
            xb_r = Ring([S.sb("xb", [128, 8, 512], F32) for _ in range(2)])
            sq = S.sb("sq", [128, 8, 512], F32)
            rstd = S.sb("rstd", [128, 512], F32); mean_sb = S.sb("mean_sb", [128, 512], F32)
            for (t0, n) in TBS:
                xb = xb_r.next()
                S.dma("sp", xb, xb[:, :, 0:n], self.XT, self.XT[:, :, t0:t0 + n].rearrange("c p t -> p c t"))
                self.ln_block(xb, n, l, 1, g_t, b_t, sq, onesf, rstd, mean_sb)
                S.dma("sp", self.XT, self.XT[:, :, t0:t0 + n].rearrange("c p t -> p c t"), xb, xb[:, :, 0:n])


def _swap_perm(n_heads, hd):
    q = hd // 4
    idx = []
    for h in range(n_heads):
        b = h * hd
        idx += list(range(b + q, b + 2 * q)) + list(range(b, b + q)) + list(range(b + 3 * q, b + 4 * q)) + list(range(b + 2 * q, b + 3 * q))
    return np.array(idx)


def _rope_tables(hd):
    half = hd // 2; q = half // 2
    inv = 10000.0 ** (-np.arange(0, half, 2, dtype=np.float32) / half)
    t = np.arange(T); row = (t // 64).astype(np.float32); col = (t % 64).astype(np.float32)
    cos = np.ones((hd, NT), np.float32); sin = np.zeros((hd, NT), np.float32)
    for f in range(hd):
        pos = row if f < half else col
        ff = f % half
        ang = pos * inv[ff % q]
        cos[f, :T] = np.cos(ang.astype(np.float32))
        sgn = -1.0 if ff < q else 1.0
        sin[f, :T] = sgn * np.sin(ang.astype(np.float32))
    return cos, sin


def host_consts():
    c = {}
    c["ident"] = np.eye(128, dtype=np.float32)
    rc, rs = _rope_tables(128); c["ret_cos"] = rc; c["ret_sin"] = rs
    gc, gs = _rope_tables(64); c["gq_cos"] = gc; c["gq_sin"] = gs
    p = np.arange(128)[:, None]; f = np.arange(512)[None, :]
    c["delta"] = (f - p).astype(np.float32)
    c["cvals"] = np.broadcast_to((128.0 * (np.arange(21) - 3))[None, :], (128, 21)).astype(np.float32).copy()
    m0 = np.zeros((128, 4, 512), np.float32)
    for i, cc in enumerate((-384, -256, -128, 0)):
        m0[:, i, :] = ((f - p + cc) >= 0)
    c["m0"] = m0
    rm = np.ones((128, NT), np.float32); rm[:, ::32] = 0.0
    c["rmask"] = rm
    j = np.arange(32)[:, None]; i = np.arange(32)[None, :]
    tr = np.zeros((32, 2, 32), np.float32); tr[:, 0, :] = (j <= i); tr[:, 1, :] = (j >= i)
    c["tril"] = tr
    kc = np.arange(64)[:, None]; qc = np.arange(64)[None, :]
    c0 = np.clip(qc - 8, 0, 48)
    v = ((kc >= c0) & (kc < c0 + 16)).astype(np.float32)
    c["validC2"] = np.concatenate([v, v], axis=0)
    sel = np.zeros((32, 32, 128), np.float32)
    for e in range(32):
        sel[e, e, :] = 1.0
    c["sel"] = sel
    return c


def host_params(P):
    o = {}
    f32 = np.float32
    o["b_modT"] = np.ascontiguousarray(P["b_mod"].reshape(4, 48, 128).transpose(0, 2, 1)).astype(f32)
    o["decb"] = np.ascontiguousarray(np.broadcast_to(P["ret_decay"].reshape(4, 1, 8), (4, 128, 8))).astype(f32)
    o["gn_gT"] = np.ascontiguousarray(P["ret_gn_g"].reshape(4, 4, 128).transpose(0, 2, 1)).astype(f32)
    o["gn_bT"] = np.ascontiguousarray(P["ret_gn_b"].reshape(4, 4, 128).transpose(0, 2, 1)).astype(f32)
    o["hg_lbT"] = np.ascontiguousarray(P["hg_lb"].reshape(2, 4, 4, 128).transpose(3, 0, 1, 2)).astype(f32)
    o["hg_ngT"] = np.ascontiguousarray(P["hg_norm_g"].reshape(4, 4, 128).transpose(0, 2, 1)).astype(f32)
    kc = np.arange(64)[:, None]; qc = np.arange(64)[None, :]
    dc = np.clip(kc - qc + 15, 0, 30)
    o["na_T"] = np.ascontiguousarray(P["na_rpb"][:, :, :, dc].transpose(0, 1, 3, 2, 4)).astype(f32)
    sw = _swap_perm(1, 64)
    o["gq_qg"] = np.ascontiguousarray(np.stack([P["gq_qn_g"], P["gq_qn_g"][:, sw]], axis=-1)).astype(f32)
    o["gq_kg"] = np.ascontiguousarray(np.stack([P["gq_kn_g"], P["gq_kn_g"][:, sw]], axis=-1)).astype(f32)
    o["ln_gT"] = np.ascontiguousarray(P["ln_g"].reshape(4, 2, 8, 128).transpose(0, 1, 3, 2)).astype(f32)
    o["ln_bT"] = np.ascontiguousarray(P["ln_b"].reshape(4, 2, 8, 128).transpose(0, 1, 3, 2)).astype(f32)
    o["w_router"] = np.ascontiguousarray(P["w_router"]).astype(f32)
    o["b_router_b"] = np.ascontiguousarray(np.broadcast_to(P["b_router"][:, None, :], (4, 128, 32))).astype(f32)
    o["b_guT"] = np.ascontiguousarray(P["b_gu"].reshape(4, 32, 16, 128).transpose(0, 3, 1, 2)).astype(f32)
    o["b_dn"] = np.ascontiguousarray(P["b_down"]).astype(f32)
    return o


def host_weights(P):
    w_in = P["w_in"]
    ext = np.concatenate([w_in,
                          w_in[:, :, C_RQ + _swap_perm(4, 128)], w_in[:, :, C_RK + _swap_perm(4, 128)],
                          w_in[:, :, C_GQ + _swap_perm(8, 64)], w_in[:, :, C_GK + _swap_perm(2, 64)]], axis=2)
    return dict(w_in=ext.reshape(4096, WIN_COLS), w_mod=P["w_mod"].reshape(4096, 6144),
                w_br=P["w_branch"].reshape(8192, 1024), w_out=P["w_out"].reshape(4096, 1024),
                w_gu=P["w_gu"].reshape(-1, 2048), w_dn=P["w_down"].reshape(-1, 1024))


def host_core_acts(P, b):
    bs = range(b * NB, (b + 1) * NB)
    cT = np.stack([np.stack([P["c"][i].reshape(8, 128).T, P["c_ctx"].reshape(8, 128).T], axis=-1) for i in bs], axis=0)
    return dict(x_in=np.ascontiguousarray(P["x"][b * NB:(b + 1) * NB]), ctx_in=np.ascontiguousarray(P["ctx"][b * NB:(b + 1) * NB]),
                cT=np.ascontiguousarray(cT).astype(np.float32))


class Builder5(Builder4):
    def rsq(self, ot, oap, it, iap, eps, mult=1.0):
        if self.epsc is None:
            self.epsc = {}
        if eps not in self.epsc:
            et = self.S.sb("epsc", [128, 1], F32, persist=True)
            self.S.op("dve", lambda e: e.memset(et[:], eps), writes=[et])
            self.epsc[eps] = et
        et = self.epsc[eps]
        self.act(ot, oap, it, iap, AF.Ln, bias=et[0:oap.shape[0], 0:1], scale=mult, rd=[et])
        self.act(ot, oap, ot, oap, AF.Exp, scale=-0.5)

    def load_hT(self):
        S = self.S
        hT = S.sb("hT", [128, 8, NT], BF16)
        for c in range(8):
            S.dma("sp", hT, hT[:, c, :], self.HT, self.HT[c], join=True)
        return hT

    def proj_fm(self, p, n, wt, wap_fn, hT, t0):
        for k in range(8):
            lap = wap_fn(k)
            self.mm(p, p[0:lap.shape[1], 0:n], wt, lap, hT, hT[:, k, t0:t0 + n], start=(k == 0), stop=(k == 7))

    def phase_gqa(self, l, lw):
        S = self.S
        with S.scope():
            hT = self.load_hT()
            raw_c = S.sb("raw_c", [64, NT], F32); raw_s = S.sb("raw_s", [64, NT], F32)
            S.dma("sp", raw_c, raw_c[:], self.c_gcos, self.c_gcos[:]); S.dma("sp", raw_s, raw_s[:], self.c_gsin, self.c_gsin[:])
            qg = S.sb("qg", [64, 2], F32); kg = S.sb("kg", [64, 2], F32)
            S.dma("sp", qg, qg[:], self.gq_qg, self.gq_qg[l]); S.dma("sp", kg, kg[:], self.gq_kg, self.gq_kg[l])
            tabs = {}
            for nm, g in (("q", qg), ("k", kg)):
                tc_ = S.sb("tc" + nm, [64, NT], F32); ts_ = S.sb("ts" + nm, [64, NT], F32)
                self.ts("dve", tc_, tc_[:], raw_c, raw_c[:], g[:, 0:1], None, ALU.mult, rd=[g])
                self.ts("dve", ts_, ts_[:], raw_s, raw_s[:], g[:, 1:2], None, ALU.mult, rd=[g])
                tabs[nm] = (tc_, ts_)
            wq = S.sb("wq", [128, 8, 512], BF16); wqs = S.sb("wqs", [128, 8, 512], BF16)
            wk = S.sb("wk", [128, 8, 128], BF16); wks = S.sb("wks", [128, 8, 128], BF16); wv = S.sb("wv", [128, 8, 128], BF16)
            self.load_w(wq, wq[:], "w_in", lw * 1024, 8, C_GQ, 512); self.load_w(wqs, wqs[:], "w_in", lw * 1024, 8, C_GQS, 512)
            self.load_w(wk, wk[:], "w_in", lw * 1024, 8, C_GK, 128); self.load_w(wks, wks[:], "w_in", lw * 1024, 8, C_GKS, 128)
            self.load_w(wv, wv[:], "w_in", lw * 1024, 8, C_GV, 128)
            psA = self.ps[0]; psB = self.ps[1]
            pr = Ring(self.ps[2:])
            sq_r = Ring([S.sb("sqn", [64, 512], BF16) for _ in range(2)])
            rs_r = Ring([S.sb("rstd", [64, 512], F32) for _ in range(2)])
            t1_r = Ring([S.sb("t1", [64, 512], F32) for _ in range(2)])
            t2_r = Ring([S.sb("t2", [64, 512], F32) for _ in range(2)])

            def normrope(dst, w_, ws_, c0, tab):
                tc_, ts_ = tab
                for (t0, n) in TBS:
                    p1 = pr.next(); p2 = pr.next(); p3 = pr.next()
                    self.proj_fm(p1, n, w_, lambda k: w_[:, k, c0:c0 + 64], hT, t0)
                    self.proj_fm(p2, n, ws_, lambda k: ws_[:, k, c0:c0 + 64], hT, t0)
                    sq = sq_r.next(); rs = rs_r.next(); t1 = t1_r.next(); t2 = t2_r.next()
                    lvl = 9
                    if lvl >= 1:
                        self.act(sq, sq[:, 0:n], p1, p1[0:64, 0:n], AF.Square)
                    if lvl >= 2:
                        self.mm(p3, p3[0:64, 0:n], self.onesb, self.onesb[0:64, 0:64], sq, sq[:, 0:n])
                    if lvl >= 3:
                        self.rsq(rs, rs[:, 0:n], p3, p3[0:64, 0:n], NORM_EPS, mult=1.0 / 64)
                    if lvl >= 4:
                        S.op("dve", lambda e, t1=t1, p1=p1, n=n, t0=t0: e.tensor_tensor(out=t1[:, 0:n], in0=p1[0:64, 0:n], in1=tc_[:, t0:t0 + n], op=ALU.mult),
                             reads=[p1, tc_, sq], writes=[t1])
                        self.tt("dve", t2, t2[:, 0:n], p2, p2[0:64, 0:n], ts_, ts_[:, t0:t0 + n], ALU.mult)
                    if lvl >= 5:
                        self.tt("pool", t1, t1[:, 0:n], t1, t1[:, 0:n], t2, t2[:, 0:n], ALU.add)
                        self.tt("pool", dst, dst[:, t0:t0 + n], t1, t1[:, 0:n], rs, rs[:, 0:n], ALU.mult)

            stop = 99
            if stop <= 1:
                return
            kT = [S.sb("kT%d" % i, [64, NT], BF16) for i in range(2)]
            for kvh in range(2):
                normrope(kT[kvh], wk, wks, kvh * 64, tabs["k"])
            if stop <= 2:
                return
            V = S.sb("V", [128, 18, 128], BF16)
            for j in range(18):
                p = pr.next()
                for k in range(8):
                    self.mm(p, p[:, 0:128], hT, hT[:, k, j * 128:(j + 1) * 128], wv, wv[:, k, :], start=(k == 0), stop=(k == 7))
                self.act(V, V[:, j, :], p, p[:, 0:128], AF.Identity)
            if stop <= 3:
                return
            qT_r = Ring([S.sb("qT", [64, NT], BF16) for _ in range(2)])
            og_r = Ring([S.sb("ogT", [64, NT], BF16) for _ in range(2)])
            pt_r = Ring([S.sb("PT", [128, 512], BF16) for _ in range(4)])
            rd_r = Ring([S.sb("rd", [64, 512], F32) for _ in range(2)])
            for h in range(8):
                kvh = h // 4
                qT = qT_r.next(); og = og_r.next()
                normrope(qT, wq, wqs, h * 64, tabs["q"])
                if stop <= 4:
                    return
                for (t0, n) in TBS:
                    kts = list(range(18)) if t0 < T else [16, 17]
                    for i, kt in enumerate(kts):
                        pS = pr.next()
                        self.mm(pS, pS[:, 0:n], kT[kvh], kT[kvh][:, kt * 128:(kt + 1) * 128], qT, qT[:, t0:t0 + n])
                        PT = pt_r.next()
                        self.act(PT, PT[:, 0:n], pS, pS[:, 0:n], AF.Exp, scale=0.125)
                        self.mm(psA, psA[0:64, 0:n], V, V[:, kt, kvh * 64:(kvh + 1) * 64], PT, PT[:, 0:n], start=(i == 0), stop=(i == len(kts) - 1))
                        self.mm(psB, psB[0:64, 0:n], self.onesb, self.onesb[:, 0:64], PT, PT[:, 0:n], start=(i == 0), stop=(i == len(kts) - 1))
                    rd = rd_r.next()
                    S.op("dve", lambda e, rd=rd, n=n: e.reciprocal(out=rd[:, 0:n], in_=psB[0:64, 0:n]), reads=[psB], writes=[rd])
                    self.tt("dve", og, og[:, t0:t0 + n], psA, psA[0:64, 0:n], rd, rd[:, 0:n], ALU.mult)
                S.dma("sp", self.OB[3], self.OB[3][h // 2, (h % 2) * 64:(h % 2 + 1) * 64, :], og, og[:], join=True)


class Builder6(Builder5):
    def phase_ret(self, l, lw):
        S = self.S
        with S.scope():
            hT = self.load_hT()
            cosT = S.sb("cosT", [128, NT], F32); sinT = S.sb("sinT", [128, NT], F32)
            S.dma("sp", cosT, cosT[:], self.c_rcos, self.c_rcos[:]); S.dma("sp", sinT, sinT[:], self.c_rsin, self.c_rsin[:])
            delta = S.sb("delta", [128, 512], F32); cv = S.sb("cv", [128, 21], F32); m0 = S.sb("m0", [128, 4, 512], F32)
            S.dma("sp", delta, delta[:], self.c_delta, self.c_delta[:]); S.dma("sp", cv, cv[:], self.c_cvals, self.c_cvals[:])
            S.dma("sp", m0, m0[:], self.c_m0, self.c_m0[:])
            dec = S.sb("dec", [128, 8], F32); lgt = S.sb("lgt", [128, 8], F32); nlg = S.sb("nlg", [128, 8], F32)
            S.dma("sp", dec, dec[:], self.decb, self.decb[l])
            self.act(lgt, lgt[:], dec, dec[:], AF.Sigmoid)
            self.act(lgt, lgt[:], lgt, lgt[:], AF.Ln)
            self.ts("dve", nlg, nlg[:], lgt, lgt[:], -1.0, None, ALU.mult)
            gng = S.sb("gng", [128, 4], F32); gnb = S.sb("gnb", [128, 4], F32)
            S.dma("sp", gng, gng[:], self.gn_gT, self.gn_gT[l]); S.dma("sp", gnb, gnb[:], self.gn_bT, self.gn_bT[l])
            onesf = S.sb("onesf", [128, 128], F32)
            S.op("dve", lambda e: e.memset(onesf[:], 1.0 / 128), writes=[onesf])
            wh_r = Ring([S.sb("wh", [128, 8, 6, 128], BF16) for _ in range(1)])
            qr = S.sb("qr", [128, NT], BF16); kr = S.sb("kr", [128, NT], BF16); sg = S.sb("sgate", [128, NT], BF16)
            V = S.sb("V", [128, 18, 128], BF16); og = S.sb("og", [128, NT], BF16)
            E0 = S.sb("E0", [128, 512], F32); E1 = S.sb("E1", [128, 512], F32)
            g0 = S.sb("g0", [128, 21], F32); g1 = S.sb("g1", [128, 21], F32)
            Dd = S.sb("Dd", [128, 4, 512], F32); Dc = S.sb("Dc", [128, 8, 512], F32)
            t1_r = Ring([S.sb("t1", [128, 512], F32) for _ in range(2)]); t2_r = Ring([S.sb("t2", [128, 512], F32) for _ in range(2)])
            pt_r = Ring([S.sb("PT", [128, 512], BF16) for _ in range(4)])
            osb = S.sb("osb", [128, 512], F32); osq = S.sb("osq", [128, 512], F32); mean_sb = S.sb("mean", [128, 512], F32)
            rstd = S.sb("rstd", [128, 512], F32)
            psA = self.ps[0]; pr = Ring(self.ps[1:])
            cols = (C_RQ, C_RQS, C_RK, C_RKS, C_RV, C_RG)
            ci = lambda c: c // 128 + 3
            for h in range(4):
                wh = wh_r.next()
                for i, c0 in enumerate(cols):
                    self.load_w(wh, wh[:, :, i, :], "w_in", lw * 1024, 8, c0 + h * 128, 128, join=True)
                for (t0, n) in TBS:
                    for (dst, a, b) in ((qr, 0, 1), (kr, 2, 3)):
                        p1 = pr.next(); p2 = pr.next()
                        self.proj_fm(p1, n, wh, lambda k, a=a: wh[:, k, a, :], hT, t0)
                        self.proj_fm(p2, n, wh, lambda k, b=b: wh[:, k, b, :], hT, t0)
                        t1 = t1_r.next(); t2 = t2_r.next()
                        self.tt("dve", t1, t1[:, 0:n], p1, p1[:, 0:n], cosT, cosT[:, t0:t0 + n], ALU.mult)
                        self.tt("dve", t2, t2[:, 0:n], p2, p2[:, 0:n], sinT, sinT[:, t0:t0 + n], ALU.mult)
                        self.tt("pool", dst, dst[:, t0:t0 + n], t1, t1[:, 0:n], t2, t2[:, 0:n], ALU.add)
                    p3 = pr.next()
                    self.proj_fm(p3, n, wh, lambda k: wh[:, k, 5, :], hT, t0)
                    self.act(sg, sg[:, t0:t0 + n], p3, p3[:, 0:n], AF.Silu)
                for j in range(18):
                    p = pr.next()
                    for k in range(8):
                        self.mm(p, p[:, 0:128], hT, hT[:, k, j * 128:(j + 1) * 128], wh, wh[:, k, 4, :], start=(k == 0), stop=(k == 7))
                    self.act(V, V[:, j, :], p, p[:, 0:128], AF.Identity)
                self.act(E0, E0[:], delta, delta[:], AF.Exp, scale=lgt[:, h:h + 1], rd=[lgt])
                self.act(E1, E1[:], delta, delta[:], AF.Exp, scale=nlg[:, 4 + h:5 + h], rd=[nlg])
                self.act(g0, g0[:], cv, cv[:], AF.Exp, scale=lgt[:, h:h + 1], rd=[lgt])
                self.act(g1, g1[:], cv, cv[:], AF.Exp, scale=lgt[:, 4 + h:5 + h], rd=[lgt])
                self.ts("dve", g0, g0[:], g0, g0[:], 128.0 ** -0.5, None, ALU.mult)
                self.ts("dve", g1, g1[:], g1, g1[:], 128.0 ** -0.5, None, ALU.mult)
                for i, c in enumerate((-384, -256, -128, 0)):
                    t1 = t1_r.next(); t2 = t2_r.next()
                    self.stt("dve", t1, t1[:], m0, m0[:, i, :], g0[:, ci(c):ci(c) + 1], E0, E0[:], ALU.mult, ALU.mult, rd=[g0])
                    self.stt("dve", t2, t2[:], m0, m0[:, i, :], g1[:, ci(-c):ci(-c) + 1], E1, E1[:], ALU.mult, ALU.mult, rd=[g1])
                    self.tt("pool", t1, t1[:], t1, t1[:], t2, t2[:], ALU.subtract)
                    self.stt("dve", Dd, Dd[:, i, :], E1, E1[:], g1[:, ci(-c):ci(-c) + 1], t1, t1[:], ALU.mult, ALU.add, rd=[g1])
                for qb in range(4):
                    for kk in range(2):
                        c0 = qb * 512 - kk * 128 + 256; c1 = 2048 - qb * 512 + kk * 128
                        t1 = t1_r.next()
                        self.ts("dve", t1, t1[:], E0, E0[:], g0[:, ci(c0):ci(c0) + 1], None, ALU.mult, rd=[g0])
                        self.stt("dve", Dc, Dc[:, qb * 2 + kk, :], E1, E1[:], g1[:, ci(c1):ci(c1) + 1], t1, t1[:], ALU.mult, ALU.add, rd=[g1])
                for qb, (t0, n) in enumerate(TBS):
                    kts = list(range(18)) if t0 < T else [16, 17]
                    for i, kt in enumerate(kts):
                        pS = pr.next()
                        self.mm(pS, pS[:, 0:n], kr, kr[:, kt * 128:(kt + 1) * 128], qr, qr[:, t0:t0 + n])
                        PT = pt_r.next()
                        if t0 >= T:
                            c = -(kt - 16) * 128
                            self.tt("dve", PT, PT[:, 0:n], pS, pS[:, 0:n], Dd, Dd[:, (c + 384) // 128, 0:n], ALU.mult)
                        elif kt >= 16:
                            self.tt("dve", PT, PT[:, 0:n], pS, pS[:, 0:n], Dc, Dc[:, qb * 2 + kt - 16, 0:n], ALU.mult)
                        else:
                            c = qb * 512 - kt * 128
                            if c >= 128:
                                self.stt("dve", PT, PT[:, 0:n], pS, pS[:, 0:n], g0[:, ci(c):ci(c) + 1], E0, E0[:, 0:n], ALU.mult, ALU.mult, rd=[g0])
                            elif c <= -512:
                                self.stt("dve", PT, PT[:, 0:n], pS, pS[:, 0:n], g1[:, ci(-c):ci(-c) + 1], E1, E1[:, 0:n], ALU.mult, ALU.mult, rd=[g1])
                            else:
                                self.tt("dve", PT, PT[:, 0:n], pS, pS[:, 0:n], Dd, Dd[:, (c + 384) // 128, 0:n], ALU.mult)
                        self.mm(psA, psA[:, 0:n], V, V[:, kt, :], PT, PT[:, 0:n], start=(i == 0), stop=(i == len(kts) - 1))
                    self.act(osb, osb[:, 0:n], psA, psA[:, 0:n], AF.Identity)
                    self.act(osq, osq[:, 0:n], psA, psA[:, 0:n], AF.Square)
                    pm = pr.next(); pe2 = pr.next()
                    self.mm(pm, pm[:, 0:n], onesf, onesf[:], osb, osb[:, 0:n])
                    self.mm(pe2, pe2[:, 0:n], onesf, onesf[:], osq, osq[:, 0:n])
                    self.act(mean_sb, mean_sb[:, 0:n], pm, pm[:, 0:n], AF.Identity)
                    self.tt("dve", rstd, rstd[:, 0:n], mean_sb, mean_sb[:, 0:n], mean_sb, mean_sb[:, 0:n], ALU.mult)
                    self.tt("dve", rstd, rstd[:, 0:n], pe2, pe2[:, 0:n], rstd, rstd[:, 0:n], ALU.subtract)
                    self.rsq(rstd, rstd[:, 0:n], rstd, rstd[:, 0:n], LN_EPS)
                    self.tt("pool", osb, osb[:, 0:n], osb, osb[:, 0:n], mean_sb, mean_sb[:, 0:n], ALU.subtract)
                    self.tt("pool", osb, osb[:, 0:n], osb, osb[:, 0:n], rstd, rstd[:, 0:n], ALU.mult)
                    self.act(osb, osb[:, 0:n], osb, osb[:, 0:n], AF.Identity, bias=gnb[:, h:h + 1], scale=gng[:, h:h + 1], rd=[gng, gnb])
                    self.tt("pool", og, og[:, t0:t0 + n], osb, osb[:, 0:n], sg, sg[:, t0:t0 + n], ALU.mult)
                S.dma("sp", self.OB[0], self.OB[0][h], og, og[:], join=True)


class Builder7(Builder6):
    def mmx(self, pt, pap, lt, lap, rt, rap, start=True, stop=True, inc=True):
        self.S.op("pe", lambda e: e.matmul(pap, lhsT=lap, rhs=rap, start=start, stop=stop),
                  reads=[lt, rt], writes=[pt], inc=inc)

    def phase_na(self, l, lw):
        S = self.S
        with S.scope():
            hT = self.load_hT()
            vc2 = S.sb("vc2", [128, 64], F32)
            S.dma("sp", vc2, vc2[:], self.c_validC2, self.c_validC2[:])
            w_r = Ring([S.sb("wna", [128, 8, 3, 64], BF16) for _ in range(2)])
            qT_r = Ring([S.sb("qT", [64, NT], BF16) for _ in range(2)]); kT_r = Ring([S.sb("kT", [64, NT], BF16) for _ in range(2)])
            Ve_r = Ring([S.sb("Ve", [128, 18, 64], BF16) for _ in range(2)]); Vo_r = Ring([S.sb("Vo", [128, 16, 64], BF16) for _ in range(2)])
            tbr_r = Ring([S.sb("tbraw", [128, 14, 64], F32) for _ in range(2)]); tb_r = Ring([S.sb("tb2", [128, 14, 64], F32) for _ in range(2)])
            og_r = Ring([S.sb("og", [64, NT], BF16) for _ in range(2)])
            P_r = Ring([S.sb("P", [128, 6, 64], BF16) for _ in range(3)])
            rd_r = Ring([S.sb("rd", [64, 64], F32) for _ in range(3)])
            psO = Ring(self.ps[0:2]); psD = Ring(self.ps[2:4]); pr = Ring(self.ps[4:8])
            for h in range(8):
                w = w_r.next(); qT = qT_r.next(); kT = kT_r.next(); Ve = Ve_r.next(); Vo = Vo_r.next()
                tbraw = tbr_r.next(); tb2 = tb_r.next(); og = og_r.next()
                for i, c0 in enumerate((C_NQ, C_NK, C_NV)):
                    self.load_w(w, w[:, :, i, :], "w_in", lw * 1024, 8, c0 + h * 64, 64, join=True)
                src = self.na_T[l, h]
                S.dma("sp", tbraw, tbraw[0:64, :, :], self.na_T, src[:, 0:14, :], join=True)
                S.dma("sp", tbraw, tbraw[64:128, :, :], self.na_T, src[:, 1:15, :], join=True)
                self.act(tbraw, tbraw[:], tbraw, tbraw[:], AF.Exp)
                self.tt("dve", tb2, tb2[:], tbraw, tbraw[:], vc2, _bcast_mid(vc2[:], 14), ALU.mult)
                for (t0, n) in TBS:
                    p1 = pr.next(); p2 = pr.next()
                    self.proj_fm(p1, n, w, lambda k: w[:, k, 0, :], hT, t0)
                    self.proj_fm(p2, n, w, lambda k: w[:, k, 1, :], hT, t0)
                    self.act(qT, qT[:, t0:t0 + n], p1, p1[0:64, 0:n], AF.Identity)
                    S.op("dve", lambda e, kT=kT, p2=p2, t0=t0, n=n: e.tensor_copy(out=kT[:, t0:t0 + n], in_=p2[0:64, 0:n]), reads=[p2], writes=[kT])
                for (Vt, off, cnt) in ((Ve, 0, 18), (Vo, 64, 15)):
                    for j0 in range(0, cnt, 8):
                        jn = min(8, cnt - j0)
                        p = pr.next()
                        for jj in range(jn):
                            tok = off + (j0 + jj) * 128
                            for k in range(8):
                                self.mmx(p, p[:, jj * 64:(jj + 1) * 64], hT, hT[:, k, tok:tok + 128], w, w[:, k, 2, :],
                                         start=(k == 0), stop=(k == 7), inc=(k == 7 and jj == jn - 1))
                        self.act(Vt, Vt[:, j0:j0 + jn, :], p, p[:, 0:jn * 64].rearrange("p (j d) -> p j d", d=64), AF.Identity)
                for qr in range(36):
                    lat = qr < 32
                    q0 = qr * 64
                    if lat:
                        R0 = min(max(qr - 4, 0), 24); dr0 = R0 - qr + 7
                        ktoks = [R0 * 64 + 128 * j for j in range(4)] + [2048, 2176]
                        if R0 % 2 == 0:
                            vts = [(Ve, R0 // 2 + j) for j in range(4)]
                        else:
                            vts = [(Vo, (R0 - 1) // 2 + j) for j in range(4)]
                        vts += [(Ve, 16), (Ve, 17)]
                    else:
                        ktoks = [2048, 2176]; vts = [(Ve, 16), (Ve, 17)]
                    nk = len(ktoks)
                    pS = pr.next(); P = P_r.next()
                    for j, tok in enumerate(ktoks):
                        self.mmx(pS, pS[:, j * 64:(j + 1) * 64], kT, kT[:, tok:tok + 128], qT, qT[:, q0:q0 + 64], inc=(j == nk - 1))
                    self.act(P, P[:, 0:nk, :], pS, pS[:, 0:nk * 64].rearrange("p (j q) -> p j q", q=64), AF.Exp, scale=0.125)
                    if lat:
                        self.tt("dve", P, P[:, 0:4, :], P, P[:, 0:4, :], tb2, tb2[:, dr0:dr0 + 7:2, :], ALU.mult)
                    pO = psO.next(); pD = psD.next()
                    for j, (Vt, vi) in enumerate(vts):
                        self.mmx(pO, pO[0:64, 0:64], Vt, Vt[:, vi, :], P, P[:, j, :], start=(j == 0), stop=(j == nk - 1), inc=False)
                        self.mmx(pD, pD[0:64, 0:64], self.onesb, self.onesb[:, 0:64], P, P[:, j, :], start=(j == 0), stop=(j == nk - 1),
                                 inc=(j == nk - 1))
                    rd = rd_r.next()
                    S.op("dve", lambda e, rd=rd, pD=pD: e.reciprocal(out=rd[:], in_=pD[0:64, 0:64]), reads=[pD], writes=[rd])
                    self.tt("dve", og, og[:, q0:q0 + 64], pO, pO[0:64, 0:64], rd, rd[:], ALU.mult)
                S.dma("sp", self.OB[2], self.OB[2][h // 2, (h % 2) * 64:(h % 2 + 1) * 64, :], og, og[:], join=True)


class Builder8(Builder7):
    def phase_hg(self, l, lw):
        S = self.S
        NCH = NT // 32
        with S.scope():
            hT = self.load_hT()
            rmask = S.sb("rmask", [128, NT], F32); tril = S.sb("tril", [32, 2, 32], F32)
            S.dma("sp", rmask, rmask[:], self.c_rmask, self.c_rmask[:]); S.dma("sp", tril, tril[:], self.c_tril, self.c_tril[:])
            lbt = S.sb("lbt", [128, 2, 4, 4], F32); ssum = S.sb("ssum", [128, 2, 4], F32)
            low = S.sb("low", [128, 2, 4], F32); oml = S.sb("oml", [128, 2, 4], F32); noml = S.sb("noml", [128, 2, 4], F32)
            S.dma("sp", lbt, lbt[:], self.hg_lbT, self.hg_lbT[:])
            self.act(lbt, lbt[:], lbt, lbt[:], AF.Exp)
            self.tt("dve", ssum, ssum[:], lbt, lbt[:, :, 0, :], lbt, lbt[:, :, 1, :], ALU.add)
            self.tt("dve", ssum, ssum[:], ssum, ssum[:], lbt, lbt[:, :, 2, :], ALU.add)
            self.tt("dve", ssum, ssum[:], ssum, ssum[:], lbt, lbt[:, :, 3, :], ALU.add)
            S.op("dve", lambda e: e.reciprocal(out=ssum[:], in_=ssum[:]), reads=[ssum], writes=[ssum])
            S.op("dve", lambda e: e.memset(low[:], 0.0), writes=[low])
            for i in range(1, l + 1):
                self.tt("dve", low, low[:], low, low[:], lbt, lbt[:, :, i, :], ALU.add)
            self.tt("dve", low, low[:], low, low[:], ssum, ssum[:], ALU.mult)
            self.ts("dve", oml, oml[:], low, low[:], -1.0, 1.0, ALU.mult, ALU.add)
            self.ts("dve", noml, noml[:], oml, oml[:], -1.0, None, ALU.mult)
            ngt = S.sb("ngt", [128, 4], F32)
            S.dma("sp", ngt, ngt[:], self.hg_ngT, self.hg_ngT[l])
            onesf = S.sb("onesf", [128, 128], F32)
            S.op("dve", lambda e: e.memset(onesf[:], 1.0 / 128), writes=[onesf])
            wh = S.sb("wh", [128, 8, 5, 128], BF16)
            qf = S.sb("qf", [128, NT], F32); sg = S.sb("sgate", [128, NT], BF16); osum = S.sb("osum", [128, NT], F32)
            vtok = S.sb("vtok", [32, NCH, 128], BF16)
            qd = [S.sb("qd%d" % d, [128, NT], BF16) for d in range(2)]; kd = [S.sb("kd%d" % d, [128, NT], BF16) for d in range(2)]
            kl = [S.sb("kl%d" % d, [128, NT], F32) for d in range(2)]; dcy = [S.sb("dcy%d" % d, [128, NCH], F32) for d in range(2)]
            kdz = [S.sb("kdz%d" % d, [128, NT], BF16) for d in range(2)]; emid = [S.sb("emid%d" % d, [128, NCH], F32) for d in range(2)]
            mid = S.sb("mid", [128, NCH], F32)
            for d in range(2):
                S.op("pool", lambda e, d=d: e.memset(kdz[d][:], 0.0), writes=[kdz[d]])
            tot = S.sb("tot", [128, NCH], F32)
            Tm = [S.sb("T%d" % i, [128, NT], F32) for i in range(4)]
            St = [S.sb("S%d" % d, [128, 128], F32) for d in range(2)]; Sb = [S.sb("Sb%d" % d, [128, 128], BF16) for d in range(2)]
            Am_r = Ring([S.sb("Am", [32, 32], BF16) for _ in range(4)]); klt_r = Ring([S.sb("klt", [32, 128], BF16) for _ in range(4)])
            og = S.sb("og", [128, NT], BF16)
            pr = self.psr
            v3 = lambda t: t[:].rearrange("p (c i) -> p c i", i=32)
            cols = (C_HQ, C_HF, C_HB, C_HI, C_HG)
            for h in range(4):
                for i, c0 in enumerate(cols):
                    self.load_w(wh, wh[:, :, i, :], "w_in", lw * 1024, 8, c0 + h * 128, 128, join=True)
                T1, T2, T3, T4 = Tm
                for (t0, n) in TBS:
                    p1 = pr.next(); p2 = pr.next(); p3 = pr.next()
                    self.proj_fm(p1, n, wh, lambda k: wh[:, k, 0, :], hT, t0)
                    self.proj_fm(p2, n, wh, lambda k: wh[:, k, 4, :], hT, t0)
                    self.proj_fm(p3, n, wh, lambda k: wh[:, k, 3, :], hT, t0)
                    self.act(qf, qf[:, t0:t0 + n], p1, p1[:, 0:n], AF.Silu)
                    self.act(sg, sg[:, t0:t0 + n], p2, p2[:, 0:n], AF.Silu)
                    S.op("dve", lambda e, p3=p3, t0=t0, n=n: e.tensor_copy(out=T4[:, t0:t0 + n], in_=p3[:, 0:n]), reads=[p3], writes=[T4])
                for c0 in range(0, NCH, 4):
                    p = pr.next()
                    for cc in range(4):
                        c = c0 + cc
                        S.op("pe", lambda e, p=p, cc=cc, c=c: e.transpose(out=p[0:32, cc * 128:(cc + 1) * 128], in_=T4[:, c * 32:(c + 1) * 32],
                                                                          identity=self.ident[:]), reads=[T4, self.ident], writes=[p], inc=(cc == 3))
                    self.act(vtok, vtok[:, c0:c0 + 4, :], p, p[0:32, :].rearrange("p (c d) -> p c d", d=128), AF.Identity)
                for d in range(2):
                    lb_ = low[:, d, h:h + 1]; om_ = oml[:, d, h:h + 1]; nom_ = noml[:, d, h:h + 1]
                    for (t0, n) in TBS:
                        p1 = pr.next()
                        self.proj_fm(p1, n, wh, lambda k, d=d: wh[:, k, 1 + d, :], hT, t0)
                        self.act(T1, T1[:, t0:t0 + n], p1, p1[:, 0:n], AF.Sigmoid)
                    self.ts("dve", T3, T3[:], T1, T1[:], om_, lb_, ALU.mult, ALU.add, rd=[oml, low])
                    self.ts("dve", T2, T2[:], T1, T1[:], nom_, om_, ALU.mult, ALU.add, rd=[oml, noml])
                    self.act(T3, T3[:], T3, T3[:], AF.Ln)
                    S.op("dve", lambda e: e.tensor_tensor_scan(out=T1[:], data0=rmask[:], data1=T3[:], initial=0.0, op0=ALU.mult, op1=ALU.add),
                         reads=[rmask, T3], writes=[T1])
                    S.op("dve", lambda e: e.tensor_copy(out=tot[:], in_=v3(T1)[:, :, 31]), reads=[T1], writes=[tot])
                    self.act(dcy[d], dcy[d][:], tot, tot[:], AF.Exp)
                    self.tt("dve", T4, v3(T4), tot, _bcast_last(tot[:], 32), T1, v3(T1), ALU.subtract)
                    if d == 0:
                        lc, ek = T1, T4
                    else:
                        self.tt("dve", T4, T4[:], T4, T4[:], T3, T3[:], ALU.add)
                        self.tt("pool", T1, T1[:], T1, T1[:], T3, T3[:], ALU.subtract)
                        lc, ek = T4, T1
                    self.act(ek, ek[:], ek, ek[:], AF.Exp)
                    self.tt("dve", kl[d], kl[d][:], T2, T2[:], ek, ek[:], ALU.mult)
                    mi = 15 if d == 0 else 16
                    S.op("dve", lambda e, lc=lc, mi=mi: e.tensor_copy(out=mid[:], in_=v3(lc)[:, :, mi]), reads=[lc], writes=[mid])
                    self.act(emid[d], emid[d][:], mid, mid[:], AF.Exp)
                    self.tt("dve", lc, v3(lc), lc, v3(lc), mid, _bcast_last(mid[:], 32), ALU.subtract)
                    self.act(ek, ek[:], lc, lc[:], AF.Exp)
                    self.tt("dve", qd[d], qd[d][:], qf, qf[:], ek, ek[:], ALU.mult)
                    self.act(ek, ek[:], lc, lc[:], AF.Exp, scale=-1.0)
                    self.tt("dve", kd[d], kd[d][:], T2, T2[:], ek, ek[:], ALU.mult)
                    keep = slice(0, 16) if d == 0 else slice(16, 32)
                    self.tt("pool", kdz[d], v3(kdz[d])[:, :, keep], T2, v3(T2)[:, :, keep], ek, v3(ek)[:, :, keep], ALU.mult)
                S.op("dve", lambda e: e.memset(osum[:], 0.0), writes=[osum])
                for d in range(2):
                    S.op("dve", lambda e, d=d: e.memset(St[d][:], 0.0), writes=[St[d]])
                    S.op("dve", lambda e, d=d: e.memset(Sb[d][:], 0.0), writes=[Sb[d]])
                order = [list(range(64, 72)) + list(range(0, 64)), list(range(71, 63, -1)) + list(range(63, -1, -1))]
                for s in range(NCH):
                    for d in range(2):
                        c = order[d][s]
                        sl = slice(c * 32, (c + 1) * 32)
                        pA = pr.next(); pT = pr.next(); pO = pr.next(); pS = pr.next()
                        h0 = slice(c * 32, c * 32 + 16); h1 = slice(c * 32 + 16, (c + 1) * 32)
                        ka, kb = (kdz[0], kd[0]) if d == 0 else (kd[1], kdz[1])
                        self.mmx(pA, pA[0:32, 0:16], ka, ka[:, sl], qd[d], qd[d][:, h0], inc=False)
                        self.mmx(pA, pA[0:32, 16:32], kb, kb[:, sl], qd[d], qd[d][:, h1], inc=True)
                        Am = Am_r.next()
                        self.tt("dve", Am, Am[:], pA, pA[0:32, 0:32], tril, tril[:, d, :], ALU.mult)
                        S.op("pe", lambda e, pT=pT, d=d, sl=sl: e.transpose(out=pT[0:32, 0:128], in_=kl[d][:, sl], identity=self.ident[:]),
                             reads=[kl[d], self.ident], writes=[pT])
                        klt = klt_r.next()
                        self.act(klt, klt[:], pT, pT[0:32, 0:128], AF.Identity)
                        self.mm(pO, pO[:, 0:32], vtok, vtok[:, c, :], Am, Am[:], start=True, stop=False)
                        self.mm(pO, pO[:, 0:32], Sb[d], Sb[d][:], qd[d], qd[d][:, sl], start=False, stop=True)
                        self.tt("dve", osum, osum[:, sl], osum, osum[:, sl], pO, pO[:, 0:32], ALU.add)
                        self.mm(pS, pS[:, 0:128], klt, klt[:], vtok, vtok[:, c, :])
                        self.stt("dve", St[d], St[d][:], St[d], St[d][:], dcy[d][:, c:c + 1], pS, pS[:, 0:128], ALU.mult, ALU.add, rd=[dcy[d]])
                        cn = order[d][s + 1] if s + 1 < NCH else c
                        self.act(Sb[d], Sb[d][:], St[d], St[d][:], AF.Identity, scale=emid[d][:, cn:cn + 1], rd=[emid[d]])
                for (t0, n) in TBS:
                    self.act(T1, T1[:, t0:t0 + n], osum, osum[:, t0:t0 + n], AF.Square)
                    pss = pr.next()
                    self.mm(pss, pss[:, 0:n], onesf, onesf[:], T1, T1[:, t0:t0 + n])
                    self.rsq(T2, T2[:, t0:t0 + n], pss, pss[:, 0:n], NORM_EPS)
                    self.tt("dve", T3, T3[:, t0:t0 + n], osum, osum[:, t0:t0 + n], T2, T2[:, t0:t0 + n], ALU.mult)
                    self.act(T3, T3[:, t0:t0 + n], T3, T3[:, t0:t0 + n], AF.Identity, scale=ngt[:, h:h + 1], rd=[ngt])
                    self.tt("pool", og, og[:, t0:t0 + n], T3, T3[:, t0:t0 + n], sg, sg[:, t0:t0 + n], ALU.mult)
                S.dma("sp", self.OB[1], self.OB[1][h], og, og[:], join=True)


class BuilderN(Builder8):
    def build_all(self):
        for bi in range(NB):
            self.bi = bi
            self.build_one()
        self.finish()

    def build_one(self):
        self.phase_init()
        for l in self.layers:
            self.phase_mod(l, l)
            self.phase_hT(0, self.XT, self.HT)
            self.phase_ret(l, l)
            self.phase_hg(l, l)
            self.phase_na(l, l)
            self.phase_gqa(l, l)
            self.phase_merge(l, l)
            self.phase_moe(l, l)
            self.phase_ln2(l)
        self.phase_final()


_W_KEYS = ("w_in", "w_mod", "w_br", "w_out", "w_gu", "w_dn")


def kernel(**inputs):
    P = {k: np.asarray(v) for k, v in inputs.items()}
    consts = host_consts(); params = host_params(P); W = host_weights(P)
    nc = bass.Bass("TRN2", target_bir_lowering=False)
    B = BuilderN(nc, full=True)
    B.build_all()
    in_maps = []
    for b in range(NCORES):
        m = {}
        m.update(consts); m.update(params); m.update(host_core_acts(P, b))
        for k in _W_KEYS:
            m[k + "_s"] = W[k]
        in_maps.append({k: np.ascontiguousarray(v, dtype=np.float32) for k, v in m.items()})
    res = run_bass_kernel_spmd(nc, in_maps, core_ids=list(range(NCORES)))
    return np.concatenate([np.asarray(res.results[b]["out"], np.float32) for b in range(NCORES)], axis=0)
```

```python
import numpy as np
import concourse.bass as bass
import concourse.mybir as mybir
from concourse.bass_utils import run_bass_kernel_spmd

F32 = mybir.dt.float32
BF16 = mybir.dt.bfloat16
AF = mybir.ActivationFunctionType
ALU = mybir.AluOpType
AX = mybir.AxisListType

D = 1024; T = 2048; LC = 256; NT = 2304; DEPTH = 4
NCORES = 8
NB = 8 // NCORES
TBS = [(0, 512), (512, 512), (1024, 512), (1536, 512), (2048, 256)]
WIN_COLS = 12672
C_RQ, C_RK, C_RV, C_RG = 0, 512, 1024, 1536
C_HQ, C_HF, C_HB, C_HI, C_HG = 2048, 2560, 3072, 3584, 4096
C_NQ, C_NK, C_NV = 4608, 5120, 5632
C_GQ, C_GK, C_GV = 6144, 6656, 6784
C_GATE = 6912
C_RQS, C_RKS, C_GQS, C_GKS = 11008, 11520, 12032, 12544
DN_ALPHA = (2 * DEPTH) ** 0.25
LN_EPS = 1e-5
NORM_EPS = 1e-6


class Buf:
    def __init__(self, ap, name):
        self.ap = ap; self.name = name
        self.writes = {}; self.reads = {}; self.dsem = None; self.dcnt = 0
        self.is_psum = name.startswith("ps")

    def __getitem__(self, idx):
        return self.ap[idx]


class Ring:
    def __init__(self, tiles):
        self.tiles = tiles; self.i = 0

    def next(self):
        t = self.tiles[self.i % len(self.tiles)]; self.i += 1
        return t


class Sched:
    def __init__(self, nc):
        self.nc = nc
        self.E = {}
        for n, e in (("pe", nc.tensor), ("act", nc.scalar), ("dve", nc.vector),
                     ("pool", nc.gpsimd), ("sp", nc.sync)):
            self.E[n] = dict(e=e, sem=nc.alloc_semaphore("s_" + n), cnt=0, seen={})
        self.bsem = nc.alloc_semaphore("s_bar"); self.bcnt = 0
        self.dsems = []; self.free_dsems = []
        self.nwait = 0; self.nins = 0; self.uid = 0

    def sb(self, name, shape, dt):
        self.uid += 1
        return Buf(self.nc.alloc_sbuf_tensor("%s_%d" % (name, self.uid), list(shape), dt).ap(), name)

    def dram(self, name, shape, dt, kind="Internal"):
        return Buf(self.nc.dram_tensor(name, list(shape), dt, kind=kind).ap(), name)

    def _wait(self, en, events):
        E = self.E[en]
        for sem, val in events.items():
            if E["seen"].get(sem, 0) >= val:
                continue
            E["e"].wait_ge(sem, val)
            E["seen"][sem] = val
            self.nwait += 1

    @staticmethod
    def _merge(dst, src):
        for s, v in src.items():
            if v > dst.get(s, 0):
                dst[s] = v

    def op(self, en, fn, reads=(), writes=(), inc=True):
        E = self.E[en]
        ev = {}
        for t in reads:
            self._merge(ev, t.writes)
            if t.is_psum and en != "pe":
                self._merge(ev, t.reads)
        for t in writes:
            self._merge(ev, t.writes); self._merge(ev, t.reads)
        if ev.get(E["sem"], 0) > E["cnt"]:
            del ev[E["sem"]]
        self._wait(en, ev)
        ins = fn(E["e"])
        self.nins += 1
        nxt = E["cnt"] + 1
        if inc:
            ins.then_inc(E["sem"], 1); E["cnt"] = nxt
        me = {E["sem"]: nxt}
        for t in reads:
            self._merge(t.reads, me)
        for t in writes:
            t.writes = dict(me); t.reads = {}
        return ins

    def dma(self, en, out_t, out_ap, in_t, in_ap, join=False, **kw):
        E = self.E[en]
        if out_t.dsem is None:
            out_t.dsem = self.nc.alloc_semaphore("d%d_%s" % (len(self.dsems), out_t.name))
            self.dsems.append(out_t)
        ev = {}
        self._merge(ev, in_t.writes)
        w = dict(out_t.writes)
        if join:
            w.pop(out_t.dsem, None)
        self._merge(ev, w); self._merge(ev, out_t.reads)
        self._wait(en, ev)
        ins = E["e"].dma_start(out=out_ap, in_=in_ap, **kw)
        self.nins += 1
        ins.then_inc(out_t.dsem, 16)
        out_t.dcnt += 16
        me = {out_t.dsem: out_t.dcnt}
        self._merge(in_t.reads, me)
        out_t.writes = dict(me); out_t.reads = {}
        return ins

    def barrier(self):
        ev = {}
        for n, E in self.E.items():
            if n != "sp" and E["cnt"] > 0:
                ev[E["sem"]] = E["cnt"]
        for t in self.dsems:
            ev[t.dsem] = t.dcnt
        self._wait("sp", ev)
        self.bcnt += 1
        self.nc.sync.sem_inc(self.bsem, 1)
        for n in self.E:
            if n != "sp":
                self._wait(n, {self.bsem: self.bcnt})


from contextlib import ExitStack, contextmanager


class SchedX(Sched):
    def __init__(self, nc):
        super().__init__(nc)
        self.es = None
        self.scope_tiles = None
        self.dsem_cnt = {}
        self.free_sems = []

    @contextmanager
    def scope(self):
        assert self.es is None
        with ExitStack() as es:
            self.es = es; self.scope_tiles = []
            yield
            self.barrier()
            for t in self.scope_tiles:
                if t.dsem is not None:
                    self.free_sems.append(t.dsem); t.dsem = None
            self.es = None; self.scope_tiles = None

    def sb(self, name, shape, dt, persist=False):
        self.uid += 1
        nm = "%s_%d" % (name, self.uid)
        if self.es is None or persist:
            h = self.nc.alloc_sbuf_tensor(nm, list(shape), dt)
            return Buf(h.ap(), name)
        h = self.es.enter_context(self.nc.sbuf_tensor(nm, list(shape), dt))
        t = Buf(h.ap(), name)
        self.scope_tiles.append(t)
        return t

    def _get_dsem(self, t):
        if t.dsem is None:
            if self.free_sems:
                t.dsem = self.free_sems.pop()
            else:
                t.dsem = self.nc.alloc_semaphore("d%d" % len(self.dsem_cnt))
                self.dsem_cnt[t.dsem] = 0
        return t.dsem

    def dma(self, en, out_t, out_ap, in_t, in_ap, join=False, cc=None, **kw):
        E = self.E[en]
        sem = self._get_dsem(out_t)
        ev = {}
        self._merge(ev, in_t.writes)
        w = dict(out_t.writes)
        if join:
            w.pop(sem, None)
        self._merge(ev, w); self._merge(ev, out_t.reads)
        self._wait(en, ev)
        if cc is None:
            ins = E["e"].dma_start(out=out_ap, in_=in_ap, **kw)
        else:
            ins = E["e"].collective_compute(cc, ALU.bypass, replica_groups=[list(range(NCORES))],
                                            ins=[in_ap], outs=[out_ap])
        self.nins += 1
        ins.then_inc(sem, 16)
        self.dsem_cnt[sem] += 16
        me = {sem: self.dsem_cnt[sem]}
        self._merge(in_t.reads, me)
        out_t.writes = dict(me); out_t.reads = {}
        return ins

    def barrier(self):
        ev = {}
        for n, E in self.E.items():
            if n != "sp" and E["cnt"] > 0:
                ev[E["sem"]] = E["cnt"]
        for sem, c in self.dsem_cnt.items():
            if c > 0:
                ev[sem] = c
        self._wait("sp", ev)
        self.bcnt += 1
        self.nc.sync.sem_inc(self.bsem, 1)
        for n in self.E:
            if n != "sp":
                self._wait(n, {self.bsem: self.bcnt})


def _bcast_mid(ap, n):
    return ap.unsqueeze(1).to_broadcast([ap.shape[0], n, ap.shape[1]])


def _bcast_last(ap, n):
    return ap.unsqueeze(2).to_broadcast([ap.shape[0], ap.shape[1], n])


class Builder:
    def __init__(self, nc, layers=range(DEPTH), gather=False, n_experts=32, dbg=(), full=False):
        self.nc = nc
        self.S = S = SchedX(nc)
        self.layers = list(layers); self.n_experts = n_experts
        self.dbg = set(dbg)
        ein = lambda n, s, dt=F32: S.dram(n, s, dt, kind="ExternalInput")
        self.x_in = ein("x_in", [NB, T, D]); self.ctx_in = ein("ctx_in", [NB, LC, D]); self.cT_in = ein("cT", [NB, 128, 8, 2])
        R = NCORES if gather else 1
        nlw = DEPTH if full else 1
        ne = 32 if full else n_experts
        self.wshapes = dict(w_in=(nlw * 1024, WIN_COLS), w_mod=(nlw * 1024, 6144), w_br=(nlw * 2048, 1024),
                            w_out=(nlw * 1024, 1024), w_gu=(nlw * ne * 1024, 2048), w_dn=(nlw * ne * 1024, 1024))
        self.ne_w = ne
        self.W = {}
        for k, (r, c) in self.wshapes.items():
            if gather:
                sh = ein(k + "_s", [r // NCORES, c])
                full = S.dram(k + "_f", [r, c], F32)
                S.dma("pool", full, full[:], sh, sh[:], cc="AllGather")
                self.W[k] = full
            else:
                self.W[k] = ein(k + "_s", [r, c])
        self.b_modT = ein("b_modT", [4, 128, 48]); self.decb = ein("decb", [4, 128, 8])
        self.gn_gT = ein("gn_gT", [4, 128, 4]); self.gn_bT = ein("gn_bT", [4, 128, 4])
        self.hg_lbT = ein("hg_lbT", [128, 2, 4, 4]); self.hg_ngT = ein("hg_ngT", [4, 128, 4])
        self.na_T = ein("na_T", [4, 8, 64, 15, 64])
        self.gq_qg = ein("gq_qg", [4, 64, 2]); self.gq_kg = ein("gq_kg", [4, 64, 2])
        self.ln_gT = ein("ln_gT", [4, 2, 128, 8]); self.ln_bT = ein("ln_bT", [4, 2, 128, 8])
        self.w_router = ein("w_router", [4, 1024, 32]); self.b_router_b = ein("b_router_b", [4, 128, 32])
        self.b_guT = ein("b_guT", [4, 128, 32, 16]); self.b_dn = ein("b_dn", [4, 32, 1024])
        self.c_ident = ein("ident", [128, 128])
        self.c_rcos = ein("ret_cos", [128, NT]); self.c_rsin = ein("ret_sin", [128, NT])
        self.c_gcos = ein("gq_cos", [64, NT]); self.c_gsin = ein("gq_sin", [64, NT])
        self.c_delta = ein("delta", [128, 512]); self.c_cvals = ein("cvals", [128, 21]); self.c_m0 = ein("m0", [128, 4, 512])
        self.c_rmask = ein("rmask", [128, NT]); self.c_tril = ein("tril", [32, 2, 32])
        self.c_validC2 = ein("validC2", [128, 64]); self.c_sel = ein("sel", [32, 32, 128])
        self.out = S.dram("out", [NB, T, D], F32, kind="ExternalOutput")
        self.bi = 0
        self.XT = S.dram("XT", [8, 128, NT], F32); self.HT = S.dram("HT", [8, 128, NT], BF16)
        self.H2T = S.dram("H2T", [8, 128, NT], BF16)
        self.OB = [S.dram("OB0", [4, 128, NT], BF16), S.dram("OB1", [4, 128, NT], BF16),
                   S.dram("OB2", [8, 64, NT], BF16), S.dram("OB3", [8, 64, NT], BF16)]
        self.dbg_out = {}
        self.ident = S.sb("ident", [128, 128], F32)
        S.dma("sp", self.ident, self.ident[:], self.c_ident, self.c_ident[:])
        self.onesb = S.sb("onesb", [128, 128], BF16)
        S.op("dve", lambda e: e.memset(self.onesb[:], 1.0), writes=[self.onesb])
        self.modT = S.sb("modT", [128, 48, 2], F32)
        self.ps = [Buf(nc.alloc_psum_tensor("ps%d" % i, [128, 512], F32).ap(), "ps%d" % i) for i in range(8)]
        self.psr = Ring(self.ps)
        self.epsc = {}
        for eps in (LN_EPS, NORM_EPS):
            et = S.sb("epsc", [128, 1], F32)
            S.op("dve", lambda e, et=et, eps=eps: e.memset(et[:], eps), writes=[et])
            self.epsc[eps] = et

    def dbg_dump(self, name, src_t, src_ap, shape, dt=F32):
        if name not in self.dbg:
            return
        o = self.S.dram("dbg_" + name, list(shape), dt, kind="ExternalOutput")
        self.S.dma("sp", o, o[:], src_t, src_ap)
        self.dbg_out[name] = o

    def mm(self, pt, pap, lt, lap, rt, rap, start=True, stop=True):
        self.S.op("pe", lambda e: e.matmul(pap, lhsT=lap, rhs=rap, start=start, stop=stop),
                  reads=[lt, rt], writes=[pt], inc=stop)

    def load_w(self, dst_t, dst_ap, key, row0, nk, col0, ncols, en="pool", join=True):
        w = self.W[key]
        src = w[row0:row0 + nk * 128, col0:col0 + ncols].rearrange("(k p) n -> p k n", p=128)
        self.S.dma(en, dst_t, dst_ap, w, src, join=join)

    def act(self, ot, oap, it, iap, func, bias=None, scale=1.0, rd=()):
        kw = {}
        if bias is not None:
            kw["bias"] = bias
        self.S.op("act", lambda e: e.activation(out=oap, in_=iap, func=func, scale=scale, **kw),
                  reads=[it] + list(rd), writes=[ot])

    def tt(self, en, ot, oap, at, aap, bt, bap, op):
        self.S.op(en, lambda e: e.tensor_tensor(out=oap, in0=aap, in1=bap, op=op), reads=[at, bt], writes=[ot])

    def ts(self, en, ot, oap, it, iap, s1, s2, op0, op1=None, rd=()):
        if op1 is None:
            self.S.op(en, lambda e: e.tensor_scalar(out=oap, in0=iap, scalar1=s1, scalar2=None, op0=op0),
                      reads=[it] + list(rd), writes=[ot])
        else:
            self.S.op(en, lambda e: e.tensor_scalar(out=oap, in0=iap, scalar1=s1, scalar2=s2, op0=op0, op1=op1),
                      reads=[it] + list(rd), writes=[ot])

    def stt(self, en, ot, oap, at, aap, scalar, bt, bap, op0, op1, rd=()):
        self.S.op(en, lambda e: e.scalar_tensor_tensor(out=oap, in0=aap, scalar=scalar, in1=bap, op0=op0, op1=op1),
                  reads=[at, bt] + list(rd), writes=[ot])

    def rsqrt_eps(self, t, ap, eps, mult=1.0):
        if self.epsc is None:
            self.epsc = {}
        if eps not in self.epsc:
            et = self.S.sb("epsc", [128, 1], F32, persist=True)
            self.S.op("dve", lambda e: e.memset(et[:], eps), writes=[et])
            self.epsc[eps] = et
        et = self.epsc[eps]
        self.act(t, ap, t, ap, AF.Ln, bias=et[0:ap.shape[0], 0:1], scale=mult, rd=[et])
        self.act(t, ap, t, ap, AF.Exp, scale=-0.5)

    def phase_init(self):
        S = self.S
        with S.scope():
            xin = Ring([S.sb("xin", [128, D], F32) for _ in range(3)])
            xtb = Ring([S.sb("xtb", [128, 8, 512], F32) for _ in range(2)])
            for (t0, n) in TBS:
                ob = xtb.next()
                tiles = []
                for j in range(n // 128):
                    xt = xin.next()
                    if t0 < T:
                        S.dma("sp", xt, xt[:], self.x_in, self.x_in[self.bi, t0 + j * 128:t0 + (j + 1) * 128, :])
                    else:
                        S.dma("sp", xt, xt[:], self.ctx_in, self.ctx_in[self.bi, t0 - T + j * 128:t0 - T + (j + 1) * 128, :])
                    tiles.append(xt)
                    if len(tiles) == 2 or j == n // 128 - 1:
                        j0 = j + 1 - len(tiles)
                        for c in range(8):
                            p = self.psr.next()
                            for jj, xt_ in enumerate(tiles):
                                S.op("pe", lambda e, p=p, jj=jj, xt_=xt_, c=c: e.transpose(
                                    out=p[:, jj * 128:(jj + 1) * 128], in_=xt_[:, c * 128:(c + 1) * 128],
                                    identity=self.ident[:]), reads=[xt_, self.ident], writes=[p])
                            w = len(tiles) * 128
                            S.op("act" if c % 2 else "dve",
                                 (lambda e, p=p, c=c, j0=j0, w=w, ob=ob: e.copy(out=ob[:, c, j0 * 128:j0 * 128 + w], in_=p[:, 0:w]))
                                 if c % 2 else
                                 (lambda e, p=p, c=c, j0=j0, w=w, ob=ob: e.tensor_copy(out=ob[:, c, j0 * 128:j0 * 128 + w], in_=p[:, 0:w])),
                                 reads=[p], writes=[ob])
                        tiles = []
                S.dma("sp", self.XT, self.XT[:, :, t0:t0 + n].rearrange("c p t -> p c t"), ob, ob[:, :, 0:n])

    def phase_final(self):
        S = self.S
        with S.scope():
            xtb = Ring([S.sb("xtb", [128, 8, 512], F32) for _ in range(2)])
            ot = Ring([S.sb("otile", [128, D], F32) for _ in range(3)])
            for (t0, n) in TBS[:4]:
                xb = xtb.next()
                S.dma("sp", xb, xb[:], self.XT, self.XT[:, :, t0:t0 + n].rearrange("c p t -> p c t"))
                for j in range(4):
                    o = ot.next()
                    for g in range(2):
                        p = self.psr.next()
                        for cc in range(4):
                            c = g * 4 + cc
                            S.op("pe", lambda e, p=p, cc=cc, c=c, j=j, xb=xb: e.transpose(
                                out=p[:, cc * 128:(cc + 1) * 128], in_=xb[:, c, j * 128:(j + 1) * 128],
                                identity=self.ident[:]), reads=[xb, self.ident], writes=[p])
                        if g:
                            S.op("act", lambda e, p=p, o=o, g=g: e.copy(out=o[:, g * 512:(g + 1) * 512], in_=p[:]), reads=[p], writes=[o])
                        else:
                            S.op("dve", lambda e, p=p, o=o, g=g: e.tensor_copy(out=o[:, g * 512:(g + 1) * 512], in_=p[:]), reads=[p], writes=[o])
                    S.dma("sp", self.out, self.out[self.bi, t0 + j * 128:t0 + (j + 1) * 128, :], o, o[:], join=True)

    def finish(self):
        S = self.S
        ev = {}
        S._merge(ev, self.out.writes)
        for o in self.dbg_out.values():
            S._merge(ev, o.writes)
        S._wait("sp", ev)
        S.barrier()


class Builder2(Builder):
    def phase_mod(self, l, lw):
        S = self.S
        with S.scope():
            cT = S.sb("cT", [128, 8, 2], F32); scT = S.sb("scT", [128, 8, 2], BF16)
            bm = S.sb("bm", [128, 48], F32)
            S.dma("sp", cT, cT[:], self.cT_in, self.cT_in[self.bi])
            S.dma("sp", bm, bm[:], self.b_modT, self.b_modT[l])
            self.act(scT, scT[:], cT, cT[:], AF.Silu)
            wr = Ring([S.sb("wmod", [128, 8, 1024], BF16) for _ in range(2)])
            pm = self.psr.next()
            for piece in range(6):
                wt = wr.next()
                self.load_w(wt, wt[:], "w_mod", lw * 1024, 8, piece * 1024, 1024, join=False)
                for j in range(8):
                    jj = piece * 8 + j
                    for k in range(8):
                        self.mm(pm, pm[:, jj * 2:jj * 2 + 2], wt, wt[:, k, j * 128:(j + 1) * 128], scT, scT[:, k, :],
                                start=(k == 0), stop=(k == 7))
            mt = self.modT
            self.tt("dve", mt, mt[:], pm, pm[:, 0:96].rearrange("p (j s) -> p j s", s=2), bm, _bcast_last(bm[:], 2), ALU.add)
            for w in (1, 4):
                self.ts("dve", mt, mt[:, w * 8:(w + 1) * 8, :], mt, mt[:, w * 8:(w + 1) * 8, :], 1.0, None, ALU.add)
            self.dbg_dump("modT", mt, mt[:], [128, 48, 2])

    def phase_hT(self, which, src, dst):
        S = self.S
        mt = self.modT
        with S.scope():
            xb_r = Ring([S.sb("xb", [128, 8, 512], F32) for _ in range(2)])
            hb_r = Ring([S.sb("hb", [128, 8, 512], BF16) for _ in range(2)])
            for (t0, n) in TBS:
                s = 0 if t0 < T else 1
                xb = xb_r.next(); hb = hb_r.next()
                S.dma("sp", xb, xb[:, :, 0:n], src, src[:, :, t0:t0 + n].rearrange("c p t -> p c t"))
                for c in range(8):
                    self.act(hb, hb[:, c, 0:n], xb, xb[:, c, 0:n], AF.Identity,
                             bias=mt[:, which * 24 + c, s:s + 1], scale=mt[:, which * 24 + 8 + c, s:s + 1], rd=[mt])
                S.dma("sp", dst, dst[:, :, t0:t0 + n].rearrange("c p t -> p c t"), hb, hb[:, :, 0:n])

    def ln_block(self, xn, n, l, i, g_t, b_t, sq, onesf, rstd, mean_sb):
        S = self.S
        pmean = self.psr.next(); pex2 = self.psr.next()
        for c in range(8):
            self.act(sq, sq[:, c, 0:n], xn, xn[:, c, 0:n], AF.Square)
        for c in range(8):
            self.mm(pmean, pmean[:, 0:n], onesf, onesf[:], xn, xn[:, c, 0:n], start=(c == 0), stop=(c == 7))
        for c in range(8):
            self.mm(pex2, pex2[:, 0:n], onesf, onesf[:], sq, sq[:, c, 0:n], start=(c == 0), stop=(c == 7))
        self.act(mean_sb, mean_sb[:, 0:n], pmean, pmean[:, 0:n], AF.Identity)
        self.tt("dve", rstd, rstd[:, 0:n], mean_sb, mean_sb[:, 0:n], mean_sb, mean_sb[:, 0:n], ALU.mult)
        self.tt("dve", rstd, rstd[:, 0:n], pex2, pex2[:, 0:n], rstd, rstd[:, 0:n], ALU.subtract)
        self.rsqrt_eps(rstd, rstd[:, 0:n], LN_EPS)
        for c in range(8):
            self.tt("dve", xn, xn[:, c, 0:n], xn, xn[:, c, 0:n], mean_sb, mean_sb[:, 0:n], ALU.subtract)
            self.tt("pool", xn, xn[:, c, 0:n], xn, xn[:, c, 0:n], rstd, rstd[:, 0:n], ALU.mult)
            self.act(xn, xn[:, c, 0:n], xn, xn[:, c, 0:n], AF.Identity, bias=b_t[:, c:c + 1], scale=g_t[:, c:c + 1], rd=[g_t, b_t])

    def phase_merge(self, l, lw):
        S = self.S
        mt = self.modT
        with S.scope():
            wg = S.sb("wg", [128, 8, 4096], BF16)
            wb01 = S.sb("wb01", [128, 2, 4, 1024], BF16); wb23 = S.sb("wb23", [64, 2, 8, 1024], BF16)
            wo = S.sb("wo", [128, 8, 1024], BF16)
            for n4 in range(4):
                self.load_w(wg, wg[:, :, n4 * 1024:(n4 + 1) * 1024], "w_in", lw * 1024, 8, C_GATE + n4 * 1024, 1024)
            wbr = self.W["w_br"]
            for n in range(2):
                S.dma("pool", wb01, wb01[:, n], wbr, wbr[lw * 2048 + n * 512: lw * 2048 + (n + 1) * 512, :].rearrange("(k p) n -> p k n", p=128), join=True)
            for n in range(2):
                S.dma("pool", wb23, wb23[:, n], wbr, wbr[lw * 2048 + (n + 2) * 512: lw * 2048 + (n + 3) * 512, :].rearrange("(k p) n -> p k n", p=64), join=True)
            self.load_w(wo, wo[:], "w_out", lw * 1024, 8, 0, 1024)
            g_t = S.sb("lng", [128, 8], F32); b_t = S.sb("lnb", [128, 8], F32)
            S.dma("sp", g_t, g_t[:], self.ln_gT, self.ln_gT[l, 0]); S.dma("sp", b_t, b_t[:], self.ln_bT, self.ln_bT[l, 0])
            onesf = S.sb("onesf", [128, 128], F32)
            S.op("dve", lambda e: e.memset(onesf[:], 1.0 / D), writes=[onesf])
            hb_r = Ring([S.sb("hb", [128, 8, 512], BF16) for _ in range(2)])
            o01_r = Ring([S.sb("o01", [128, 2, 4, 512], BF16) for _ in range(2)])
            o23_r = Ring([S.sb("o23", [64, 2, 8, 512], BF16) for _ in range(2)])
            xb_r = Ring([S.sb("xb", [128, 8, 512], F32) for _ in range(2)])
            mb = S.sb("mb", [128, 8, 512], BF16)
            sg_r = Ring([S.sb("sg", [128, 512], F32) for _ in range(3)])
            macc_r = Ring([S.sb("macc", [128, 512], F32) for _ in range(2)])
            tmp_r = Ring([S.sb("tmpm", [128, 512], F32) for _ in range(2)])
            sq = S.sb("sq", [128, 8, 512], F32)
            rstd = S.sb("rstd", [128, 512], F32); mean_sb = S.sb("mean_sb", [128, 512], F32)
            h2b_r = Ring([S.sb("h2b", [128, 8, 512], BF16) for _ in range(2)])
            for (t0, n) in TBS:
                s = 0 if t0 < T else 1
                hb = hb_r.next(); o01 = o01_r.next(); o23 = o23_r.next(); xb = xb_r.next()
                S.dma("sp", hb, hb[:, :, 0:n], self.HT, self.HT[:, :, t0:t0 + n].rearrange("c p t -> p c t"))
                for nb in range(2):
                    S.dma("sp", o01, o01[:, nb, :, 0:n], self.OB[nb], self.OB[nb][:, :, t0:t0 + n].rearrange("c p t -> p c t"), join=True)
                    S.dma("sp", o23, o23[:, nb, :, 0:n], self.OB[2 + nb], self.OB[2 + nb][:, :, t0:t0 + n].rearrange("c p t -> p c t"), join=True)
                S.dma("sp", xb, xb[:, :, 0:n], self.XT, self.XT[:, :, t0:t0 + n].rearrange("c p t -> p c t"))
                for oc in range(8):
                    macc = macc_r.next()
                    for nb in range(4):
                        pg = self.psr.next(); py = self.psr.next()
                        for k in range(8):
                            self.mm(pg, pg[:, 0:n], wg, wg[:, k, nb * 1024 + oc * 128: nb * 1024 + (oc + 1) * 128], hb, hb[:, k, 0:n],
                                    start=(k == 0), stop=(k == 7))
                        sg = sg_r.next()
                        self.act(sg, sg[:, 0:n], pg, pg[:, 0:n], AF.Sigmoid)
                        if nb < 2:
                            for c in range(4):
                                self.mm(py, py[:, 0:n], wb01, wb01[:, nb, c, oc * 128:(oc + 1) * 128], o01, o01[:, nb, c, 0:n],
                                        start=(c == 0), stop=(c == 3))
                        else:
                            for c in range(8):
                                self.mm(py, py[:, 0:n], wb23, wb23[:, nb - 2, c, oc * 128:(oc + 1) * 128], o23, o23[:, nb - 2, c, 0:n],
                                        start=(c == 0), stop=(c == 7))
                        if nb == 0:
                            self.tt("dve", macc, macc[:, 0:n], sg, sg[:, 0:n], py, py[:, 0:n], ALU.mult)
                        else:
                            tmp = tmp_r.next()
                            self.tt("dve", tmp, tmp[:, 0:n], sg, sg[:, 0:n], py, py[:, 0:n], ALU.mult)
                            if nb < 3:
                                self.tt("pool", macc, macc[:, 0:n], macc, macc[:, 0:n], tmp, tmp[:, 0:n], ALU.add)
                            else:
                                self.tt("pool", mb, mb[:, oc, 0:n], macc, macc[:, 0:n], tmp, tmp[:, 0:n], ALU.add)
                for oc2 in range(8):
                    py = self.psr.next()
                    for k in range(8):
                        self.mm(py, py[:, 0:n], wo, wo[:, k, oc2 * 128:(oc2 + 1) * 128], mb, mb[:, k, 0:n], start=(k == 0), stop=(k == 7))
                    self.act(xb, xb[:, oc2, 0:n], xb, xb[:, oc2, 0:n], AF.Identity, scale=DN_ALPHA)
                    self.stt("dve", xb, xb[:, oc2, 0:n], py, py[:, 0:n], mt[:, 16 + oc2, s:s + 1], xb, xb[:, oc2, 0:n], ALU.mult, ALU.add, rd=[mt])
                self.ln_block(xb, n, l, 0, g_t, b_t, sq, onesf, rstd, mean_sb)
                S.dma("sp", self.XT, self.XT[:, :, t0:t0 + n].rearrange("c p t -> p c t"), xb, xb[:, :, 0:n])
                h2b = h2b_r.next()
                for c in range(8):
                    self.act(h2b, h2b[:, c, 0:n], xb, xb[:, c, 0:n], AF.Identity,
                             bias=mt[:, 24 + c, s:s + 1], scale=mt[:, 32 + c, s:s + 1], rd=[mt])
                S.dma("sp", self.H2T, self.H2T[:, :, t0:t0 + n].rearrange("c p t -> p c t"), h2b, h2b[:, :, 0:n])


class Builder3(Builder2):
    def __init__(self, *a, **k):
        super().__init__(*a, **k)
        self.OB[2] = self.S.dram("OB2b", [4, 128, NT], BF16); self.OB[3] = self.S.dram("OB3b", [4, 128, NT], BF16)

    def phase_merge(self, l, lw):
        S = self.S
        mt = self.modT
        with S.scope():
            wgr = Ring([S.sb("wg", [128, 8, 1024], BF16) for _ in range(2)])
            wb = S.sb("wb", [128, 4, 4, 1024], BF16)
            wo = S.sb("wo", [128, 8, 1024], BF16)
            wbr = self.W["w_br"]
            for n in range(4):
                S.dma("pool", wb, wb[:, n], wbr, wbr[lw * 2048 + n * 512: lw * 2048 + (n + 1) * 512, :].rearrange("(k p) n -> p k n", p=128), join=True)
            self.load_w(wo, wo[:], "w_out", lw * 1024, 8, 0, 1024)
            g_t = S.sb("lng", [128, 8], F32); b_t = S.sb("lnb", [128, 8], F32)
            S.dma("sp", g_t, g_t[:], self.ln_gT, self.ln_gT[l, 0]); S.dma("sp", b_t, b_t[:], self.ln_bT, self.ln_bT[l, 0])
            onesf = S.sb("onesf", [128, 128], F32)
            S.op("dve", lambda e: e.memset(onesf[:], 1.0 / D), writes=[onesf])
            hb = S.sb("hb", [128, 8, 512], BF16)
            oall = S.sb("oall", [128, 4, 4, 512], BF16)
            xb = S.sb("xb", [128, 8, 512], F32)
            mb = S.sb("mb", [128, 8, 512], BF16)
            macc8 = S.sb("macc8", [128, 8, 512], F32)
            sg_r = Ring([S.sb("sg", [128, 512], F32) for _ in range(3)])
            tmp_r = Ring([S.sb("tmpm", [128, 512], F32) for _ in range(2)])
            rstd = S.sb("rstd", [128, 512], F32); mean_sb = S.sb("mean_sb", [128, 512], F32)
            h2b = S.sb("h2b", [128, 8, 512], BF16)
            for (t0, n) in TBS:
                s = 0 if t0 < T else 1
                S.dma("sp", hb, hb[:, :, 0:n], self.HT, self.HT[:, :, t0:t0 + n].rearrange("c p t -> p c t"))
                for nb in range(4):
                    S.dma("sp", oall, oall[:, nb, :, 0:n], self.OB[nb], self.OB[nb][:, :, t0:t0 + n].rearrange("c p t -> p c t"), join=True)
                S.dma("sp", xb, xb[:, :, 0:n], self.XT, self.XT[:, :, t0:t0 + n].rearrange("c p t -> p c t"))
                for nb in range(4):
                    wg = wgr.next()
                    self.load_w(wg, wg[:], "w_in", lw * 1024, 8, C_GATE + nb * 1024, 1024, join=False)
                    for oc in range(8):
                        pg = self.psr.next(); py = self.psr.next()
                        for k in range(8):
                            self.mm(pg, pg[:, 0:n], wg, wg[:, k, oc * 128:(oc + 1) * 128], hb, hb[:, k, 0:n], start=(k == 0), stop=(k == 7))
                        sg = sg_r.next()
                        self.act(sg, sg[:, 0:n], pg, pg[:, 0:n], AF.Sigmoid)
                        for c in range(4):
                            self.mm(py, py[:, 0:n], wb, wb[:, nb, c, oc * 128:(oc + 1) * 128], oall, oall[:, nb, c, 0:n], start=(c == 0), stop=(c == 3))
                        if nb == 0:
                            self.tt("dve", macc8, macc8[:, oc, 0:n], sg, sg[:, 0:n], py, py[:, 0:n], ALU.mult)
                        else:
                            tmp = tmp_r.next()
                            self.tt("dve", tmp, tmp[:, 0:n], sg, sg[:, 0:n], py, py[:, 0:n], ALU.mult)
                            if nb < 3:
                                self.tt("pool", macc8, macc8[:, oc, 0:n], macc8, macc8[:, oc, 0:n], tmp, tmp[:, 0:n], ALU.add)
                            else:
                                self.tt("pool", mb, mb[:, oc, 0:n], macc8, macc8[:, oc, 0:n], tmp, tmp[:, 0:n], ALU.add)
                for oc2 in range(8):
                    py = self.psr.next()
                    for k in range(8):
                        self.mm(py, py[:, 0:n], wo, wo[:, k, oc2 * 128:(oc2 + 1) * 128], mb, mb[:, k, 0:n], start=(k == 0), stop=(k == 7))
                    self.act(xb, xb[:, oc2, 0:n], xb, xb[:, oc2, 0:n], AF.Identity, scale=DN_ALPHA)
                    self.stt("dve", xb, xb[:, oc2, 0:n], py, py[:, 0:n], mt[:, 16 + oc2, s:s + 1], xb, xb[:, oc2, 0:n], ALU.mult, ALU.add, rd=[mt])
                self.ln_block(xb, n, l, 0, g_t, b_t, macc8, onesf, rstd, mean_sb)
                S.dma("sp", self.XT, self.XT[:, :, t0:t0 + n].rearrange("c p t -> p c t"), xb, xb[:, :, 0:n])
                for c in range(8):
                    self.act(h2b, h2b[:, c, 0:n], xb, xb[:, c, 0:n], AF.Identity,
                             bias=mt[:, 24 + c, s:s + 1], scale=mt[:, 32 + c, s:s + 1], rd=[mt])
                S.dma("sp", self.H2T, self.H2T[:, :, t0:t0 + n].rearrange("c p t -> p c t"), h2b, h2b[:, :, 0:n])


class Builder4(Builder3):
    def __init__(self, *a, **k):
        super().__init__(*a, **k)
        self.GW = self.S.dram("GW", [32, NT], F32)

    def phase_moe(self, l, lw):
        S = self.S
        mt = self.modT
        ne = self.n_experts
        with S.scope():
            xacc = S.sb("xacc", [128, 8, NT], F32)
            for c in range(8):
                S.dma("sp", xacc, xacc[:, c, :], self.XT, self.XT[c], join=True)
            wr = S.sb("wr", [128, 8, 32], F32); brb = S.sb("brb", [128, 32], F32)
            S.dma("sp", wr, wr[:], self.w_router, self.w_router[l].rearrange("(k p) e -> p k e", p=128))
            S.dma("sp", brb, brb[:], self.b_router_b, self.b_router_b[l])
            bgu = S.sb("bgu", [128, 32, 16], F32); bdn = S.sb("bdn", [32, 1024], F32)
            S.dma("sp", bgu, bgu[:], self.b_guT, self.b_guT[l]); S.dma("sp", bdn, bdn[:], self.b_dn, self.b_dn[l])
            h2f_r = Ring([S.sb("h2f", [128, 8, 128], F32) for _ in range(2)])
            sm_r = Ring([S.sb("rsm", [128, 4, 32], F32) for _ in range(2)])
            sc_r = Ring([S.sb("rsc", [128, 16], F32) for _ in range(2)])
            gwt_r = Ring([S.sb("gwt", [32, 128], F32) for _ in range(2)])
            for j in range(NT // 128):
                s = 0 if j < 16 else 1
                h2f = h2f_r.next(); sm = sm_r.next(); sc = sc_r.next()
                for c in range(8):
                    self.act(h2f, h2f[:, c, :], xacc, xacc[:, c, j * 128:(j + 1) * 128], AF.Identity,
                             bias=mt[:, 24 + c, s:s + 1], scale=mt[:, 32 + c, s:s + 1], rd=[mt])
                pl = self.psr.next()
                for c in range(8):
                    self.mm(pl, pl[:, 0:32], h2f, h2f[:, c, :], wr, wr[:, c, :], start=(c == 0), stop=(c == 7))
                lg = sm[:, 0, :]; ex = sm[:, 1, :]; mk = sm[:, 2, :]; gw = sm[:, 3, :]
                self.tt("dve", sm, lg, pl, pl[:, 0:32], brb, brb[:], ALU.add)
                S.op("dve", lambda e, sc=sc, lg=lg: e.max(out=sc[:, 0:8], in_=lg), reads=[sm], writes=[sc])
                self.ts("dve", sc, sc[:, 8:9], sc, sc[:, 0:1], -1.0, None, ALU.mult)
                self.act(sm, ex, sm, lg, AF.Exp, bias=sc[:, 8:9], rd=[sc])
                self.ts("dve", sm, mk, sm, lg, sc[:, 3:4], None, ALU.is_ge, rd=[sc])
                self.tt("dve", sm, ex, sm, ex, sm, mk, ALU.mult)
                S.op("dve", lambda e, sc=sc, ex=ex: e.reduce_sum(out=sc[:, 9:10], in_=ex, axis=AX.X), reads=[sm], writes=[sc])
                S.op("dve", lambda e, sc=sc: e.reciprocal(out=sc[:, 10:11], in_=sc[:, 9:10]), reads=[sc], writes=[sc])
                self.ts("dve", sm, gw, sm, ex, sc[:, 10:11], None, ALU.mult, rd=[sc])
                pT = self.psr.next()
                S.op("pe", lambda e, pT=pT, gw=gw: e.transpose(out=pT[0:32, 0:128], in_=gw, identity=self.ident[:]),
                     reads=[sm, self.ident], writes=[pT])
                gwt = gwt_r.next()
                self.act(gwt, gwt[:], pT, pT[0:32, 0:128], AF.Identity)
                S.dma("sp", self.GW, self.GW[:, j * 128:(j + 1) * 128], gwt, gwt[:], join=True)
            self.dbg_dump("GW", self.GW, self.GW[:], [32, NT])
            for c in range(8):
                self.act(xacc, xacc[:, c, :], xacc, xacc[:, c, :], AF.Identity, scale=DN_ALPHA)
            wu_r = Ring([S.sb("wu", [128, 8, 512], BF16) for _ in range(6)])
            h2b_r = Ring([S.sb("h2b", [128, 8, 512], BF16) for _ in range(2)])
            act_r = Ring([S.sb("actT", [128, 8, 512], BF16) for _ in range(2)])
            gwb_r = Ring([S.sb("gwb", [128, NT], F32) for _ in range(2)])
            gwblk = S.sb("gwblk", [32, 512], F32)
            tg_r = Ring([S.sb("tg", [128, 512], F32) for _ in range(2)])
            tsg_r = Ring([S.sb("tsg", [128, 512], F32) for _ in range(2)])
            tu_r = Ring([S.sb("tu", [128, 512], F32) for _ in range(2)])
            for e_ in range(ne):
                gwb = gwb_r.next()
                S.dma("sp", gwb, gwb[:], self.GW, self.GW[e_:e_ + 1, :].partition_broadcast(128))
                row0 = (lw * self.ne_w + e_) * 1024
                units = []
                for q in range(4):
                    u = wu_r.next(); self.load_w(u, u[:], "w_gu", row0, 8, q * 512, 512, join=False); units.append(u)
                dunits = []
                for q in range(2):
                    u = wu_r.next(); self.load_w(u, u[:], "w_dn", row0, 8, q * 512, 512, join=False); dunits.append(u)
                for (t0, n) in TBS:
                    s = 0 if t0 < T else 1
                    h2b = h2b_r.next()
                    S.dma("sp", h2b, h2b[:, :, 0:n], self.H2T, self.H2T[:, :, t0:t0 + n].rearrange("c p t -> p c t"))
                    if e_ == 0:
                        S.dma("sp", gwblk, gwblk[:, 0:n], self.GW, self.GW[:, t0:t0 + n])
                    aT = act_r.next()
                    for fc in range(8):
                        ug = units[fc // 4]; uu = units[2 + fc // 4]; co = (fc % 4) * 128
                        pg = self.psr.next(); pu = self.psr.next()
                        for k in range(8):
                            self.mm(pg, pg[:, 0:n], ug, ug[:, k, co:co + 128], h2b, h2b[:, k, 0:n], start=(k == 0), stop=(k == 7))
                        for k in range(8):
                            self.mm(pu, pu[:, 0:n], uu, uu[:, k, co:co + 128], h2b, h2b[:, k, 0:n], start=(k == 0), stop=(k == 7))
                        tg = tg_r.next(); tsg = tsg_r.next(); tu = tu_r.next()
                        self.ts("dve", tg, tg[:, 0:n], pg, pg[:, 0:n], bgu[:, e_, fc:fc + 1], 7.0, ALU.add, ALU.min, rd=[bgu])
                        self.act(tsg, tsg[:, 0:n], tg, tg[:, 0:n], AF.Sigmoid, scale=1.702)
                        self.ts("dve", tu, tu[:, 0:n], pu, pu[:, 0:n], bgu[:, e_, 8 + fc:9 + fc], 7.0, ALU.add, ALU.min, rd=[bgu])
                        self.ts("pool", tu, tu[:, 0:n], tu, tu[:, 0:n], -7.0, 1.0, ALU.max, ALU.add)
                        self.tt("pool", tg, tg[:, 0:n], tg, tg[:, 0:n], tsg, tsg[:, 0:n], ALU.mult)
                        self.tt("pool", tg, tg[:, 0:n], tg, tg[:, 0:n], tu, tu[:, 0:n], ALU.mult)
                        self.tt("dve", aT, aT[:, fc, 0:n], tg, tg[:, 0:n], gwb, gwb[:, t0:t0 + n], ALU.mult)
                    for oc in range(8):
                        ud = dunits[oc // 4]; co = (oc % 4) * 128
                        py = self.psr.next()
                        for fc in range(8):
                            self.mm(py, py[:, 0:n], ud, ud[:, fc, co:co + 128], aT, aT[:, fc, 0:n], start=(fc == 0), stop=(fc == 7))
                        if e_ == 0:
                            pyb = self.psr.next()
                            self.mm(pyb, pyb[:, 0:n], bdn, bdn[:, oc * 128:(oc + 1) * 128], gwblk, gwblk[:, 0:n], start=True, stop=True)
                            self.stt("dve", xacc, xacc[:, oc, t0:t0 + n], pyb, pyb[:, 0:n], mt[:, 40 + oc, s:s + 1],
                                     xacc, xacc[:, oc, t0:t0 + n], ALU.mult, ALU.add, rd=[mt])
                        self.stt("dve", xacc, xacc[:, oc, t0:t0 + n], py, py[:, 0:n], mt[:, 40 + oc, s:s + 1],
                                 xacc, xacc[:, oc, t0:t0 + n], ALU.mult, ALU.add, rd=[mt])
            for c in range(8):
                S.dma("sp", self.XT, self.XT[c], xacc, xacc[:, c, :], join=True)

    def phase_ln2(self, l):
        S = self.S
        with S.scope():
            g_t = S.sb("lng", [128, 8], F32); b_t = S.sb("lnb", [128, 8], F32)
            S.dma("sp", g_t, g_t[:], self.ln_gT, self.ln_gT[l, 1]); S.dma("sp", b_t, b_t[:], self.ln_bT, self.ln_bT[l, 1])
            onesf = S.sb("onesf", [128, 128], F32)
            S.op("dve", lambda e: e.memset(onesf[:], 1.0 / D), writes=[onesf])
            xb_r = Ring([S.sb("xb", [128, 8, 512], F32) for _ in range(2)])
            sq = S.sb("sq", [128, 8, 512], F32)
            rstd = S.sb("rstd", [128, 512], F32); mean_sb = S.sb("mean_sb", [128, 512], F32)
            for (t0, n) in TBS:
                xb = xb_r.next()
                S.dma("sp", xb, xb[:, :, 0:n], self.XT, self.XT[:, :, t0:t0 + n].rearrange("c p t -> p c t"))
                self.ln_block(xb, n, l, 1, g_t, b_t, sq, onesf, rstd, mean_sb)
                S.dma("sp", self.XT, self.XT[:, :, t0:t0 + n].rearrange("c p t -> p c t"), xb, xb[:, :, 0:n])


def _swap_perm(n_heads, hd):
    q = hd // 4
    idx = []
    for h in range(n_heads):
        b = h * hd
        idx += list(range(b + q, b + 2 * q)) + list(range(b, b + q)) + list(range(b + 3 * q, b + 4 * q)) + list(range(b + 2 * q, b + 3 * q))
    return np.array(idx)


def _rope_tables(hd):
    half = hd // 2; q = half // 2
    inv = 10000.0 ** (-np.arange(0, half, 2, dtype=np.float32) / half)
    t = np.arange(T); row = (t // 64).astype(np.float32); col = (t % 64).astype(np.float32)
    cos = np.ones((hd, NT), np.float32); sin = np.zeros((hd, NT), np.float32)
    for f in range(hd):
        pos = row if f < half else col
        ff = f % half
        ang = pos * inv[ff % q]
        cos[f, :T] = np.cos(ang.astype(np.float32))
        sgn = -1.0 if ff < q else 1.0
        sin[f, :T] = sgn * np.sin(ang.astype(np.float32))
    return cos, sin


def host_consts():
    c = {}
    c["ident"] = np.eye(128, dtype=np.float32)
    rc, rs = _rope_tables(128); c["ret_cos"] = rc; c["ret_sin"] = rs
    gc, gs = _rope_tables(64); c["gq_cos"] = gc; c["gq_sin"] = gs
    p = np.arange(128)[:, None]; f = np.arange(512)[None, :]
    c["delta"] = (f - p).astype(np.float32)
    c["cvals"] = np.broadcast_to((128.0 * (np.arange(21) - 3))[None, :], (128, 21)).astype(np.float32).copy()
    m0 = np.zeros((128, 4, 512), np.float32)
    for i, cc in enumerate((-384, -256, -128, 0)):
        m0[:, i, :] = ((f - p + cc) >= 0)
    c["m0"] = m0
    rm = np.ones((128, NT), np.float32); rm[:, ::32] = 0.0
    c["rmask"] = rm
    j = np.arange(32)[:, None]; i = np.arange(32)[None, :]
    tr = np.zeros((32, 2, 32), np.float32); tr[:, 0, :] = (j <= i); tr[:, 1, :] = (j >= i)
    c["tril"] = tr
    kc = np.arange(64)[:, None]; qc = np.arange(64)[None, :]
    c0 = np.clip(qc - 8, 0, 48)
    v = ((kc >= c0) & (kc < c0 + 16)).astype(np.float32)
    c["validC2"] = np.concatenate([v, v], axis=0)
    sel = np.zeros((32, 32, 128), np.float32)
    for e in range(32):
        sel[e, e, :] = 1.0
    c["sel"] = sel
    return c


def host_params(P):
    o = {}
    f32 = np.float32
    o["b_modT"] = np.ascontiguousarray(P["b_mod"].reshape(4, 48, 128).transpose(0, 2, 1)).astype(f32)
    o["decb"] = np.ascontiguousarray(np.broadcast_to(P["ret_decay"].reshape(4, 1, 8), (4, 128, 8))).astype(f32)
    o["gn_gT"] = np.ascontiguousarray(P["ret_gn_g"].reshape(4, 4, 128).transpose(0, 2, 1)).astype(f32)
    o["gn_bT"] = np.ascontiguousarray(P["ret_gn_b"].reshape(4, 4, 128).transpose(0, 2, 1)).astype(f32)
    o["hg_lbT"] = np.ascontiguousarray(P["hg_lb"].reshape(2, 4, 4, 128).transpose(3, 0, 1, 2)).astype(f32)
    o["hg_ngT"] = np.ascontiguousarray(P["hg_norm_g"].reshape(4, 4, 128).transpose(0, 2, 1)).astype(f32)
    kc = np.arange(64)[:, None]; qc = np.arange(64)[None, :]
    dc = np.clip(kc - qc + 15, 0, 30)
    o["na_T"] = np.ascontiguousarray(P["na_rpb"][:, :, :, dc].transpose(0, 1, 3, 2, 4)).astype(f32)
    sw = _swap_perm(1, 64)
    o["gq_qg"] = np.ascontiguousarray(np.stack([P["gq_qn_g"], P["gq_qn_g"][:, sw]], axis=-1)).astype(f32)
    o["gq_kg"] = np.ascontiguousarray(np.stack([P["gq_kn_g"], P["gq_kn_g"][:, sw]], axis=-1)).astype(f32)
    o["ln_gT"] = np.ascontiguousarray(P["ln_g"].reshape(4, 2, 8, 128).transpose(0, 1, 3, 2)).astype(f32)
    o["ln_bT"] = np.ascontiguousarray(P["ln_b"].reshape(4, 2, 8, 128).transpose(0, 1, 3, 2)).astype(f32)
    o["w_router"] = np.ascontiguousarray(P["w_router"]).astype(f32)
    o["b_router_b"] = np.ascontiguousarray(np.broadcast_to(P["b_router"][:, None, :], (4, 128, 32))).astype(f32)
    o["b_guT"] = np.ascontiguousarray(P["b_gu"].reshape(4, 32, 16, 128).transpose(0, 3, 1, 2)).astype(f32)
    o["b_dn"] = np.ascontiguousarray(P["b_down"]).astype(f32)
    return o


def host_weights(P):
    w_in = P["w_in"]
    ext = np.concatenate([w_in,
                          w_in[:, :, C_RQ + _swap_perm(4, 128)], w_in[:, :, C_RK + _swap_perm(4, 128)],
                          w_in[:, :, C_GQ + _swap_perm(8, 64)], w_in[:, :, C_GK + _swap_perm(2, 64)]], axis=2)
    return dict(w_in=ext.reshape(4096, WIN_COLS), w_mod=P["w_mod"].reshape(4096, 6144),
                w_br=P["w_branch"].reshape(8192, 1024), w_out=P["w_out"].reshape(4096, 1024),
                w_gu=P["w_gu"].reshape(-1, 2048), w_dn=P["w_down"].reshape(-1, 1024))


def host_core_acts(P, b):
    bs = range(b * NB, (b + 1) * NB)
    cT = np.stack([np.stack([P["c"][i].reshape(8, 128).T, P["c_ctx"].reshape(8, 128).T], axis=-1) for i in bs], axis=0)
    return dict(x_in=np.ascontiguousarray(P["x"][b * NB:(b + 1) * NB]), ctx_in=np.ascontiguousarray(P["ctx"][b * NB:(b + 1) * NB]),
                cT=np.ascontiguousarray(cT).astype(np.float32))


class Builder5(Builder4):
    def rsq(self, ot, oap, it, iap, eps, mult=1.0):
        if self.epsc is None:
            self.epsc = {}
        if eps not in self.epsc:
            et = self.S.sb("epsc", [128, 1], F32, persist=True)
            self.S.op("dve", lambda e: e.memset(et[:], eps), writes=[et])
            self.epsc[eps] = et
        et = self.epsc[eps]
        self.act(ot, oap, it, iap, AF.Ln, bias=et[0:oap.shape[0], 0:1], scale=mult, rd=[et])
        self.act(ot, oap, ot, oap, AF.Exp, scale=-0.5)

    def load_hT(self):
        S = self.S
        hT = S.sb("hT", [128, 8, NT], BF16)
        for c in range(8):
            S.dma("sp", hT, hT[:, c, :], self.HT, self.HT[c], join=True)
        return hT

    def proj_fm(self, p, n, wt, wap_fn, hT, t0):
        for k in range(8):
            lap = wap_fn(k)
            self.mm(p, p[0:lap.shape[1], 0:n], wt, lap, hT, hT[:, k, t0:t0 + n], start=(k == 0), stop=(k == 7))

    def phase_gqa(self, l, lw):
        S = self.S
        with S.scope():
            hT = self.load_hT()
            raw_c = S.sb("raw_c", [64, NT], F32); raw_s = S.sb("raw_s", [64, NT], F32)
            S.dma("sp", raw_c, raw_c[:], self.c_gcos, self.c_gcos[:]); S.dma("sp", raw_s, raw_s[:], self.c_gsin, self.c_gsin[:])
            qg = S.sb("qg", [64, 2], F32); kg = S.sb("kg", [64, 2], F32)
            S.dma("sp", qg, qg[:], self.gq_qg, self.gq_qg[l]); S.dma("sp", kg, kg[:], self.gq_kg, self.gq_kg[l])
            tabs = {}
            for nm, g in (("q", qg), ("k", kg)):
                tc_ = S.sb("tc" + nm, [64, NT], F32); ts_ = S.sb("ts" + nm, [64, NT], F32)
                self.ts("dve", tc_, tc_[:], raw_c, raw_c[:], g[:, 0:1], None, ALU.mult, rd=[g])
                self.ts("dve", ts_, ts_[:], raw_s, raw_s[:], g[:, 1:2], None, ALU.mult, rd=[g])
                tabs[nm] = (tc_, ts_)
            wq = S.sb("wq", [128, 8, 512], BF16); wqs = S.sb("wqs", [128, 8, 512], BF16)
            wk = S.sb("wk", [128, 8, 128], BF16); wks = S.sb("wks", [128, 8, 128], BF16); wv = S.sb("wv", [128, 8, 128], BF16)
            self.load_w(wq, wq[:], "w_in", lw * 1024, 8, C_GQ, 512); self.load_w(wqs, wqs[:], "w_in", lw * 1024, 8, C_GQS, 512)
            self.load_w(wk, wk[:], "w_in", lw * 1024, 8, C_GK, 128); self.load_w(wks, wks[:], "w_in", lw * 1024, 8, C_GKS, 128)
            self.load_w(wv, wv[:], "w_in", lw * 1024, 8, C_GV, 128)
            psA = self.ps[0]; psB = self.ps[1]
            pr = Ring(self.ps[2:])
            sq_r = Ring([S.sb("sqn", [64, 512], BF16) for _ in range(2)])
            rs_r = Ring([S.sb("rstd", [64, 512], F32) for _ in range(2)])
            t1_r = Ring([S.sb("t1", [64, 512], F32) for _ in range(2)])
            t2_r = Ring([S.sb("t2", [64, 512], F32) for _ in range(2)])

            def normrope(dst, w_, ws_, c0, tab):
                tc_, ts_ = tab
                for (t0, n) in TBS:
                    p1 = pr.next(); p2 = pr.next(); p3 = pr.next()
                    self.proj_fm(p1, n, w_, lambda k: w_[:, k, c0:c0 + 64], hT, t0)
                    self.proj_fm(p2, n, ws_, lambda k: ws_[:, k, c0:c0 + 64], hT, t0)
                    sq = sq_r.next(); rs = rs_r.next(); t1 = t1_r.next(); t2 = t2_r.next()
                    lvl = 9
                    if lvl >= 1:
                        self.act(sq, sq[:, 0:n], p1, p1[0:64, 0:n], AF.Square)
                    if lvl >= 2:
                        self.mm(p3, p3[0:64, 0:n], self.onesb, self.onesb[0:64, 0:64], sq, sq[:, 0:n])
                    if lvl >= 3:
                        self.rsq(rs, rs[:, 0:n], p3, p3[0:64, 0:n], NORM_EPS, mult=1.0 / 64)
                    if lvl >= 4:
                        S.op("dve", lambda e, t1=t1, p1=p1, n=n, t0=t0: e.tensor_tensor(out=t1[:, 0:n], in0=p1[0:64, 0:n], in1=tc_[:, t0:t0 + n], op=ALU.mult),
                             reads=[p1, tc_, sq], writes=[t1])
                        self.tt("dve", t2, t2[:, 0:n], p2, p2[0:64, 0:n], ts_, ts_[:, t0:t0 + n], ALU.mult)
                    if lvl >= 5:
                        self.tt("pool", t1, t1[:, 0:n], t1, t1[:, 0:n], t2, t2[:, 0:n], ALU.add)
                        self.tt("pool", dst, dst[:, t0:t0 + n], t1, t1[:, 0:n], rs, rs[:, 0:n], ALU.mult)

            stop = 99
            if stop <= 1:
                return
            kT = [S.sb("kT%d" % i, [64, NT], BF16) for i in range(2)]
            for kvh in range(2):
                normrope(kT[kvh], wk, wks, kvh * 64, tabs["k"])
            if stop <= 2:
                return
            V = S.sb("V", [128, 18, 128], BF16)
            for j in range(18):
                p = pr.next()
                for k in range(8):
                    self.mm(p, p[:, 0:128], hT, hT[:, k, j * 128:(j + 1) * 128], wv, wv[:, k, :], start=(k == 0), stop=(k == 7))
                self.act(V, V[:, j, :], p, p[:, 0:128], AF.Identity)
            if stop <= 3:
                return
            qT_r = Ring([S.sb("qT", [64, NT], BF16) for _ in range(2)])
            og_r = Ring([S.sb("ogT", [64, NT], BF16) for _ in range(2)])
            pt_r = Ring([S.sb("PT", [128, 512], BF16) for _ in range(4)])
            rd_r = Ring([S.sb("rd", [64, 512], F32) for _ in range(2)])
            for h in range(8):
                kvh = h // 4
                qT = qT_r.next(); og = og_r.next()
                normrope(qT, wq, wqs, h * 64, tabs["q"])
                if stop <= 4:
                    return
                for (t0, n) in TBS:
                    kts = list(range(18)) if t0 < T else [16, 17]
                    for i, kt in enumerate(kts):
                        pS = pr.next()
                        self.mm(pS, pS[:, 0:n], kT[kvh], kT[kvh][:, kt * 128:(kt + 1) * 128], qT, qT[:, t0:t0 + n])
                        PT = pt_r.next()
                        self.act(PT, PT[:, 0:n], pS, pS[:, 0:n], AF.Exp, scale=0.125)
                        self.mm(psA, psA[0:64, 0:n], V, V[:, kt, kvh * 64:(kvh + 1) * 64], PT, PT[:, 0:n], start=(i == 0), stop=(i == len(kts) - 1))
                        self.mm(psB, psB[0:64, 0:n], self.onesb, self.onesb[:, 0:64], PT, PT[:, 0:n], start=(i == 0), stop=(i == len(kts) - 1))
                    rd = rd_r.next()
                    S.op("dve", lambda e, rd=rd, n=n: e.reciprocal(out=rd[:, 0:n], in_=psB[0:64, 0:n]), reads=[psB], writes=[rd])
                    self.tt("dve", og, og[:, t0:t0 + n], psA, psA[0:64, 0:n], rd, rd[:, 0:n], ALU.mult)
                S.dma("sp", self.OB[3], self.OB[3][h // 2, (h % 2) * 64:(h % 2 + 1) * 64, :], og, og[:], join=True)


class Builder6(Builder5):
    def phase_ret(self, l, lw):
        S = self.S
        with S.scope():
            hT = self.load_hT()
            cosT = S.sb("cosT", [128, NT], F32); sinT = S.sb("sinT", [128, NT], F32)
            S.dma("sp", cosT, cosT[:], self.c_rcos, self.c_rcos[:]); S.dma("sp", sinT, sinT[:], self.c_rsin, self.c_rsin[:])
            delta = S.sb("delta", [128, 512], F32); cv = S.sb("cv", [128, 21], F32); m0 = S.sb("m0", [128, 4, 512], F32)
            S.dma("sp", delta, delta[:], self.c_delta, self.c_delta[:]); S.dma("sp", cv, cv[:], self.c_cvals, self.c_cvals[:])
            S.dma("sp", m0, m0[:], self.c_m0, self.c_m0[:])
            dec = S.sb("dec", [128, 8], F32); lgt = S.sb("lgt", [128, 8], F32); nlg = S.sb("nlg", [128, 8], F32)
            S.dma("sp", dec, dec[:], self.decb, self.decb[l])
            self.act(lgt, lgt[:], dec, dec[:], AF.Sigmoid)
            self.act(lgt, lgt[:], lgt, lgt[:], AF.Ln)
            self.ts("dve", nlg, nlg[:], lgt, lgt[:], -1.0, None, ALU.mult)
            gng = S.sb("gng", [128, 4], F32); gnb = S.sb("gnb", [128, 4], F32)
            S.dma("sp", gng, gng[:], self.gn_gT, self.gn_gT[l]); S.dma("sp", gnb, gnb[:], self.gn_bT, self.gn_bT[l])
            onesf = S.sb("onesf", [128, 128], F32)
            S.op("dve", lambda e: e.memset(onesf[:], 1.0 / 128), writes=[onesf])
            wh_r = Ring([S.sb("wh", [128, 8, 6, 128], BF16) for _ in range(1)])
            qr = S.sb("qr", [128, NT], BF16); kr = S.sb("kr", [128, NT], BF16); sg = S.sb("sgate", [128, NT], BF16)
            V = S.sb("V", [128, 18, 128], BF16); og = S.sb("og", [128, NT], BF16)
            E0 = S.sb("E0", [128, 512], F32); E1 = S.sb("E1", [128, 512], F32)
            g0 = S.sb("g0", [128, 21], F32); g1 = S.sb("g1", [128, 21], F32)
            Dd = S.sb("Dd", [128, 4, 512], F32); Dc = S.sb("Dc", [128, 8, 512], F32)
            t1_r = Ring([S.sb("t1", [128, 512], F32) for _ in range(2)]); t2_r = Ring([S.sb("t2", [128, 512], F32) for _ in range(2)])
            pt_r = Ring([S.sb("PT", [128, 512], BF16) for _ in range(4)])
            osb = S.sb("osb", [128, 512], F32); osq = S.sb("osq", [128, 512], F32); mean_sb = S.sb("mean", [128, 512], F32)
            rstd = S.sb("rstd", [128, 512], F32)
            psA = self.ps[0]; pr = Ring(self.ps[1:])
            cols = (C_RQ, C_RQS, C_RK, C_RKS, C_RV, C_RG)
            ci = lambda c: c // 128 + 3
            for h in range(4):
                wh = wh_r.next()
                for i, c0 in enumerate(cols):
                    self.load_w(wh, wh[:, :, i, :], "w_in", lw * 1024, 8, c0 + h * 128, 128, join=True)
                for (t0, n) in TBS:
                    for (dst, a, b) in ((qr, 0, 1), (kr, 2, 3)):
                        p1 = pr.next(); p2 = pr.next()
                        self.proj_fm(p1, n, wh, lambda k, a=a: wh[:, k, a, :], hT, t0)
                        self.proj_fm(p2, n, wh, lambda k, b=b: wh[:, k, b, :], hT, t0)
                        t1 = t1_r.next(); t2 = t2_r.next()
                        self.tt("dve", t1, t1[:, 0:n], p1, p1[:, 0:n], cosT, cosT[:, t0:t0 + n], ALU.mult)
                        self.tt("dve", t2, t2[:, 0:n], p2, p2[:, 0:n], sinT, sinT[:, t0:t0 + n], ALU.mult)
                        self.tt("pool", dst, dst[:, t0:t0 + n], t1, t1[:, 0:n], t2, t2[:, 0:n], ALU.add)
                    p3 = pr.next()
                    self.proj_fm(p3, n, wh, lambda k: wh[:, k, 5, :], hT, t0)
                    self.act(sg, sg[:, t0:t0 + n], p3, p3[:, 0:n], AF.Silu)
                for j in range(18):
                    p = pr.next()
                    for k in range(8):
                        self.mm(p, p[:, 0:128], hT, hT[:, k, j * 128:(j + 1) * 128], wh, wh[:, k, 4, :], start=(k == 0), stop=(k == 7))
                    self.act(V, V[:, j, :], p, p[:, 0:128], AF.Identity)
                self.act(E0, E0[:], delta, delta[:], AF.Exp, scale=lgt[:, h:h + 1], rd=[lgt])
                self.act(E1, E1[:], delta, delta[:], AF.Exp, scale=nlg[:, 4 + h:5 + h], rd=[nlg])
                self.act(g0, g0[:], cv, cv[:], AF.Exp, scale=lgt[:, h:h + 1], rd=[lgt])
                self.act(g1, g1[:], cv, cv[:], AF.Exp, scale=lgt[:, 4 + h:5 + h], rd=[lgt])
                self.ts("dve", g0, g0[:], g0, g0[:], 128.0 ** -0.5, None, ALU.mult)
                self.ts("dve", g1, g1[:], g1, g1[:], 128.0 ** -0.5, None, ALU.mult)
                for i, c in enumerate((-384, -256, -128, 0)):
                    t1 = t1_r.next(); t2 = t2_r.next()
                    self.stt("dve", t1, t1[:], m0, m0[:, i, :], g0[:, ci(c):ci(c) + 1], E0, E0[:], ALU.mult, ALU.mult, rd=[g0])
                    self.stt("dve", t2, t2[:], m0, m0[:, i, :], g1[:, ci(-c):ci(-c) + 1], E1, E1[:], ALU.mult, ALU.mult, rd=[g1])
                    self.tt("pool", t1, t1[:], t1, t1[:], t2, t2[:], ALU.subtract)
                    self.stt("dve", Dd, Dd[:, i, :], E1, E1[:], g1[:, ci(-c):ci(-c) + 1], t1, t1[:], ALU.mult, ALU.add, rd=[g1])
                for qb in range(4):
                    for kk in range(2):
                        c0 = qb * 512 - kk * 128 + 256; c1 = 2048 - qb * 512 + kk * 128
                        t1 = t1_r.next()
                        self.ts("dve", t1, t1[:], E0, E0[:], g0[:, ci(c0):ci(c0) + 1], None, ALU.mult, rd=[g0])
                        self.stt("dve", Dc, Dc[:, qb * 2 + kk, :], E1, E1[:], g1[:, ci(c1):ci(c1) + 1], t1, t1[:], ALU.mult, ALU.add, rd=[g1])
                for qb, (t0, n) in enumerate(TBS):
                    kts = list(range(18)) if t0 < T else [16, 17]
                    for i, kt in enumerate(kts):
                        pS = pr.next()
                        self.mm(pS, pS[:, 0:n], kr, kr[:, kt * 128:(kt + 1) * 128], qr, qr[:, t0:t0 + n])
                        PT = pt_r.next()
                        if t0 >= T:
                            c = -(kt - 16) * 128
                            self.tt("dve", PT, PT[:, 0:n], pS, pS[:, 0:n], Dd, Dd[:, (c + 384) // 128, 0:n], ALU.mult)
                        elif kt >= 16:
                            self.tt("dve", PT, PT[:, 0:n], pS, pS[:, 0:n], Dc, Dc[:, qb * 2 + kt - 16, 0:n], ALU.mult)
                        else:
                            c = qb * 512 - kt * 128
                            if c >= 128:
                                self.stt("dve", PT, PT[:, 0:n], pS, pS[:, 0:n], g0[:, ci(c):ci(c) + 1], E0, E0[:, 0:n], ALU.mult, ALU.mult, rd=[g0])
                            elif c <= -512:
                                self.stt("dve", PT, PT[:, 0:n], pS, pS[:, 0:n], g1[:, ci(-c):ci(-c) + 1], E1, E1[:, 0:n], ALU.mult, ALU.mult, rd=[g1])
                            else:
                                self.tt("dve", PT, PT[:, 0:n], pS, pS[:, 0:n], Dd, Dd[:, (c + 384) // 128, 0:n], ALU.mult)
                        self.mm(psA, psA[:, 0:n], V, V[:, kt, :], PT, PT[:, 0:n], start=(i == 0), stop=(i == len(kts) - 1))
                    self.act(osb, osb[:, 0:n], psA, psA[:, 0:n], AF.Identity)
                    self.act(osq, osq[:, 0:n], psA, psA[:, 0:n], AF.Square)
                    pm = pr.next(); pe2 = pr.next()
                    self.mm(pm, pm[:, 0:n], onesf, onesf[:], osb, osb[:, 0:n])
                    self.mm(pe2, pe2[:, 0:n], onesf, onesf[:], osq, osq[:, 0:n])
                    self.act(mean_sb, mean_sb[:, 0:n], pm, pm[:, 0:n], AF.Identity)
                    self.tt("dve", rstd, rstd[:, 0:n], mean_sb, mean_sb[:, 0:n], mean_sb, mean_sb[:, 0:n], ALU.mult)
                    self.tt("dve", rstd, rstd[:, 0:n], pe2, pe2[:, 0:n], rstd, rstd[:, 0:n], ALU.subtract)
                    self.rsq(rstd, rstd[:, 0:n], rstd, rstd[:, 0:n], LN_EPS)
                    self.tt("pool", osb, osb[:, 0:n], osb, osb[:, 0:n], mean_sb, mean_sb[:, 0:n], ALU.subtract)
                    self.tt("pool", osb, osb[:, 0:n], osb, osb[:, 0:n], rstd, rstd[:, 0:n], ALU.mult)
                    self.act(osb, osb[:, 0:n], osb, osb[:, 0:n], AF.Identity, bias=gnb[:, h:h + 1], scale=gng[:, h:h + 1], rd=[gng, gnb])
                    self.tt("pool", og, og[:, t0:t0 + n], osb, osb[:, 0:n], sg, sg[:, t0:t0 + n], ALU.mult)
                S.dma("sp", self.OB[0], self.OB[0][h], og, og[:], join=True)


class Builder7(Builder6):
    def mmx(self, pt, pap, lt, lap, rt, rap, start=True, stop=True, inc=True):
        self.S.op("pe", lambda e: e.matmul(pap, lhsT=lap, rhs=rap, start=start, stop=stop),
                  reads=[lt, rt], writes=[pt], inc=inc)

    def phase_na(self, l, lw):
        S = self.S
        with S.scope():
            hT = self.load_hT()
            vc2 = S.sb("vc2", [128, 64], F32)
            S.dma("sp", vc2, vc2[:], self.c_validC2, self.c_validC2[:])
            w_r = Ring([S.sb("wna", [128, 8, 3, 64], BF16) for _ in range(2)])
            qT_r = Ring([S.sb("qT", [64, NT], BF16) for _ in range(2)]); kT_r = Ring([S.sb("kT", [64, NT], BF16) for _ in range(2)])
            Ve_r = Ring([S.sb("Ve", [128, 18, 64], BF16) for _ in range(2)]); Vo_r = Ring([S.sb("Vo", [128, 16, 64], BF16) for _ in range(2)])
            tbr_r = Ring([S.sb("tbraw", [128, 14, 64], F32) for _ in range(2)]); tb_r = Ring([S.sb("tb2", [128, 14, 64], F32) for _ in range(2)])
            og_r = Ring([S.sb("og", [64, NT], BF16) for _ in range(2)])
            P_r = Ring([S.sb("P", [128, 6, 64], BF16) for _ in range(3)])
            rd_r = Ring([S.sb("rd", [64, 64], F32) for _ in range(3)])
            psO = Ring(self.ps[0:2]); psD = Ring(self.ps[2:4]); pr = Ring(self.ps[4:8])
            for h in range(8):
                w = w_r.next(); qT = qT_r.next(); kT = kT_r.next(); Ve = Ve_r.next(); Vo = Vo_r.next()
                tbraw = tbr_r.next(); tb2 = tb_r.next(); og = og_r.next()
                for i, c0 in enumerate((C_NQ, C_NK, C_NV)):
                    self.load_w(w, w[:, :, i, :], "w_in", lw * 1024, 8, c0 + h * 64, 64, join=True)
                src = self.na_T[l, h]
                S.dma("sp", tbraw, tbraw[0:64, :, :], self.na_T, src[:, 0:14, :], join=True)
                S.dma("sp", tbraw, tbraw[64:128, :, :], self.na_T, src[:, 1:15, :], join=True)
                self.act(tbraw, tbraw[:], tbraw, tbraw[:], AF.Exp)
                self.tt("dve", tb2, tb2[:], tbraw, tbraw[:], vc2, _bcast_mid(vc2[:], 14), ALU.mult)
                for (t0, n) in TBS:
                    p1 = pr.next(); p2 = pr.next()
                    self.proj_fm(p1, n, w, lambda k: w[:, k, 0, :], hT, t0)
                    self.proj_fm(p2, n, w, lambda k: w[:, k, 1, :], hT, t0)
                    self.act(qT, qT[:, t0:t0 + n], p1, p1[0:64, 0:n], AF.Identity)
                    S.op("dve", lambda e, kT=kT, p2=p2, t0=t0, n=n: e.tensor_copy(out=kT[:, t0:t0 + n], in_=p2[0:64, 0:n]), reads=[p2], writes=[kT])
                for (Vt, off, cnt) in ((Ve, 0, 18), (Vo, 64, 15)):
                    for j0 in range(0, cnt, 8):
                        jn = min(8, cnt - j0)
                        p = pr.next()
                        for jj in range(jn):
                            tok = off + (j0 + jj) * 128
                            for k in range(8):
                                self.mmx(p, p[:, jj * 64:(jj + 1) * 64], hT, hT[:, k, tok:tok + 128], w, w[:, k, 2, :],
                                         start=(k == 0), stop=(k == 7), inc=(k == 7 and jj == jn - 1))
                        self.act(Vt, Vt[:, j0:j0 + jn, :], p, p[:, 0:jn * 64].rearrange("p (j d) -> p j d", d=64), AF.Identity)
                for qr in range(36):
                    lat = qr < 32
                    q0 = qr * 64
                    if lat:
                        R0 = min(max(qr - 4, 0), 24); dr0 = R0 - qr + 7
                        ktoks = [R0 * 64 + 128 * j for j in range(4)] + [2048, 2176]
                        if R0 % 2 == 0:
                            vts = [(Ve, R0 // 2 + j) for j in range(4)]
                        else:
                            vts = [(Vo, (R0 - 1) // 2 + j) for j in range(4)]
                        vts += [(Ve, 16), (Ve, 17)]
                    else:
                        ktoks = [2048, 2176]; vts = [(Ve, 16), (Ve, 17)]
                    nk = len(ktoks)
                    pS = pr.next(); P = P_r.next()
                    for j, tok in enumerate(ktoks):
                        self.mmx(pS, pS[:, j * 64:(j + 1) * 64], kT, kT[:, tok:tok + 128], qT, qT[:, q0:q0 + 64], inc=(j == nk - 1))
                    self.act(P, P[:, 0:nk, :], pS, pS[:, 0:nk * 64].rearrange("p (j q) -> p j q", q=64), AF.Exp, scale=0.125)
                    if lat:
                        self.tt("dve", P, P[:, 0:4, :], P, P[:, 0:4, :], tb2, tb2[:, dr0:dr0 + 7:2, :], ALU.mult)
                    pO = psO.next(); pD = psD.next()
                    for j, (Vt, vi) in enumerate(vts):
                        self.mmx(pO, pO[0:64, 0:64], Vt, Vt[:, vi, :], P, P[:, j, :], start=(j == 0), stop=(j == nk - 1), inc=False)
                        self.mmx(pD, pD[0:64, 0:64], self.onesb, self.onesb[:, 0:64], P, P[:, j, :], start=(j == 0), stop=(j == nk - 1),
                                 inc=(j == nk - 1))
                    rd = rd_r.next()
                    S.op("dve", lambda e, rd=rd, pD=pD: e.reciprocal(out=rd[:], in_=pD[0:64, 0:64]), reads=[pD], writes=[rd])
                    self.tt("dve", og, og[:, q0:q0 + 64], pO, pO[0:64, 0:64], rd, rd[:], ALU.mult)
                S.dma("sp", self.OB[2], self.OB[2][h // 2, (h % 2) * 64:(h % 2 + 1) * 64, :], og, og[:], join=True)


class Builder8(Builder7):
    def phase_hg(self, l, lw):
        S = self.S
        NCH = NT // 32
        with S.scope():
            hT = self.load_hT()
            rmask = S.sb("rmask", [128, NT], F32); tril = S.sb("tril", [32, 2, 32], F32)
            S.dma("sp", rmask, rmask[:], self.c_rmask, self.c_rmask[:]); S.dma("sp", tril, tril[:], self.c_tril, self.c_tril[:])
            lbt = S.sb("lbt", [128, 2, 4, 4], F32); ssum = S.sb("ssum", [128, 2, 4], F32)
            low = S.sb("low", [128, 2, 4], F32); oml = S.sb("oml", [128, 2, 4], F32); noml = S.sb("noml", [128, 2, 4], F32)
            S.dma("sp", lbt, lbt[:], self.hg_lbT, self.hg_lbT[:])
            self.act(lbt, lbt[:], lbt, lbt[:], AF.Exp)
            self.tt("dve", ssum, ssum[:], lbt, lbt[:, :, 0, :], lbt, lbt[:, :, 1, :], ALU.add)
            self.tt("dve", ssum, ssum[:], ssum, ssum[:], lbt, lbt[:, :, 2, :], ALU.add)
            self.tt("dve", ssum, ssum[:], ssum, ssum[:], lbt, lbt[:, :, 3, :], ALU.add)
            S.op("dve", lambda e: e.reciprocal(out=ssum[:], in_=ssum[:]), reads=[ssum], writes=[ssum])
            S.op("dve", lambda e: e.memset(low[:], 0.0), writes=[low])
            for i in range(1, l + 1):
                self.tt("dve", low, low[:], low, low[:], lbt, lbt[:, :, i, :], ALU.add)
            self.tt("dve", low, low[:], low, low[:], ssum, ssum[:], ALU.mult)
            self.ts("dve", oml, oml[:], low, low[:], -1.0, 1.0, ALU.mult, ALU.add)
            self.ts("dve", noml, noml[:], oml, oml[:], -1.0, None, ALU.mult)
            ngt = S.sb("ngt", [128, 4], F32)
            S.dma("sp", ngt, ngt[:], self.hg_ngT, self.hg_ngT[l])
            onesf = S.sb("onesf", [128, 128], F32)
            S.op("dve", lambda e: e.memset(onesf[:], 1.0 / 128), writes=[onesf])
            wh = S.sb("wh", [128, 8, 5, 128], BF16)
            qf = S.sb("qf", [128, NT], F32); sg = S.sb("sgate", [128, NT], BF16); osum = S.sb("osum", [128, NT], F32)
            vtok = S.sb("vtok", [32, NCH, 128], BF16)
            qd = [S.sb("qd%d" % d, [128, NT], BF16) for d in range(2)]; kd = [S.sb("kd%d" % d, [128, NT], BF16) for d in range(2)]
            kl = [S.sb("kl%d" % d, [128, NT], F32) for d in range(2)]; dcy = [S.sb("dcy%d" % d, [128, NCH], F32) for d in range(2)]
            kdz = [S.sb("kdz%d" % d, [128, NT], BF16) for d in range(2)]; emid = [S.sb("emid%d" % d, [128, NCH], F32) for d in range(2)]
            mid = S.sb("mid", [128, NCH], F32)
            for d in range(2):
                S.op("pool", lambda e, d=d: e.memset(kdz[d][:], 0.0), writes=[kdz[d]])
            tot = S.sb("tot", [128, NCH], F32)
            Tm = [S.sb("T%d" % i, [128, NT], F32) for i in range(4)]
            St = [S.sb("S%d" % d, [128, 128], F32) for d in range(2)]; Sb = [S.sb("Sb%d" % d, [128, 128], BF16) for d in range(2)]
            Am_r = Ring([S.sb("Am", [32, 32], BF16) for _ in range(4)]); klt_r = Ring([S.sb("klt", [32, 128], BF16) for _ in range(4)])
            og = S.sb("og", [128, NT], BF16)
            pr = self.psr
            v3 = lambda t: t[:].rearrange("p (c i) -> p c i", i=32)
            cols = (C_HQ, C_HF, C_HB, C_HI, C_HG)
            for h in range(4):
                for i, c0 in enumerate(cols):
                    self.load_w(wh, wh[:, :, i, :], "w_in", lw * 1024, 8, c0 + h * 128, 128, join=True)
                T1, T2, T3, T4 = Tm
                for (t0, n) in TBS:
                    p1 = pr.next(); p2 = pr.next(); p3 = pr.next()
                    self.proj_fm(p1, n, wh, lambda k: wh[:, k, 0, :], hT, t0)
                    self.proj_fm(p2, n, wh, lambda k: wh[:, k, 4, :], hT, t0)
                    self.proj_fm(p3, n, wh, lambda k: wh[:, k, 3, :], hT, t0)
                    self.act(qf, qf[:, t0:t0 + n], p1, p1[:, 0:n], AF.Silu)
                    self.act(sg, sg[:, t0:t0 + n], p2, p2[:, 0:n], AF.Silu)
                    S.op("dve", lambda e, p3=p3, t0=t0, n=n: e.tensor_copy(out=T4[:, t0:t0 + n], in_=p3[:, 0:n]), reads=[p3], writes=[T4])
                for c0 in range(0, NCH, 4):
                    p = pr.next()
                    for cc in range(4):
                        c = c0 + cc
                        S.op("pe", lambda e, p=p, cc=cc, c=c: e.transpose(out=p[0:32, cc * 128:(cc + 1) * 128], in_=T4[:, c * 32:(c + 1) * 32],
                                                                          identity=self.ident[:]), reads=[T4, self.ident], writes=[p], inc=(cc == 3))
                    self.act(vtok, vtok[:, c0:c0 + 4, :], p, p[0:32, :].rearrange("p (c d) -> p c d", d=128), AF.Identity)
                for d in range(2):
                    lb_ = low[:, d, h:h + 1]; om_ = oml[:, d, h:h + 1]; nom_ = noml[:, d, h:h + 1]
                    for (t0, n) in TBS:
                        p1 = pr.next()
                        self.proj_fm(p1, n, wh, lambda k, d=d: wh[:, k, 1 + d, :], hT, t0)
                        self.act(T1, T1[:, t0:t0 + n], p1, p1[:, 0:n], AF.Sigmoid)
                    self.ts("dve", T3, T3[:], T1, T1[:], om_, lb_, ALU.mult, ALU.add, rd=[oml, low])
                    self.ts("dve", T2, T2[:], T1, T1[:], nom_, om_, ALU.mult, ALU.add, rd=[oml, noml])
                    self.act(T3, T3[:], T3, T3[:], AF.Ln)
                    S.op("dve", lambda e: e.tensor_tensor_scan(out=T1[:], data0=rmask[:], data1=T3[:], initial=0.0, op0=ALU.mult, op1=ALU.add),
                         reads=[rmask, T3], writes=[T1])
                    S.op("dve", lambda e: e.tensor_copy(out=tot[:], in_=v3(T1)[:, :, 31]), reads=[T1], writes=[tot])
                    self.act(dcy[d], dcy[d][:], tot, tot[:], AF.Exp)
                    self.tt("dve", T4, v3(T4), tot, _bcast_last(tot[:], 32), T1, v3(T1), ALU.subtract)
                    if d == 0:
                        lc, ek = T1, T4
                    else:
                        self.tt("dve", T4, T4[:], T4, T4[:], T3, T3[:], ALU.add)
                        self.tt("pool", T1, T1[:], T1, T1[:], T3, T3[:], ALU.subtract)
                        lc, ek = T4, T1
                    self.act(ek, ek[:], ek, ek[:], AF.Exp)
                    self.tt("dve", kl[d], kl[d][:], T2, T2[:], ek, ek[:], ALU.mult)
                    mi = 15 if d == 0 else 16
                    S.op("dve", lambda e, lc=lc, mi=mi: e.tensor_copy(out=mid[:], in_=v3(lc)[:, :, mi]), reads=[lc], writes=[mid])
                    self.act(emid[d], emid[d][:], mid, mid[:], AF.Exp)
                    self.tt("dve", lc, v3(lc), lc, v3(lc), mid, _bcast_last(mid[:], 32), ALU.subtract)
                    self.act(ek, ek[:], lc, lc[:], AF.Exp)
                    self.tt("dve", qd[d], qd[d][:], qf, qf[:], ek, ek[:], ALU.mult)
                    self.act(ek, ek[:], lc, lc[:], AF.Exp, scale=-1.0)
                    self.tt("dve", kd[d], kd[d][:], T2, T2[:], ek, ek[:], ALU.mult)
                    keep = slice(0, 16) if d == 0 else slice(16, 32)
                    self.tt("pool", kdz[d], v3(kdz[d])[:, :, keep], T2, v3(T2)[:, :, keep], ek, v3(ek)[:, :, keep], ALU.mult)
                S.op("dve", lambda e: e.memset(osum[:], 0.0), writes=[osum])
                for d in range(2):
                    S.op("dve", lambda e, d=d: e.memset(St[d][:], 0.0), writes=[St[d]])
                    S.op("dve", lambda e, d=d: e.memset(Sb[d][:], 0.0), writes=[Sb[d]])
                order = [list(range(64, 72)) + list(range(0, 64)), list(range(71, 63, -1)) + list(range(63, -1, -1))]
                for s in range(NCH):
                    for d in range(2):
                        c = order[d][s]
                        sl = slice(c * 32, (c + 1) * 32)
                        pA = pr.next(); pT = pr.next(); pO = pr.next(); pS = pr.next()
                        h0 = slice(c * 32, c * 32 + 16); h1 = slice(c * 32 + 16, (c + 1) * 32)
                        ka, kb = (kdz[0], kd[0]) if d == 0 else (kd[1], kdz[1])
                        self.mmx(pA, pA[0:32, 0:16], ka, ka[:, sl], qd[d], qd[d][:, h0], inc=False)
                        self.mmx(pA, pA[0:32, 16:32], kb, kb[:, sl], qd[d], qd[d][:, h1], inc=True)
                        Am = Am_r.next()
                        self.tt("dve", Am, Am[:], pA, pA[0:32, 0:32], tril, tril[:, d, :], ALU.mult)
                        S.op("pe", lambda e, pT=pT, d=d, sl=sl: e.transpose(out=pT[0:32, 0:128], in_=kl[d][:, sl], identity=self.ident[:]),
                             reads=[kl[d], self.ident], writes=[pT])
                        klt = klt_r.next()
                        self.act(klt, klt[:], pT, pT[0:32, 0:128], AF.Identity)
                        self.mm(pO, pO[:, 0:32], vtok, vtok[:, c, :], Am, Am[:], start=True, stop=False)
                        self.mm(pO, pO[:, 0:32], Sb[d], Sb[d][:], qd[d], qd[d][:, sl], start=False, stop=True)
                        self.tt("dve", osum, osum[:, sl], osum, osum[:, sl], pO, pO[:, 0:32], ALU.add)
                        self.mm(pS, pS[:, 0:128], klt, klt[:], vtok, vtok[:, c, :])
                        self.stt("dve", St[d], St[d][:], St[d], St[d][:], dcy[d][:, c:c + 1], pS, pS[:, 0:128], ALU.mult, ALU.add, rd=[dcy[d]])
                        cn = order[d][s + 1] if s + 1 < NCH else c
                        self.act(Sb[d], Sb[d][:], St[d], St[d][:], AF.Identity, scale=emid[d][:, cn:cn + 1], rd=[emid[d]])
                for (t0, n) in TBS:
                    self.act(T1, T1[:, t0:t0 + n], osum, osum[:, t0:t0 + n], AF.Square)
                    pss = pr.next()
                    self.mm(pss, pss[:, 0:n], onesf, onesf[:], T1, T1[:, t0:t0 + n])
                    self.rsq(T2, T2[:, t0:t0 + n], pss, pss[:, 0:n], NORM_EPS)
                    self.tt("dve", T3, T3[:, t0:t0 + n], osum, osum[:, t0:t0 + n], T2, T2[:, t0:t0 + n], ALU.mult)
                    self.act(T3, T3[:, t0:t0 + n], T3, T3[:, t0:t0 + n], AF.Identity, scale=ngt[:, h:h + 1], rd=[ngt])
                    self.tt("pool", og, og[:, t0:t0 + n], T3, T3[:, t0:t0 + n], sg, sg[:, t0:t0 + n], ALU.mult)
                S.dma("sp", self.OB[1], self.OB[1][h], og, og[:], join=True)


class BuilderN(Builder8):
    def build_all(self):
        for bi in range(NB):
            self.bi = bi
            self.build_one()
        self.finish()

    def build_one(self):
        self.phase_init()
        for l in self.layers:
            self.phase_mod(l, l)
            self.phase_hT(0, self.XT, self.HT)
            self.phase_ret(l, l)
            self.phase_hg(l, l)
            self.phase_na(l, l)
            self.phase_gqa(l, l)
            self.phase_merge(l, l)
            self.phase_moe(l, l)
            self.phase_ln2(l)
        self.phase_final()


_W_KEYS = ("w_in", "w_mod", "w_br", "w_out", "w_gu", "w_dn")


def kernel(**inputs):
    P = {k: np.asarray(v) for k, v in inputs.items()}
    consts = host_consts(); params = host_params(P); W = host_weights(P)
    nc = bass.Bass("TRN2", target_bir_lowering=False)
    B = BuilderN(nc, full=True)
    B.build_all()
    in_maps = []
    for b in range(NCORES):
        m = {}
        m.update(consts); m.update(params); m.update(host_core_acts(P, b))
        for k in _W_KEYS:
            m[k + "_s"] = W[k]
        in_maps.append({k: np.ascontiguousarray(v, dtype=np.float32) for k, v in m.items()})
    res = run_bass_kernel_spmd(nc, in_maps, core_ids=list(range(NCORES)))
    return np.concatenate([np.asarray(res.results[b]["out"], np.float32) for b in range(NCORES)], axis=0)
```

```python
import numpy as np
import concourse.bass as bass
import concourse.mybir as mybir
from concourse.bass_utils import run_bass_kernel_spmd

F32 = mybir.dt.float32
BF16 = mybir.dt.bfloat16
AF = mybir.ActivationFunctionType
ALU = mybir.AluOpType
AX = mybir.AxisListType

D = 1024; T = 2048; LC = 256; NT = 2304; DEPTH = 4
NCORES = 8
NB = 8 // NCORES
TBS = [(0, 512), (512, 512), (1024, 512), (1536, 512), (2048, 256)]
WIN_COLS = 12672
C_RQ, C_RK, C_RV, C_RG = 0, 512, 1024, 1536
C_HQ, C_HF, C_HB, C_HI, C_HG = 2048, 2560, 3072, 3584, 4096
C_NQ, C_NK, C_NV = 4608, 5120, 5632
C_GQ, C_GK, C_GV = 6144, 6656, 6784
C_GATE = 6912
C_RQS, C_RKS, C_GQS, C_GKS = 11008, 11520, 12032, 12544
DN_ALPHA = (2 * DEPTH) ** 0.25
LN_EPS = 1e-5
NORM_EPS = 1e-6


class Buf:
    def __init__(self, ap, name):
        self.ap = ap; self.name = name
        self.writes = {}; self.reads = {}; self.dsem = None; self.dcnt = 0
        self.is_psum = name.startswith("ps")

    def __getitem__(self, idx):
        return self.ap[idx]


class Ring:
    def __init__(self, tiles):
        self.tiles = tiles; self.i = 0

    def next(self):
        t = self.tiles[self.i % len(self.tiles)]; self.i += 1
        return t


class Sched:
    def __init__(self, nc):
        self.nc = nc
        self.E = {}
        for n, e in (("pe", nc.tensor), ("act", nc.scalar), ("dve", nc.vector),
                     ("pool", nc.gpsimd), ("sp", nc.sync)):
            self.E[n] = dict(e=e, sem=nc.alloc_semaphore("s_" + n), cnt=0, seen={})
        self.bsem = nc.alloc_semaphore("s_bar"); self.bcnt = 0
        self.dsems = []; self.free_dsems = []
        self.nwait = 0; self.nins = 0; self.uid = 0

    def sb(self, name, shape, dt):
        self.uid += 1
        return Buf(self.nc.alloc_sbuf_tensor("%s_%d" % (name, self.uid), list(shape), dt).ap(), name)

    def dram(self, name, shape, dt, kind="Internal"):
        return Buf(self.nc.dram_tensor(name, list(shape), dt, kind=kind).ap(), name)

    def _wait(self, en, events):
        E = self.E[en]
        for sem, val in events.items():
            if E["seen"].get(sem, 0) >= val:
                continue
            E["e"].wait_ge(sem, val)
            E["seen"][sem] = val
            self.nwait += 1

    @staticmethod
    def _merge(dst, src):
        for s, v in src.items():
            if v > dst.get(s, 0):
                dst[s] = v

    def op(self, en, fn, reads=(), writes=(), inc=True):
        E = self.E[en]
        ev = {}
        for t in reads:
            self._merge(ev, t.writes)
            if t.is_psum and en != "pe":
                self._merge(ev, t.reads)
        for t in writes:
            self._merge(ev, t.writes); self._merge(ev, t.reads)
        if ev.get(E["sem"], 0) > E["cnt"]:
            del ev[E["sem"]]
        self._wait(en, ev)
        ins = fn(E["e"])
        self.nins += 1
        nxt = E["cnt"] + 1
        if inc:
            ins.then_inc(E["sem"], 1); E["cnt"] = nxt
        me = {E["sem"]: nxt}
        for t in reads:
            self._merge(t.reads, me)
        for t in writes:
            t.writes = dict(me); t.reads = {}
        return ins

    def dma(self, en, out_t, out_ap, in_t, in_ap, join=False, **kw):
        E = self.E[en]
        if out_t.dsem is None:
            out_t.dsem = self.nc.alloc_semaphore("d%d_%s" % (len(self.dsems), out_t.name))
            self.dsems.append(out_t)
        ev = {}
        self._merge(ev, in_t.writes)
        w = dict(out_t.writes)
        if join:
            w.pop(out_t.dsem, None)
        self._merge(ev, w); self._merge(ev, out_t.reads)
        self._wait(en, ev)
        ins = E["e"].dma_start(out=out_ap, in_=in_ap, **kw)
        self.nins += 1
        ins.then_inc(out_t.dsem, 16)
        out_t.dcnt += 16
        me = {out_t.dsem: out_t.dcnt}
        self._merge(in_t.reads, me)
        out_t.writes = dict(me); out_t.reads = {}
        return ins

    def barrier(self):
        ev = {}
        for n, E in self.E.items():
            if n != "sp" and E["cnt"] > 0:
                ev[E["sem"]] = E["cnt"]
        for t in self.dsems:
            ev[t.dsem] = t.dcnt
        self._wait("sp", ev)
        self.bcnt += 1
        self.nc.sync.sem_inc(self.bsem, 1)
        for n in self.E:
            if n != "sp":
                self._wait(n, {self.bsem: self.bcnt})


from contextlib import ExitStack, contextmanager


class SchedX(Sched):
    def __init__(self, nc):
        super().__init__(nc)
        self.es = None
        self.scope_tiles = None
        self.dsem_cnt = {}
        self.free_sems = []

    @contextmanager
    def scope(self):
        assert self.es is None
        with ExitStack() as es:
            self.es = es; self.scope_tiles = []
            yield
            self.barrier()
            for t in self.scope_tiles:
                if t.dsem is not None:
                    self.free_sems.append(t.dsem); t.dsem = None
            self.es = None; self.scope_tiles = None

    def sb(self, name, shape, dt, persist=False):
        self.uid += 1
        nm = "%s_%d" % (name, self.uid)
        if self.es is None or persist:
            h = self.nc.alloc_sbuf_tensor(nm, list(shape), dt)
            return Buf(h.ap(), name)
        h = self.es.enter_context(self.nc.sbuf_tensor(nm, list(shape), dt))
        t = Buf(h.ap(), name)
        self.scope_tiles.append(t)
        return t

    def _get_dsem(self, t):
        if t.dsem is None:
            if self.free_sems:
                t.dsem = self.free_sems.pop()
            else:
                t.dsem = self.nc.alloc_semaphore("d%d" % len(self.dsem_cnt))
                self.dsem_cnt[t.dsem] = 0
        return t.dsem

    def dma(self, en, out_t, out_ap, in_t, in_ap, join=False, cc=None, **kw):
        E = self.E[en]
        sem = self._get_dsem(out_t)
        ev = {}
        self._merge(ev, in_t.writes)
        w = dict(out_t.writes)
        if join:
            w.pop(sem, None)
        self._merge(ev, w); self._merge(ev, out_t.reads)
        self._wait(en, ev)
        if cc is None:
            ins = E["e"].dma_start(out=out_ap, in_=in_ap, **kw)
        else:
            ins = E["e"].collective_compute(cc, ALU.bypass, replica_groups=[list(range(NCORES))],
                                            ins=[in_ap], outs=[out_ap])
        self.nins += 1
        ins.then_inc(sem, 16)
        self.dsem_cnt[sem] += 16
        me = {sem: self.dsem_cnt[sem]}
        self._merge(in_t.reads, me)
        out_t.writes = dict(me); out_t.reads = {}
        return ins

    def barrier(self):
        ev = {}
        for n, E in self.E.items():
            if n != "sp" and E["cnt"] > 0:
                ev[E["sem"]] = E["cnt"]
        for sem, c in self.dsem_cnt.items():
            if c > 0:
                ev[sem] = c
        self._wait("sp", ev)
        self.bcnt += 1
        self.nc.sync.sem_inc(self.bsem, 1)
        for n in self.E:
            if n != "sp":
                self._wait(n, {self.bsem: self.bcnt})


def _bcast_mid(ap, n):
    return ap.unsqueeze(1).to_broadcast([ap.shape[0], n, ap.shape[1]])


def _bcast_last(ap, n):
    return ap.unsqueeze(2).to_broadcast([ap.shape[0], ap.shape[1], n])


class Builder:
    def __init__(self, nc, layers=range(DEPTH), gather=False, n_experts=32, dbg=(), full=False):
        self.nc = nc
        self.S = S = SchedX(nc)
        self.layers = list(layers); self.n_experts = n_experts
        self.dbg = set(dbg)
        ein = lambda n, s, dt=F32: S.dram(n, s, dt, kind="ExternalInput")
        self.x_in = ein("x_in", [NB, T, D]); self.ctx_in = ein("ctx_in", [NB, LC, D]); self.cT_in = ein("cT", [NB, 128, 8, 2])
        R = NCORES if gather else 1
        nlw = DEPTH if full else 1
        ne = 32 if full else n_experts
        self.wshapes = dict(w_in=(nlw * 1024, WIN_COLS), w_mod=(nlw * 1024, 6144), w_br=(nlw * 2048, 1024),
                            w_out=(nlw * 1024, 1024), w_gu=(nlw * ne * 1024, 2048), w_dn=(nlw * ne * 1024, 1024))
        self.ne_w = ne
        self.W = {}
        for k, (r, c) in self.wshapes.items():
            if gather:
                sh = ein(k + "_s", [r // NCORES, c])
                full = S.dram(k + "_f", [r, c], F32)
                S.dma("pool", full, full[:], sh, sh[:], cc="AllGather")
                self.W[k] = full
            else:
                self.W[k] = ein(k + "_s", [r, c])
        self.b_modT = ein("b_modT", [4, 128, 48]); self.decb = ein("decb", [4, 128, 8])
        self.gn_gT = ein("gn_gT", [4, 128, 4]); self.gn_bT = ein("gn_bT", [4, 128, 4])
        self.hg_lbT = ein("hg_lbT", [128, 2, 4, 4]); self.hg_ngT = ein("hg_ngT", [4, 128, 4])
        self.na_T = ein("na_T", [4, 8, 64, 15, 64])
        self.gq_qg = ein("gq_qg", [4, 64, 2]); self.gq_kg = ein("gq_kg", [4, 64, 2])
        self.ln_gT = ein("ln_gT", [4, 2, 128, 8]); self.ln_bT = ein("ln_bT", [4, 2, 128, 8])
        self.w_router = ein("w_router", [4, 1024, 32]); self.b_router_b = ein("b_router_b", [4, 128, 32])
        self.b_guT = ein("b_guT", [4, 128, 32, 16]); self.b_dn = ein("b_dn", [4, 32, 1024])
        self.c_ident = ein("ident", [128, 128])
        self.c_rcos = ein("ret_cos", [128, NT]); self.c_rsin = ein("ret_sin", [128, NT])
        self.c_gcos = ein("gq_cos", [64, NT]); self.c_gsin = ein("gq_sin", [64, NT])
        self.c_delta = ein("delta", [128, 512]); self.c_cvals = ein("cvals", [128, 21]); self.c_m0 = ein("m0", [128, 4, 512])
        self.c_rmask = ein("rmask", [128, NT]); self.c_tril = ein("tril", [32, 2, 32])
        self.c_validC2 = ein("validC2", [128, 64]); self.c_sel = ein("sel", [32, 32, 128])
        self.out = S.dram("out", [NB, T, D], F32, kind="ExternalOutput")
        self.bi = 0
        self.XT = S.dram("XT", [8, 128, NT], F32); self.HT = S.dram("HT", [8, 128, NT], BF16)
        self.H2T = S.dram("H2T", [8, 128, NT], BF16)
        self.OB = [S.dram("OB0", [4, 128, NT], BF16), S.dram("OB1", [4, 128, NT], BF16),
                   S.dram("OB2", [8, 64, NT], BF16), S.dram("OB3", [8, 64, NT], BF16)]
        self.dbg_out = {}
        self.ident = S.sb("ident", [128, 128], F32)
        S.dma("sp", self.ident, self.ident[:], self.c_ident, self.c_ident[:])
        self.onesb = S.sb("onesb", [128, 128], BF16)
        S.op("dve", lambda e: e.memset(self.onesb[:], 1.0), writes=[self.onesb])
        self.modT = S.sb("modT", [128, 48, 2], F32)
        self.ps = [Buf(nc.alloc_psum_tensor("ps%d" % i, [128, 512], F32).ap(), "ps%d" % i) for i in range(8)]
        self.psr = Ring(self.ps)
        self.epsc = {}
        for eps in (LN_EPS, NORM_EPS):
            et = S.sb("epsc", [128, 1], F32)
            S.op("dve", lambda e, et=et, eps=eps: e.memset(et[:], eps), writes=[et])
            self.epsc[eps] = et

    def dbg_dump(self, name, src_t, src_ap, shape, dt=F32):
        if name not in self.dbg:
            return
        o = self.S.dram("dbg_" + name, list(shape), dt, kind="ExternalOutput")
        self.S.dma("sp", o, o[:], src_t, src_ap)
        self.dbg_out[name] = o

    def mm(self, pt, pap, lt, lap, rt, rap, start=True, stop=True):
        self.S.op("pe", lambda e: e.matmul(pap, lhsT=lap, rhs=rap, start=start, stop=stop),
                  reads=[lt, rt], writes=[pt], inc=stop)

    def load_w(self, dst_t, dst_ap, key, row0, nk, col0, ncols, en="pool", join=True):
        w = self.W[key]
        src = w[row0:row0 + nk * 128, col0:col0 + ncols].rearrange("(k p) n -> p k n", p=128)
        self.S.dma(en, dst_t, dst_ap, w, src, join=join)

    def act(self, ot, oap, it, iap, func, bias=None, scale=1.0, rd=()):
        kw = {}
        if bias is not None:
            kw["bias"] = bias
        self.S.op("act", lambda e: e.activation(out=oap, in_=iap, func=func, scale=scale, **kw),
                  reads=[it] + list(rd), writes=[ot])

    def tt(self, en, ot, oap, at, aap, bt, bap, op):
        self.S.op(en, lambda e: e.tensor_tensor(out=oap, in0=aap, in1=bap, op=op), reads=[at, bt], writes=[ot])

    def ts(self, en, ot, oap, it, iap, s1, s2, op0, op1=None, rd=()):
        if op1 is None:
            self.S.op(en, lambda e: e.tensor_scalar(out=oap, in0=iap, scalar1=s1, scalar2=None, op0=op0),
                      reads=[it] + list(rd), writes=[ot])
        else:
            self.S.op(en, lambda e: e.tensor_scalar(out=oap, in0=iap, scalar1=s1, scalar2=s2, op0=op0, op1=op1),
                      reads=[it] + list(rd), writes=[ot])

    def stt(self, en, ot, oap, at, aap, scalar, bt, bap, op0, op1, rd=()):
        self.S.op(en, lambda e: e.scalar_tensor_tensor(out=oap, in0=aap, scalar=scalar, in1=bap, op0=op0, op1=op1),
                  reads=[at, bt] + list(rd), writes=[ot])

    def rsqrt_eps(self, t, ap, eps, mult=1.0):
        if self.epsc is None:
            self.epsc = {}
        if eps not in self.epsc:
            et = self.S.sb("epsc", [128, 1], F32, persist=True)
            self.S.op("dve", lambda e: e.memset(et[:], eps), writes=[et])
            self.epsc[eps] = et
        et = self.epsc[eps]
        self.act(t, ap, t, ap, AF.Ln, bias=et[0:ap.shape[0], 0:1], scale=mult, rd=[et])
        self.act(t, ap, t, ap, AF.Exp, scale=-0.5)

    def phase_init(self):
        S = self.S
        with S.scope():
            xin = Ring([S.sb("xin", [128, D], F32) for _ in range(3)])
            xtb = Ring([S.sb("xtb", [128, 8, 512], F32) for _ in range(2)])
            for (t0, n) in TBS:
                ob = xtb.next()
                tiles = []
                for j in range(n // 128):
                    xt = xin.next()
                    if t0 < T:
                        S.dma("sp", xt, xt[:], self.x_in, self.x_in[self.bi, t0 + j * 128:t0 + (j + 1) * 128, :])
                    else:
                        S.dma("sp", xt, xt[:], self.ctx_in, self.ctx_in[self.bi, t0 - T + j * 128:t0 - T + (j + 1) * 128, :])
                    tiles.append(xt)
                    if len(tiles) == 2 or j == n // 128 - 1:
                        j0 = j + 1 - len(tiles)
                        for c in range(8):
                            p = self.psr.next()
                            for jj, xt_ in enumerate(tiles):
                                S.op("pe", lambda e, p=p, jj=jj, xt_=xt_, c=c: e.transpose(
                                    out=p[:, jj * 128:(jj + 1) * 128], in_=xt_[:, c * 128:(c + 1) * 128],
                                    identity=self.ident[:]), reads=[xt_, self.ident], writes=[p])
                            w = len(tiles) * 128
                            S.op("act" if c % 2 else "dve",
                                 (lambda e, p=p, c=c, j0=j0, w=w, ob=ob: e.copy(out=ob[:, c, j0 * 128:j0 * 128 + w], in_=p[:, 0:w]))
                                 if c % 2 else
                                 (lambda e, p=p, c=c, j0=j0, w=w, ob=ob: e.tensor_copy(out=ob[:, c, j0 * 128:j0 * 128 + w], in_=p[:, 0:w])),
                                 reads=[p], writes=[ob])
                        tiles = []
                S.dma("sp", self.XT, self.XT[:, :, t0:t0 + n].rearrange("c p t -> p c t"), ob, ob[:, :, 0:n])

    def phase_final(self):
        S = self.S
        with S.scope():
            xtb = Ring([S.sb("xtb", [128, 8, 512], F32) for _ in range(2)])
            ot = Ring([S.sb("otile", [128, D], F32) for _ in range(3)])
            for (t0, n) in TBS[:4]:
                xb = xtb.next()
                S.dma("sp", xb, xb[:], self.XT, self.XT[:, :, t0:t0 + n].rearrange("c p t -> p c t"))
                for j in range(4):
                    o = ot.next()
                    for g in range(2):
                        p = self.psr.next()
                        for cc in range(4):
                            c = g * 4 + cc
                            S.op("pe", lambda e, p=p, cc=cc, c=c, j=j, xb=xb: e.transpose(
                                out=p[:, cc * 128:(cc + 1) * 128], in_=xb[:, c, j * 128:(j + 1) * 128],
                                identity=self.ident[:]), reads=[xb, self.ident], writes=[p])
                        if g:
                            S.op("act", lambda e, p=p, o=o, g=g: e.copy(out=o[:, g * 512:(g + 1) * 512], in_=p[:]), reads=[p], writes=[o])
                        else:
                            S.op("dve", lambda e, p=p, o=o, g=g: e.tensor_copy(out=o[:, g * 512:(g + 1) * 512], in_=p[:]), reads=[p], writes=[o])
                    S.dma("sp", self.out, self.out[self.bi, t0 + j * 128:t0 + (j + 1) * 128, :], o, o[:], join=True)

    def finish(self):
        S = self.S
        ev = {}
        S._merge(ev, self.out.writes)
        for o in self.dbg_out.values():
            S._merge(ev, o.writes)
        S._wait("sp", ev)
        S.barrier()


class Builder2(Builder):
    def phase_mod(self, l, lw):
        S = self.S
        with S.scope():
            cT = S.sb("cT", [128, 8, 2], F32); scT = S.sb("scT", [128, 8, 2], BF16)
            bm = S.sb("bm", [128, 48], F32)
            S.dma("sp", cT, cT[:], self.cT_in, self.cT_in[self.bi])
            S.dma("sp", bm, bm[:], self.b_modT, self.b_modT[l])
            self.act(scT, scT[:], cT, cT[:], AF.Silu)
            wr = Ring([S.sb("wmod", [128, 8, 1024], BF16) for _ in range(2)])
            pm = self.psr.next()
            for piece in range(6):
                wt = wr.next()
                self.load_w(wt, wt[:], "w_mod", lw * 1024, 8, piece * 1024, 1024, join=False)
                for j in range(8):
                    jj = piece * 8 + j
                    for k in range(8):
                        self.mm(pm, pm[:, jj * 2:jj * 2 + 2], wt, wt[:, k, j * 128:(j + 1) * 128], scT, scT[:, k, :],
                                start=(k == 0), stop=(k == 7))
            mt = self.modT
            self.tt("dve", mt, mt[:], pm, pm[:, 0:96].rearrange("p (j s) -> p j s", s=2), bm, _bcast_last(bm[:], 2), ALU.add)
            for w in (1, 4):
                self.ts("dve", mt, mt[:, w * 8:(w + 1) * 8, :], mt, mt[:, w * 8:(w + 1) * 8, :], 1.0, None, ALU.add)
            self.dbg_dump("modT", mt, mt[:], [128, 48, 2])

    def phase_hT(self, which, src, dst):
        S = self.S
        mt = self.modT
        with S.scope():
            xb_r = Ring([S.sb("xb", [128, 8, 512], F32) for _ in range(2)])
            hb_r = Ring([S.sb("hb", [128, 8, 512], BF16) for _ in range(2)])
            for (t0, n) in TBS:
                s = 0 if t0 < T else 1
                xb = xb_r.next(); hb = hb_r.next()
                S.dma("sp", xb, xb[:, :, 0:n], src, src[:, :, t0:t0 + n].rearrange("c p t -> p c t"))
                for c in range(8):
                    self.act(hb, hb[:, c, 0:n], xb, xb[:, c, 0:n], AF.Identity,
                             bias=mt[:, which * 24 + c, s:s + 1], scale=mt[:, which * 24 + 8 + c, s:s + 1], rd=[mt])
                S.dma("sp", dst, dst[:, :, t0:t0 + n].rearrange("c p t -> p c t"), hb, hb[:, :, 0:n])

    def ln_block(self, xn, n, l, i, g_t, b_t, sq, onesf, rstd, mean_sb):
        S = self.S
        pmean = self.psr.next(); pex2 = self.psr.next()
        for c in range(8):
            self.act(sq, sq[:, c, 0:n], xn, xn[:, c, 0:n], AF.Square)
        for c in range(8):
            self.mm(pmean, pmean[:, 0:n], onesf, onesf[:], xn, xn[:, c, 0:n], start=(c == 0), stop=(c == 7))
        for c in range(8):
            self.mm(pex2, pex2[:, 0:n], onesf, onesf[:], sq, sq[:, c, 0:n], start=(c == 0), stop=(c == 7))
        self.act(mean_sb, mean_sb[:, 0:n], pmean, pmean[:, 0:n], AF.Identity)
        self.tt("dve", rstd, rstd[:, 0:n], mean_sb, mean_sb[:, 0:n], mean_sb, mean_sb[:, 0:n], ALU.mult)
        self.tt("dve", rstd, rstd[:, 0:n], pex2, pex2[:, 0:n], rstd, rstd[:, 0:n], ALU.subtract)
        self.rsqrt_eps(rstd, rstd[:, 0:n], LN_EPS)
        for c in range(8):
            self.tt("dve", xn, xn[:, c, 0:n], xn, xn[:, c, 0:n], mean_sb, mean_sb[:, 0:n], ALU.subtract)
            self.tt("pool", xn, xn[:, c, 0:n], xn, xn[:, c, 0:n], rstd, rstd[:, 0:n], ALU.mult)
            self.act(xn, xn[:, c, 0:n], xn, xn[:, c, 0:n], AF.Identity, bias=b_t[:, c:c + 1], scale=g_t[:, c:c + 1], rd=[g_t, b_t])

    def phase_merge(self, l, lw):
        S = self.S
        mt = self.modT
        with S.scope():
            wg = S.sb("wg", [128, 8, 4096], BF16)
            wb01 = S.sb("wb01", [128, 2, 4, 1024], BF16); wb23 = S.sb("wb23", [64, 2, 8, 1024], BF16)
            wo = S.sb("wo", [128, 8, 1024], BF16)
            for n4 in range(4):
                self.load_w(wg, wg[:, :, n4 * 1024:(n4 + 1) * 1024], "w_in", lw * 1024, 8, C_GATE + n4 * 1024, 1024)
            wbr = self.W["w_br"]
            for n in range(2):
                S.dma("pool", wb01, wb01[:, n], wbr, wbr[lw * 2048 + n * 512: lw * 2048 + (n + 1) * 512, :].rearrange("(k p) n -> p k n", p=128), join=True)
            for n in range(2):
                S.dma("pool", wb23, wb23[:, n], wbr, wbr[lw * 2048 + (n + 2) * 512: lw * 2048 + (n + 3) * 512, :].rearrange("(k p) n -> p k n", p=64), join=True)
            self.load_w(wo, wo[:], "w_out", lw * 1024, 8, 0, 1024)
            g_t = S.sb("lng", [128, 8], F32); b_t = S.sb("lnb", [128, 8], F32)
            S.dma("sp", g_t, g_t[:], self.ln_gT, self.ln_gT[l, 0]); S.dma("sp", b_t, b_t[:], self.ln_bT, self.ln_bT[l, 0])
            onesf = S.sb("onesf", [128, 128], F32)
            S.op("dve", lambda e: e.memset(onesf[:], 1.0 / D), writes=[onesf])
            hb_r = Ring([S.sb("hb", [128, 8, 512], BF16) for _ in range(2)])
            o01_r = Ring([S.sb("o01", [128, 2, 4, 512], BF16) for _ in range(2)])
            o23_r = Ring([S.sb("o23", [64, 2, 8, 512], BF16) for _ in range(2)])
            xb_r = Ring([S.sb("xb", [128, 8, 512], F32) for _ in range(2)])
            mb = S.sb("mb", [128, 8, 512], BF16)
            sg_r = Ring([S.sb("sg", [128, 512], F32) for _ in range(3)])
            macc_r = Ring([S.sb("macc", [128, 512], F32) for _ in range(2)])
            tmp_r = Ring([S.sb("tmpm", [128, 512], F32) for _ in range(2)])
            sq = S.sb("sq", [128, 8, 512], F32)
            rstd = S.sb("rstd", [128, 512], F32); mean_sb = S.sb("mean_sb", [128, 512], F32)
            h2b_r = Ring([S.sb("h2b", [128, 8, 512], BF16) for _ in range(2)])
            for (t0, n) in TBS:
                s = 0 if t0 < T else 1
                hb = hb_r.next(); o01 = o01_r.next(); o23 = o23_r.next(); xb = xb_r.next()
                S.dma("sp", hb, hb[:, :, 0:n], self.HT, self.HT[:, :, t0:t0 + n].rearrange("c p t -> p c t"))
                for nb in range(2):
                    S.dma("sp", o01, o01[:, nb, :, 0:n], self.OB[nb], self.OB[nb][:, :, t0:t0 + n].rearrange("c p t -> p c t"), join=True)
                    S.dma("sp", o23, o23[:, nb, :, 0:n], self.OB[2 + nb], self.OB[2 + nb][:, :, t0:t0 + n].rearrange("c p t -> p c t"), join=True)
                S.dma("sp", xb, xb[:, :, 0:n], self.XT, self.XT[:, :, t0:t0 + n].rearrange("c p t -> p c t"))
                for oc in range(8):
                    macc = macc_r.next()
                    for nb in range(4):
                        pg = self.psr.next(); py = self.psr.next()
                        for k in range(8):
                            self.mm(pg, pg[:, 0:n], wg, wg[:, k, nb * 1024 + oc * 128: nb * 1024 + (oc + 1) * 128], hb, hb[:, k, 0:n],
                                    start=(k == 0), stop=(k == 7))
                        sg = sg_r.next()
                        self.act(sg, sg[:, 0:n], pg, pg[:, 0:n], AF.Sigmoid)
                        if nb < 2:
                            for c in range(4):
                                self.mm(py, py[:, 0:n], wb01, wb01[:, nb, c, oc * 128:(oc + 1) * 128], o01, o01[:, nb, c, 0:n],
                                        start=(c == 0), stop=(c == 3))
                        else:
                            for c in range(8):
                                self.mm(py, py[:, 0:n], wb23, wb23[:, nb - 2, c, oc * 128:(oc + 1) * 128], o23, o23[:, nb - 2, c, 0:n],
                                        start=(c == 0), stop=(c == 7))
                        if nb == 0:
                            self.tt("dve", macc, macc[:, 0:n], sg, sg[:, 0:n], py, py[:, 0:n], ALU.mult)
                        else:
                            tmp = tmp_r.next()
                            self.tt("dve", tmp, tmp[:, 0:n], sg, sg[:, 0:n], py, py[:, 0:n], ALU.mult)
                            if nb < 3:
                                self.tt("pool", macc, macc[:, 0:n], macc, macc[:, 0:n], tmp, tmp[:, 0:n], ALU.add)
                            else:
                                self.tt("pool", mb, mb[:, oc, 0:n], macc, macc[:, 0:n], tmp, tmp[:, 0:n], ALU.add)
                for oc2 in range(8):
                    py = self.psr.next()
                    for k in range(8):
                        self.mm(py, py[:, 0:n], wo, wo[:, k, oc2 * 128:(oc2 + 1) * 128], mb, mb[:, k, 0:n], start=(k == 0), stop=(k == 7))
                    self.act(xb, xb[:, oc2, 0:n], xb, xb[:, oc2, 0:n], AF.Identity, scale=DN_ALPHA)
                    self.stt("dve", xb, xb[:, oc2, 0:n], py, py[:, 0:n], mt[:, 16 + oc2, s:s + 1], xb, xb[:, oc2, 0:n], ALU.mult, ALU.add, rd=[mt])
                self.ln_block(xb, n, l, 0, g_t, b_t, sq, onesf, rstd, mean_sb)
                S.dma("sp", self.XT, self.XT[:, :, t0:t0 + n].rearrange("c p t -> p c t"), xb, xb[:, :, 0:n])
                h2b = h2b_r.next()
                for c in range(8):
                    self.act(h2b, h2b[:, c, 0:n], xb, xb[:, c, 0:n], AF.Identity,
                             bias=mt[:, 24 + c, s:s + 1], scale=mt[:, 32 + c, s:s + 1], rd=[mt])
                S.dma("sp", self.H2T, self.H2T[:, :, t0:t0 + n].rearrange("c p t -> p c t"), h2b, h2b[:, :, 0:n])


class Builder3(Builder2):
    def __init__(self, *a, **k):
        super().__init__(*a, **k)
        self.OB[2] = self.S.dram("OB2b", [4, 128, NT], BF16); self.OB[3] = self.S.dram("OB3b", [4, 128, NT], BF16)

    def phase_merge(self, l, lw):
        S = self.S
        mt = self.modT
        with S.scope():
            wgr = Ring([S.sb("wg", [128, 8, 1024], BF16) for _ in range(2)])
            wb = S.sb("wb", [128, 4, 4, 1024], BF16)
            wo = S.sb("wo", [128, 8, 1024], BF16)
            wbr = self.W["w_br"]
            for n in range(4):
                S.dma("pool", wb, wb[:, n], wbr, wbr[lw * 2048 + n * 512: lw * 2048 + (n + 1) * 512, :].rearrange("(k p) n -> p k n", p=128), join=True)
            self.load_w(wo, wo[:], "w_out", lw * 1024, 8, 0, 1024)
            g_t = S.sb("lng", [128, 8], F32); b_t = S.sb("lnb", [128, 8], F32)
            S.dma("sp", g_t, g_t[:], self.ln_gT, self.ln_gT[l, 0]); S.dma("sp", b_t, b_t[:], self.ln_bT, self.ln_bT[l, 0])
            onesf = S.sb("onesf", [128, 128], F32)
            S.op("dve", lambda e: e.memset(onesf[:], 1.0 / D), writes=[onesf])
            hb = S.sb("hb", [128, 8, 512], BF16)
            oall = S.sb("oall", [128, 4, 4, 512], BF16)
            xb = S.sb("xb", [128, 8, 512], F32)
            mb = S.sb("mb", [128, 8, 512], BF16)
            macc8 = S.sb("macc8", [128, 8, 512], F32)
            sg_r = Ring([S.sb("sg", [128, 512], F32) for _ in range(3)])
            tmp_r = Ring([S.sb("tmpm", [128, 512], F32) for _ in range(2)])
            rstd = S.sb("rstd", [128, 512], F32); mean_sb = S.sb("mean_sb", [128, 512], F32)
            h2b = S.sb("h2b", [128, 8, 512], BF16)
            for (t0, n) in TBS:
                s = 0 if t0 < T else 1
                S.dma("sp", hb, hb[:, :, 0:n], self.HT, self.HT[:, :, t0:t0 + n].rearrange("c p t -> p c t"))
                for nb in range(4):
                    S.dma("sp", oall, oall[:, nb, :, 0:n], self.OB[nb], self.OB[nb][:, :, t0:t0 + n].rearrange("c p t -> p c t"), join=True)
                S.dma("sp", xb, xb[:, :, 0:n], self.XT, self.XT[:, :, t0:t0 + n].rearrange("c p t -> p c t"))
                for nb in range(4):
                    wg = wgr.next()
                    self.load_w(wg, wg[:], "w_in", lw * 1024, 8, C_GATE + nb * 1024, 1024, join=False)
                    for oc in range(8):
                        pg = self.psr.next(); py = self.psr.next()
                        for k in range(8):
                            self.mm(pg, pg[:, 0:n], wg, wg[:, k, oc * 128:(oc + 1) * 128], hb, hb[:, k, 0:n], start=(k == 0), stop=(k == 7))
                        sg = sg_r.next()
                        self.act(sg, sg[:, 0:n], pg, pg[:, 0:n], AF.Sigmoid)
                        for c in range(4):
                            self.mm(py, py[:, 0:n], wb, wb[:, nb, c, oc * 128:(oc + 1) * 128], oall, oall[:, nb, c, 0:n], start=(c == 0), stop=(c == 3))
                        if nb == 0:
                            self.tt("dve", macc8, macc8[:, oc, 0:n], sg, sg[:, 0:n], py, py[:, 0:n], ALU.mult)
                        else:
                            tmp = tmp_r.next()
                            self.tt("dve", tmp, tmp[:, 0:n], sg, sg[:, 0:n], py, py[:, 0:n], ALU.mult)
                            if nb < 3:
                                self.tt("pool", macc8, macc8[:, oc, 0:n], macc8, macc8[:, oc, 0:n], tmp, tmp[:, 0:n], ALU.add)
                            else:
                                self.tt("pool", mb, mb[:, oc, 0:n], macc8, macc8[:, oc, 0:n], tmp, tmp[:, 0:n], ALU.add)
                for oc2 in range(8):
                    py = self.psr.next()
                    for k in range(8):
                        self.mm(py, py[:, 0:n], wo, wo[:, k, oc2 * 128:(oc2 + 1) * 128], mb, mb[:, k, 0:n], start=(k == 0), stop=(k == 7))
                    self.act(xb, xb[:, oc2, 0:n], xb, xb[:, oc2, 0:n], AF.Identity, scale=DN_ALPHA)
                    self.stt("dve", xb, xb[:, oc2, 0:n], py, py[:, 0:n], mt[:, 16 + oc2, s:s + 1], xb, xb[:, oc2, 0:n], ALU.mult, ALU.add, rd=[mt])
                self.ln_block(xb, n, l, 0, g_t, b_t, macc8, onesf, rstd, mean_sb)
                S.dma("sp", self.XT, self.XT[:, :, t0:t0 + n].rearrange("c p t -> p c t"), xb, xb[:, :, 0:n])
                for c in range(8):
                    self.act(h2b, h2b[:, c, 0:n], xb, xb[:, c, 0:n], AF.Identity,
                             bias=mt[:, 24 + c, s:s + 1], scale=mt[:, 32 + c, s:s + 1], rd=[mt])
                S.dma("sp", self.H2T, self.H2T[:, :, t0:t0 + n].rearrange("c p t -> p c t"), h2b, h2b[:, :, 0:n])


class Builder4(Builder3):
    def __init__(self, *a, **k):
        super().__init__(*a, **k)
        self.GW = self.S.dram("GW", [32, NT], F32)

    def phase_moe(self, l, lw):
        S = self.S
        mt = self.modT
        ne = self.n_experts
        with S.scope():
            xacc = S.sb("xacc", [128, 8, NT], F32)
            for c in range(8):
                S.dma("sp", xacc, xacc[:, c, :], self.XT, self.XT[c], join=True)
            wr = S.sb("wr", [128, 8, 32], F32); brb = S.sb("brb", [128, 32], F32)
            S.dma("sp", wr, wr[:], self.w_router, self.w_router[l].rearrange("(k p) e -> p k e", p=128))
            S.dma("sp", brb, brb[:], self.b_router_b, self.b_router_b[l])
            bgu = S.sb("bgu", [128, 32, 16], F32); bdn = S.sb("bdn", [32, 1024], F32)
            S.dma("sp", bgu, bgu[:], self.b_guT, self.b_guT[l]); S.dma("sp", bdn, bdn[:], self.b_dn, self.b_dn[l])
            h2f_r = Ring([S.sb("h2f", [128, 8, 128], F32) for _ in range(2)])
            sm_r = Ring([S.sb("rsm", [128, 4, 32], F32) for _ in range(2)])
            sc_r = Ring([S.sb("rsc", [128, 16], F32) for _ in range(2)])
            gwt_r = Ring([S.sb("gwt", [32, 128], F32) for _ in range(2)])
            for j in range(NT // 128):
                s = 0 if j < 16 else 1
                h2f = h2f_r.next(); sm = sm_r.next(); sc = sc_r.next()
                for c in range(8):
                    self.act(h2f, h2f[:, c, :], xacc, xacc[:, c, j * 128:(j + 1) * 128], AF.Identity,
                             bias=mt[:, 24 + c, s:s + 1], scale=mt[:, 32 + c, s:s + 1], rd=[mt])
                pl = self.psr.next()
                for c in range(8):
                    self.mm(pl, pl[:, 0:32], h2f, h2f[:, c, :], wr, wr[:, c, :], start=(c == 0), stop=(c == 7))
                lg = sm[:, 0, :]; ex = sm[:, 1, :]; mk = sm[:, 2, :]; gw = sm[:, 3, :]
                self.tt("dve", sm, lg, pl, pl[:, 0:32], brb, brb[:], ALU.add)
                S.op("dve", lambda e, sc=sc, lg=lg: e.max(out=sc[:, 0:8], in_=lg), reads=[sm], writes=[sc])
                self.ts("dve", sc, sc[:, 8:9], sc, sc[:, 0:1], -1.0, None, ALU.mult)
                self.act(sm, ex, sm, lg, AF.Exp, bias=sc[:, 8:9], rd=[sc])
                self.ts("dve", sm, mk, sm, lg, sc[:, 3:4], None, ALU.is_ge, rd=[sc])
                self.tt("dve", sm, ex, sm, ex, sm, mk, ALU.mult)
                S.op("dve", lambda e, sc=sc, ex=ex: e.reduce_sum(out=sc[:, 9:10], in_=ex, axis=AX.X), reads=[sm], writes=[sc])
                S.op("dve", lambda e, sc=sc: e.reciprocal(out=sc[:, 10:11], in_=sc[:, 9:10]), reads=[sc], writes=[sc])
                self.ts("dve", sm, gw, sm, ex, sc[:, 10:11], None, ALU.mult, rd=[sc])
                pT = self.psr.next()
                S.op("pe", lambda e, pT=pT, gw=gw: e.transpose(out=pT[0:32, 0:128], in_=gw, identity=self.ident[:]),
                     reads=[sm, self.ident], writes=[pT])
                gwt = gwt_r.next()
                self.act(gwt, gwt[:], pT, pT[0:32, 0:128], AF.Identity)
                S.dma("sp", self.GW, self.GW[:, j * 128:(j + 1) * 128], gwt, gwt[:], join=True)
            self.dbg_dump("GW", self.GW, self.GW[:], [32, NT])
            for c in range(8):
                self.act(xacc, xacc[:, c, :], xacc, xacc[:, c, :], AF.Identity, scale=DN_ALPHA)
            wu_r = Ring([S.sb("wu", [128, 8, 512], BF16) for _ in range(6)])
            h2b_r = Ring([S.sb("h2b", [128, 8, 512], BF16) for _ in range(2)])
            act_r = Ring([S.sb("actT", [128, 8, 512], BF16) for _ in range(2)])
            gwb_r = Ring([S.sb("gwb", [128, NT], F32) for _ in range(2)])
            gwblk = S.sb("gwblk", [32, 512], F32)
            tg_r = Ring([S.sb("tg", [128, 512], F32) for _ in range(2)])
            tsg_r = Ring([S.sb("tsg", [128, 512], F32) for _ in range(2)])
            tu_r = Ring([S.sb("tu", [128, 512], F32) for _ in range(2)])
            for e_ in range(ne):
                gwb = gwb_r.next()
                S.dma("sp", gwb, gwb[:], self.GW, self.GW[e_:e_ + 1, :].partition_broadcast(128))
                row0 = (lw * self.ne_w + e_) * 1024
                units = []
                for q in range(4):
                    u = wu_r.next(); self.load_w(u, u[:], "w_gu", row0, 8, q * 512, 512, join=False); units.append(u)
                dunits = []
                for q in range(2):
                    u = wu_r.next(); self.load_w(u, u[:], "w_dn", row0, 8, q * 512, 512, join=False); dunits.append(u)
                for (t0, n) in TBS:
                    if l == DEPTH - 1 and t0 >= T:
                        continue
                    s = 0 if t0 < T else 1
                    h2b = h2b_r.next()
                    S.dma("sp", h2b, h2b[:, :, 0:n], self.H2T, self.H2T[:, :, t0:t0 + n].rearrange("c p t -> p c t"))
                    if e_ == 0:
                        S.dma("sp", gwblk, gwblk[:, 0:n], self.GW, self.GW[:, t0:t0 + n])
                    aT = act_r.next()
                    for fc in range(8):
                        ug = units[fc // 4]; uu = units[2 + fc // 4]; co = (fc % 4) * 128
                        pg = self.psr.next(); pu = self.psr.next()
                        for k in range(8):
                            self.mm(pg, pg[:, 0:n], ug, ug[:, k, co:co + 128], h2b, h2b[:, k, 0:n], start=(k == 0), stop=(k == 7))
                        for k in range(8):
                            self.mm(pu, pu[:, 0:n], uu, uu[:, k, co:co + 128], h2b, h2b[:, k, 0:n], start=(k == 0), stop=(k == 7))
                        tg = tg_r.next(); tsg = tsg_r.next(); tu = tu_r.next()
                        self.ts("dve", tg, tg[:, 0:n], pg, pg[:, 0:n], bgu[:, e_, fc:fc + 1], 7.0, ALU.add, ALU.min, rd=[bgu])
                        self.act(tsg, tsg[:, 0:n], tg, tg[:, 0:n], AF.Sigmoid, scale=1.702)
                        self.ts("dve", tu, tu[:, 0:n], pu, pu[:, 0:n], bgu[:, e_, 8 + fc:9 + fc], 7.0, ALU.add, ALU.min, rd=[bgu])
                        self.ts("pool", tu, tu[:, 0:n], tu, tu[:, 0:n], -7.0, 1.0, ALU.max, ALU.add)
                        self.tt("pool", tg, tg[:, 0:n], tg, tg[:, 0:n], tsg, tsg[:, 0:n], ALU.mult)
                        self.tt("pool", tg, tg[:, 0:n], tg, tg[:, 0:n], tu, tu[:, 0:n], ALU.mult)
                        self.tt("dve", aT, aT[:, fc, 0:n], tg, tg[:, 0:n], gwb, gwb[:, t0:t0 + n], ALU.mult)
                    for oc in range(8):
                        ud = dunits[oc // 4]; co = (oc % 4) * 128
                        py = self.psr.next()
                        for fc in range(8):
                            self.mm(py, py[:, 0:n], ud, ud[:, fc, co:co + 128], aT, aT[:, fc, 0:n], start=(fc == 0), stop=(fc == 7))
                        if e_ == 0:
                            pyb = self.psr.next()
                            self.mm(pyb, pyb[:, 0:n], bdn, bdn[:, oc * 128:(oc + 1) * 128], gwblk, gwblk[:, 0:n], start=True, stop=True)
                            self.stt("dve", xacc, xacc[:, oc, t0:t0 + n], pyb, pyb[:, 0:n], mt[:, 40 + oc, s:s + 1],
                                     xacc, xacc[:, oc, t0:t0 + n], ALU.mult, ALU.add, rd=[mt])
                        self.stt("dve", xacc, xacc[:, oc, t0:t0 + n], py, py[:, 0:n], mt[:, 40 + oc, s:s + 1],
                                 xacc, xacc[:, oc, t0:t0 + n], ALU.mult, ALU.add, rd=[mt])
            for c in range(8):
                S.dma("sp", self.XT, self.XT[c], xacc, xacc[:, c, :], join=True)

    def phase_ln2(self, l):
        S = self.S
        with S.scope():
            g_t = S.sb("lng", [128, 8], F32); b_t = S.sb("lnb", [128, 8], F32)
            S.dma("sp", g_t, g_t[:], self.ln_gT, self.ln_gT[l, 1]); S.dma("sp", b_t, b_t[:], self.ln_bT, self.ln_bT[l, 1])
            onesf = S.sb("onesf", [128, 128], F32)
            S.op("dve", lambda e: e.memset(onesf[:], 1.0 / D), writes=[onesf])
            xb_r = Ring([S.sb("xb", [128, 8, 512], F32) for _ in range(2)])
            sq = S.sb("sq", [128, 8, 512], F32)
            rstd = S.sb("rstd", [128, 512], F32); mean_sb = S.sb("mean_sb", [128, 512], F32)
            for (t0, n) in TBS:
                xb = xb_r.next()
                S.dma("sp", xb, xb[:, :, 0:n], self.XT, self.XT[:, :, t0:t0 + n].rearrange("c p t -> p c t"))
                self.ln_block(xb, n, l, 1, g_t, b_t, sq, onesf, rstd, mean_sb)
                S.dma("sp", self.XT, self.XT[:, :, t0:t0 + n].rearrange("c p t -> p c t"), xb, xb[:, :, 0:n])


def _swap_perm(n_heads, hd):
    q = hd // 4
    idx = []
    for h in range(n_heads):
        b = h * hd
        idx += list(range(b + q, b + 2 * q)) + list(range(b, b + q)) + list(range(b + 3 * q, b + 4 * q)) + list(range(b + 2 * q, b + 3 * q))
    return np.array(idx)


def _rope_tables(hd):
    half = hd // 2; q = half // 2
    inv = 10000.0 ** (-np.arange(0, half, 2, dtype=np.float32) / half)
    t = np.arange(T); row = (t // 64).astype(np.float32); col = (t % 64).astype(np.float32)
    cos = np.ones((hd, NT), np.float32); sin = np.zeros((hd, NT), np.float32)
    for f in range(hd):
        pos = row if f < half else col
        ff = f % half
        ang = pos * inv[ff % q]
        cos[f, :T] = np.cos(ang.astype(np.float32))
        sgn = -1.0 if ff < q else 1.0
        sin[f, :T] = sgn * np.sin(ang.astype(np.float32))
    return cos, sin


def host_consts():
    c = {}
    c["ident"] = np.eye(128, dtype=np.float32)
    rc, rs = _rope_tables(128); c["ret_cos"] = rc; c["ret_sin"] = rs
    gc, gs = _rope_tables(64); c["gq_cos"] = gc; c["gq_sin"] = gs
    p = np.arange(128)[:, None]; f = np.arange(512)[None, :]
    c["delta"] = (f - p).astype(np.float32)
    c["cvals"] = np.broadcast_to((128.0 * (np.arange(21) - 3))[None, :], (128, 21)).astype(np.float32).copy()
    m0 = np.zeros((128, 4, 512), np.float32)
    for i, cc in enumerate((-384, -256, -128, 0)):
        m0[:, i, :] = ((f - p + cc) >= 0)
    c["m0"] = m0
    rm = np.ones((128, NT), np.float32); rm[:, ::32] = 0.0
    c["rmask"] = rm
    j = np.arange(32)[:, None]; i = np.arange(32)[None, :]
    tr = np.zeros((32, 2, 32), np.float32); tr[:, 0, :] = (j <= i); tr[:, 1, :] = (j >= i)
    c["tril"] = tr
    kc = np.arange(64)[:, None]; qc = np.arange(64)[None, :]
    c0 = np.clip(qc - 8, 0, 48)
    v = ((kc >= c0) & (kc < c0 + 16)).astype(np.float32)
    c["validC2"] = np.concatenate([v, v], axis=0)
    sel = np.zeros((32, 32, 128), np.float32)
    for e in range(32):
        sel[e, e, :] = 1.0
    c["sel"] = sel
    return c


def host_params(P):
    o = {}
    f32 = np.float32
    o["b_modT"] = np.ascontiguousarray(P["b_mod"].reshape(4, 48, 128).transpose(0, 2, 1)).astype(f32)
    o["decb"] = np.ascontiguousarray(np.broadcast_to(P["ret_decay"].reshape(4, 1, 8), (4, 128, 8))).astype(f32)
    o["gn_gT"] = np.ascontiguousarray(P["ret_gn_g"].reshape(4, 4, 128).transpose(0, 2, 1)).astype(f32)
    o["gn_bT"] = np.ascontiguousarray(P["ret_gn_b"].reshape(4, 4, 128).transpose(0, 2, 1)).astype(f32)
    o["hg_lbT"] = np.ascontiguousarray(P["hg_lb"].reshape(2, 4, 4, 128).transpose(3, 0, 1, 2)).astype(f32)
    o["hg_ngT"] = np.ascontiguousarray(P["hg_norm_g"].reshape(4, 4, 128).transpose(0, 2, 1)).astype(f32)
    kc = np.arange(64)[:, None]; qc = np.arange(64)[None, :]
    dc = np.clip(kc - qc + 15, 0, 30)
    o["na_T"] = np.ascontiguousarray(P["na_rpb"][:, :, :, dc].transpose(0, 1, 3, 2, 4)).astype(f32)
    sw = _swap_perm(1, 64)
    o["gq_qg"] = np.ascontiguousarray(np.stack([P["gq_qn_g"], P["gq_qn_g"][:, sw]], axis=-1)).astype(f32)
    o["gq_kg"] = np.ascontiguousarray(np.stack([P["gq_kn_g"], P["gq_kn_g"][:, sw]], axis=-1)).astype(f32)
    o["ln_gT"] = np.ascontiguousarray(P["ln_g"].reshape(4, 2, 8, 128).transpose(0, 1, 3, 2)).astype(f32)
    o["ln_bT"] = np.ascontiguousarray(P["ln_b"].reshape(4, 2, 8, 128).transpose(0, 1, 3, 2)).astype(f32)
    o["w_router"] = np.ascontiguousarray(P["w_router"]).astype(f32)
    o["b_router_b"] = np.ascontiguousarray(np.broadcast_to(P["b_router"][:, None, :], (4, 128, 32))).astype(f32)
    o["b_guT"] = np.ascontiguousarray(P["b_gu"].reshape(4, 32, 16, 128).transpose(0, 3, 1, 2)).astype(f32)
    o["b_dn"] = np.ascontiguousarray(P["b_down"]).astype(f32)
    return o


def host_weights(P):
    w_in = P["w_in"]
    ext = np.concatenate([w_in,
                          w_in[:, :, C_RQ + _swap_perm(4, 128)], w_in[:, :, C_RK + _swap_perm(4, 128)],
                          w_in[:, :, C_GQ + _swap_perm(8, 64)], w_in[:, :, C_GK + _swap_perm(2, 64)]], axis=2)
    return dict(w_in=ext.reshape(4096, WIN_COLS), w_mod=P["w_mod"].reshape(4096, 6144),
                w_br=P["w_branch"].reshape(8192, 1024), w_out=P["w_out"].reshape(4096, 1024),
                w_gu=P["w_gu"].reshape(-1, 2048), w_dn=P["w_down"].reshape(-1, 1024))


def host_core_acts(P, b):
    bs = range(b * NB, (b + 1) * NB)
    cT = np.stack([np.stack([P["c"][i].reshape(8, 128).T, P["c_ctx"].reshape(8, 128).T], axis=-1) for i in bs], axis=0)
    return dict(x_in=np.ascontiguousarray(P["x"][b * NB:(b + 1) * NB]), ctx_in=np.ascontiguousarray(P["ctx"][b * NB:(b + 1) * NB]),
                cT=np.ascontiguousarray(cT).astype(np.float32))


class Builder5(Builder4):
    def rsq(self, ot, oap, it, iap, eps, mult=1.0):
        if self.epsc is None:
            self.epsc = {}
        if eps not in self.epsc:
            et = self.S.sb("epsc", [128, 1], F32, persist=True)
            self.S.op("dve", lambda e: e.memset(et[:], eps), writes=[et])
            self.epsc[eps] = et
        et = self.epsc[eps]
        self.act(ot, oap, it, iap, AF.Ln, bias=et[0:oap.shape[0], 0:1], scale=mult, rd=[et])
        self.act(ot, oap, ot, oap, AF.Exp, scale=-0.5)

    def load_hT(self):
        S = self.S
        hT = S.sb("hT", [128, 8, NT], BF16)
        for c in range(8):
            S.dma("sp", hT, hT[:, c, :], self.HT, self.HT[c], join=True)
        return hT

    def proj_fm(self, p, n, wt, wap_fn, hT, t0):
        for k in range(8):
            lap = wap_fn(k)
            self.mm(p, p[0:lap.shape[1], 0:n], wt, lap, hT, hT[:, k, t0:t0 + n], start=(k == 0), stop=(k == 7))

    def phase_gqa(self, l, lw):
        S = self.S
        with S.scope():
            hT = self.load_hT()
            raw_c = S.sb("raw_c", [64, NT], F32); raw_s = S.sb("raw_s", [64, NT], F32)
            S.dma("sp", raw_c, raw_c[:], self.c_gcos, self.c_gcos[:]); S.dma("sp", raw_s, raw_s[:], self.c_gsin, self.c_gsin[:])
            qg = S.sb("qg", [64, 2], F32); kg = S.sb("kg", [64, 2], F32)
            S.dma("sp", qg, qg[:], self.gq_qg, self.gq_qg[l]); S.dma("sp", kg, kg[:], self.gq_kg, self.gq_kg[l])
            tabs = {}
            for nm, g in (("q", qg), ("k", kg)):
                tc_ = S.sb("tc" + nm, [64, NT], F32); ts_ = S.sb("ts" + nm, [64, NT], F32)
                self.ts("dve", tc_, tc_[:], raw_c, raw_c[:], g[:, 0:1], None, ALU.mult, rd=[g])
                self.ts("dve", ts_, ts_[:], raw_s, raw_s[:], g[:, 1:2], None, ALU.mult, rd=[g])
                tabs[nm] = (tc_, ts_)
            wq = S.sb("wq", [128, 8, 512], BF16); wqs = S.sb("wqs", [128, 8, 512], BF16)
            wk = S.sb("wk", [128, 8, 128], BF16); wks = S.sb("wks", [128, 8, 128], BF16); wv = S.sb("wv", [128, 8, 128], BF16)
            self.load_w(wq, wq[:], "w_in", lw * 1024, 8, C_GQ, 512); self.load_w(wqs, wqs[:], "w_in", lw * 1024, 8, C_GQS, 512)
            self.load_w(wk, wk[:], "w_in", lw * 1024, 8, C_GK, 128); self.load_w(wks, wks[:], "w_in", lw * 1024, 8, C_GKS, 128)
            self.load_w(wv, wv[:], "w_in", lw * 1024, 8, C_GV, 128)
            psA = self.ps[0]; psB = self.ps[1]
            pr = Ring(self.ps[2:])
            sq_r = Ring([S.sb("sqn", [64, 512], BF16) for _ in range(2)])
            rs_r = Ring([S.sb("rstd", [64, 512], F32) for _ in range(2)])
            t1_r = Ring([S.sb("t1", [64, 512], F32) for _ in range(2)])
            t2_r = Ring([S.sb("t2", [64, 512], F32) for _ in range(2)])

            def normrope(dst, w_, ws_, c0, tab):
                tc_, ts_ = tab
                for (t0, n) in TBS:
                    p1 = pr.next(); p2 = pr.next(); p3 = pr.next()
                    self.proj_fm(p1, n, w_, lambda k: w_[:, k, c0:c0 + 64], hT, t0)
                    self.proj_fm(p2, n, ws_, lambda k: ws_[:, k, c0:c0 + 64], hT, t0)
                    sq = sq_r.next(); rs = rs_r.next(); t1 = t1_r.next(); t2 = t2_r.next()
                    lvl = 9
                    if lvl >= 1:
                        self.act(sq, sq[:, 0:n], p1, p1[0:64, 0:n], AF.Square)
                    if lvl >= 2:
                        self.mm(p3, p3[0:64, 0:n], self.onesb, self.onesb[0:64, 0:64], sq, sq[:, 0:n])
                    if lvl >= 3:
                        self.rsq(rs, rs[:, 0:n], p3, p3[0:64, 0:n], NORM_EPS, mult=1.0 / 64)
                    if lvl >= 4:
                        S.op("dve", lambda e, t1=t1, p1=p1, n=n, t0=t0: e.tensor_tensor(out=t1[:, 0:n], in0=p1[0:64, 0:n], in1=tc_[:, t0:t0 + n], op=ALU.mult),
                             reads=[p1, tc_, sq], writes=[t1])
                        self.tt("dve", t2, t2[:, 0:n], p2, p2[0:64, 0:n], ts_, ts_[:, t0:t0 + n], ALU.mult)
                    if lvl >= 5:
                        self.tt("pool", t1, t1[:, 0:n], t1, t1[:, 0:n], t2, t2[:, 0:n], ALU.add)
                        self.tt("pool", dst, dst[:, t0:t0 + n], t1, t1[:, 0:n], rs, rs[:, 0:n], ALU.mult)

            stop = 99
            if stop <= 1:
                return
            kT = [S.sb("kT%d" % i, [64, NT], BF16) for i in range(2)]
            for kvh in range(2):
                normrope(kT[kvh], wk, wks, kvh * 64, tabs["k"])
            if stop <= 2:
                return
            V = S.sb("V", [128, 18, 128], BF16)
            for j in range(18):
                p = pr.next()
                for k in range(8):
                    self.mm(p, p[:, 0:128], hT, hT[:, k, j * 128:(j + 1) * 128], wv, wv[:, k, :], start=(k == 0), stop=(k == 7))
                self.act(V, V[:, j, :], p, p[:, 0:128], AF.Identity)
            if stop <= 3:
                return
            qT_r = Ring([S.sb("qT", [64, NT], BF16) for _ in range(2)])
            og_r = Ring([S.sb("ogT", [64, NT], BF16) for _ in range(2)])
            pt_r = Ring([S.sb("PT", [128, 512], BF16) for _ in range(4)])
            rd_r = Ring([S.sb("rd", [64, 512], F32) for _ in range(2)])
            for h in range(8):
                kvh = h // 4
                qT = qT_r.next(); og = og_r.next()
                normrope(qT, wq, wqs, h * 64, tabs["q"])
                if stop <= 4:
                    return
                for (t0, n) in TBS:
                    kts = list(range(18)) if t0 < T else [16, 17]
                    for i, kt in enumerate(kts):
                        pS = pr.next()
                        self.mm(pS, pS[:, 0:n], kT[kvh], kT[kvh][:, kt * 128:(kt + 1) * 128], qT, qT[:, t0:t0 + n])
                        PT = pt_r.next()
                        self.act(PT, PT[:, 0:n], pS, pS[:, 0:n], AF.Exp, scale=0.125)
                        self.mm(psA, psA[0:64, 0:n], V, V[:, kt, kvh * 64:(kvh + 1) * 64], PT, PT[:, 0:n], start=(i == 0), stop=(i == len(kts) - 1))
                        self.mm(psB, psB[0:64, 0:n], self.onesb, self.onesb[:, 0:64], PT, PT[:, 0:n], start=(i == 0), stop=(i == len(kts) - 1))
                    rd = rd_r.next()
                    S.op("dve", lambda e, rd=rd, n=n: e.reciprocal(out=rd[:, 0:n], in_=psB[0:64, 0:n]), reads=[psB], writes=[rd])
                    self.tt("dve", og, og[:, t0:t0 + n], psA, psA[0:64, 0:n], rd, rd[:, 0:n], ALU.mult)
                S.dma("sp", self.OB[3], self.OB[3][h // 2, (h % 2) * 64:(h % 2 + 1) * 64, :], og, og[:], join=True)


class Builder6(Builder5):
    def phase_ret(self, l, lw):
        S = self.S
        with S.scope():
            hT = self.load_hT()
            cosT = S.sb("cosT", [128, NT], F32); sinT = S.sb("sinT", [128, NT], F32)
            S.dma("sp", cosT, cosT[:], self.c_rcos, self.c_rcos[:]); S.dma("sp", sinT, sinT[:], self.c_rsin, self.c_rsin[:])
            delta = S.sb("delta", [128, 512], F32); cv = S.sb("cv", [128, 21], F32); m0 = S.sb("m0", [128, 4, 512], F32)
            S.dma("sp", delta, delta[:], self.c_delta, self.c_delta[:]); S.dma("sp", cv, cv[:], self.c_cvals, self.c_cvals[:])
            S.dma("sp", m0, m0[:], self.c_m0, self.c_m0[:])
            dec = S.sb("dec", [128, 8], F32); lgt = S.sb("lgt", [128, 8], F32); nlg = S.sb("nlg", [128, 8], F32)
            S.dma("sp", dec, dec[:], self.decb, self.decb[l])
            self.act(lgt, lgt[:], dec, dec[:], AF.Sigmoid)
            self.act(lgt, lgt[:], lgt, lgt[:], AF.Ln)
            self.ts("dve", nlg, nlg[:], lgt, lgt[:], -1.0, None, ALU.mult)
            gng = S.sb("gng", [128, 4], F32); gnb = S.sb("gnb", [128, 4], F32)
            S.dma("sp", gng, gng[:], self.gn_gT, self.gn_gT[l]); S.dma("sp", gnb, gnb[:], self.gn_bT, self.gn_bT[l])
            onesf = S.sb("onesf", [128, 128], F32)
            S.op("dve", lambda e: e.memset(onesf[:], 1.0 / 128), writes=[onesf])
            wh_r = Ring([S.sb("wh", [128, 8, 6, 128], BF16) for _ in range(1)])
            qr = S.sb("qr", [128, NT], BF16); kr = S.sb("kr", [128, NT], BF16); sg = S.sb("sgate", [128, NT], BF16)
            V = S.sb("V", [128, 18, 128], BF16); og = S.sb("og", [128, NT], BF16)
            E0 = S.sb("E0", [128, 512], F32); E1 = S.sb("E1", [128, 512], F32)
            g0 = S.sb("g0", [128, 21], F32); g1 = S.sb("g1", [128, 21], F32)
            Dd = S.sb("Dd", [128, 4, 512], F32); Dc = S.sb("Dc", [128, 8, 512], F32)
            t1_r = Ring([S.sb("t1", [128, 512], F32) for _ in range(2)]); t2_r = Ring([S.sb("t2", [128, 512], F32) for _ in range(2)])
            pt_r = Ring([S.sb("PT", [128, 512], BF16) for _ in range(4)])
            osb = S.sb("osb", [128, 512], F32); osq = S.sb("osq", [128, 512], F32); mean_sb = S.sb("mean", [128, 512], F32)
            rstd = S.sb("rstd", [128, 512], F32)
            psA = self.ps[0]; pr = Ring(self.ps[1:])
            cols = (C_RQ, C_RQS, C_RK, C_RKS, C_RV, C_RG)
            ci = lambda c: c // 128 + 3
            for h in range(4):
                wh = wh_r.next()
                for i, c0 in enumerate(cols):
                    self.load_w(wh, wh[:, :, i, :], "w_in", lw * 1024, 8, c0 + h * 128, 128, join=True)
                for (t0, n) in TBS:
                    for (dst, a, b) in ((qr, 0, 1), (kr, 2, 3)):
                        p1 = pr.next(); p2 = pr.next()
                        self.proj_fm(p1, n, wh, lambda k, a=a: wh[:, k, a, :], hT, t0)
                        self.proj_fm(p2, n, wh, lambda k, b=b: wh[:, k, b, :], hT, t0)
                        t1 = t1_r.next(); t2 = t2_r.next()
                        self.tt("dve", t1, t1[:, 0:n], p1, p1[:, 0:n], cosT, cosT[:, t0:t0 + n], ALU.mult)
                        self.tt("dve", t2, t2[:, 0:n], p2, p2[:, 0:n], sinT, sinT[:, t0:t0 + n], ALU.mult)
                        self.tt("pool", dst, dst[:, t0:t0 + n], t1, t1[:, 0:n], t2, t2[:, 0:n], ALU.add)
                    p3 = pr.next()
                    self.proj_fm(p3, n, wh, lambda k: wh[:, k, 5, :], hT, t0)
                    self.act(sg, sg[:, t0:t0 + n], p3, p3[:, 0:n], AF.Silu)
                for j in range(18):
                    p = pr.next()
                    for k in range(8):
                        self.mm(p, p[:, 0:128], hT, hT[:, k, j * 128:(j + 1) * 128], wh, wh[:, k, 4, :], start=(k == 0), stop=(k == 7))
                    self.act(V, V[:, j, :], p, p[:, 0:128], AF.Identity)
                self.act(E0, E0[:], delta, delta[:], AF.Exp, scale=lgt[:, h:h + 1], rd=[lgt])
                self.act(E1, E1[:], delta, delta[:], AF.Exp, scale=nlg[:, 4 + h:5 + h], rd=[nlg])
                self.act(g0, g0[:], cv, cv[:], AF.Exp, scale=lgt[:, h:h + 1], rd=[lgt])
                self.act(g1, g1[:], cv, cv[:], AF.Exp, scale=lgt[:, 4 + h:5 + h], rd=[lgt])
                self.ts("dve", g0, g0[:], g0, g0[:], 128.0 ** -0.5, None, ALU.mult)
                self.ts("dve", g1, g1[:], g1, g1[:], 128.0 ** -0.5, None, ALU.mult)
                for i, c in enumerate((-384, -256, -128, 0)):
                    t1 = t1_r.next(); t2 = t2_r.next()
                    self.stt("dve", t1, t1[:], m0, m0[:, i, :], g0[:, ci(c):ci(c) + 1], E0, E0[:], ALU.mult, ALU.mult, rd=[g0])
                    self.stt("dve", t2, t2[:], m0, m0[:, i, :], g1[:, ci(-c):ci(-c) + 1], E1, E1[:], ALU.mult, ALU.mult, rd=[g1])
                    self.tt("pool", t1, t1[:], t1, t1[:], t2, t2[:], ALU.subtract)
                    self.stt("dve", Dd, Dd[:, i, :], E1, E1[:], g1[:, ci(-c):ci(-c) + 1], t1, t1[:], ALU.mult, ALU.add, rd=[g1])
                for qb in range(4):
                    for kk in range(2):
                        c0 = qb * 512 - kk * 128 + 256; c1 = 2048 - qb * 512 + kk * 128
                        t1 = t1_r.next()
                        self.ts("dve", t1, t1[:], E0, E0[:], g0[:, ci(c0):ci(c0) + 1], None, ALU.mult, rd=[g0])
                        self.stt("dve", Dc, Dc[:, qb * 2 + kk, :], E1, E1[:], g1[:, ci(c1):ci(c1) + 1], t1, t1[:], ALU.mult, ALU.add, rd=[g1])
                for qb, (t0, n) in enumerate(TBS):
                    kts = list(range(18)) if t0 < T else [16, 17]
                    for i, kt in enumerate(kts):
                        pS = pr.next()
                        self.mm(pS, pS[:, 0:n], kr, kr[:, kt * 128:(kt + 1) * 128], qr, qr[:, t0:t0 + n])
                        PT = pt_r.next()
                        if t0 >= T:
                            c = -(kt - 16) * 128
                            self.tt("dve", PT, PT[:, 0:n], pS, pS[:, 0:n], Dd, Dd[:, (c + 384) // 128, 0:n], ALU.mult)
                        elif kt >= 16:
                            self.tt("dve", PT, PT[:, 0:n], pS, pS[:, 0:n], Dc, Dc[:, qb * 2 + kt - 16, 0:n], ALU.mult)
                        else:
                            c = qb * 512 - kt * 128
                            if c >= 128:
                                self.stt("dve", PT, PT[:, 0:n], pS, pS[:, 0:n], g0[:, ci(c):ci(c) + 1], E0, E0[:, 0:n], ALU.mult, ALU.mult, rd=[g0])
                            elif c <= -512:
                                self.stt("dve", PT, PT[:, 0:n], pS, pS[:, 0:n], g1[:, ci(-c):ci(-c) + 1], E1, E1[:, 0:n], ALU.mult, ALU.mult, rd=[g1])
                            else:
                                self.tt("dve", PT, PT[:, 0:n], pS, pS[:, 0:n], Dd, Dd[:, (c + 384) // 128, 0:n], ALU.mult)
                        self.mm(psA, psA[:, 0:n], V, V[:, kt, :], PT, PT[:, 0:n], start=(i == 0), stop=(i == len(kts) - 1))
                    self.act(osb, osb[:, 0:n], psA, psA[:, 0:n], AF.Identity)
                    self.act(osq, osq[:, 0:n], psA, psA[:, 0:n], AF.Square)
                    pm = pr.next(); pe2 = pr.next()
                    self.mm(pm, pm[:, 0:n], onesf, onesf[:], osb, osb[:, 0:n])
                    self.mm(pe2, pe2[:, 0:n], onesf, onesf[:], osq, osq[:, 0:n])
                    self.act(mean_sb, mean_sb[:, 0:n], pm, pm[:, 0:n], AF.Identity)
                    self.tt("dve", rstd, rstd[:, 0:n], mean_sb, mean_sb[:, 0:n], mean_sb, mean_sb[:, 0:n], ALU.mult)
                    self.tt("dve", rstd, rstd[:, 0:n], pe2, pe2[:, 0:n], rstd, rstd[:, 0:n], ALU.subtract)
                    self.rsq(rstd, rstd[:, 0:n], rstd, rstd[:, 0:n], LN_EPS)
                    self.tt("pool", osb, osb[:, 0:n], osb, osb[:, 0:n], mean_sb, mean_sb[:, 0:n], ALU.subtract)
                    self.tt("pool", osb, osb[:, 0:n], osb, osb[:, 0:n], rstd, rstd[:, 0:n], ALU.mult)
                    self.act(osb, osb[:, 0:n], osb, osb[:, 0:n], AF.Identity, bias=gnb[:, h:h + 1], scale=gng[:, h:h + 1], rd=[gng, gnb])
                    self.tt("pool", og, og[:, t0:t0 + n], osb, osb[:, 0:n], sg, sg[:, t0:t0 + n], ALU.mult)
                S.dma("sp", self.OB[0], self.OB[0][h], og, og[:], join=True)


class Builder7(Builder6):
    def mmx(self, pt, pap, lt, lap, rt, rap, start=True, stop=True, inc=True):
        self.S.op("pe", lambda e: e.matmul(pap, lhsT=lap, rhs=rap, start=start, stop=stop),
                  reads=[lt, rt], writes=[pt], inc=inc)

    def phase_na(self, l, lw):
        S = self.S
        with S.scope():
            hT = self.load_hT()
            vc2 = S.sb("vc2", [128, 64], F32)
            S.dma("sp", vc2, vc2[:], self.c_validC2, self.c_validC2[:])
            w_r = Ring([S.sb("wna", [128, 8, 3, 64], BF16) for _ in range(2)])
            qT_r = Ring([S.sb("qT", [64, NT], BF16) for _ in range(2)]); kT_r = Ring([S.sb("kT", [64, NT], BF16) for _ in range(2)])
            Ve_r = Ring([S.sb("Ve", [128, 18, 64], BF16) for _ in range(2)]); Vo_r = Ring([S.sb("Vo", [128, 16, 64], BF16) for _ in range(2)])
            tbr_r = Ring([S.sb("tbraw", [128, 14, 64], F32) for _ in range(2)]); tb_r = Ring([S.sb("tb2", [128, 14, 64], F32) for _ in range(2)])
            og_r = Ring([S.sb("og", [64, NT], BF16) for _ in range(2)])
            P_r = Ring([S.sb("P", [128, 6, 64], BF16) for _ in range(3)])
            rd_r = Ring([S.sb("rd", [64, 64], F32) for _ in range(3)])
            psO = Ring(self.ps[0:2]); psD = Ring(self.ps[2:4]); pr = Ring(self.ps[4:8])
            for h in range(8):
                w = w_r.next(); qT = qT_r.next(); kT = kT_r.next(); Ve = Ve_r.next(); Vo = Vo_r.next()
                tbraw = tbr_r.next(); tb2 = tb_r.next(); og = og_r.next()
                for i, c0 in enumerate((C_NQ, C_NK, C_NV)):
                    self.load_w(w, w[:, :, i, :], "w_in", lw * 1024, 8, c0 + h * 64, 64, join=True)
                src = self.na_T[l, h]
                S.dma("sp", tbraw, tbraw[0:64, :, :], self.na_T, src[:, 0:14, :], join=True)
                S.dma("sp", tbraw, tbraw[64:128, :, :], self.na_T, src[:, 1:15, :], join=True)
                self.act(tbraw, tbraw[:], tbraw, tbraw[:], AF.Exp)
                self.tt("dve", tb2, tb2[:], tbraw, tbraw[:], vc2, _bcast_mid(vc2[:], 14), ALU.mult)
                for (t0, n) in TBS:
                    p1 = pr.next(); p2 = pr.next()
                    self.proj_fm(p1, n, w, lambda k: w[:, k, 0, :], hT, t0)
                    self.proj_fm(p2, n, w, lambda k: w[:, k, 1, :], hT, t0)
                    self.act(qT, qT[:, t0:t0 + n], p1, p1[0:64, 0:n], AF.Identity)
                    S.op("dve", lambda e, kT=kT, p2=p2, t0=t0, n=n: e.tensor_copy(out=kT[:, t0:t0 + n], in_=p2[0:64, 0:n]), reads=[p2], writes=[kT])
                for (Vt, off, cnt) in ((Ve, 0, 18), (Vo, 64, 15)):
                    for j0 in range(0, cnt, 8):
                        jn = min(8, cnt - j0)
                        p = pr.next()
                        for jj in range(jn):
                            tok = off + (j0 + jj) * 128
                            for k in range(8):
                                self.mmx(p, p[:, jj * 64:(jj + 1) * 64], hT, hT[:, k, tok:tok + 128], w, w[:, k, 2, :],
                                         start=(k == 0), stop=(k == 7), inc=(k == 7 and jj == jn - 1))
                        self.act(Vt, Vt[:, j0:j0 + jn, :], p, p[:, 0:jn * 64].rearrange("p (j d) -> p j d", d=64), AF.Identity)
                for qr in range(36):
                    lat = qr < 32
                    q0 = qr * 64
                    if lat:
                        R0 = min(max(qr - 4, 0), 24); dr0 = R0 - qr + 7
                        ktoks = [R0 * 64 + 128 * j for j in range(4)] + [2048, 2176]
                        if R0 % 2 == 0:
                            vts = [(Ve, R0 // 2 + j) for j in range(4)]
                        else:
                            vts = [(Vo, (R0 - 1) // 2 + j) for j in range(4)]
                        vts += [(Ve, 16), (Ve, 17)]
                    else:
                        ktoks = [2048, 2176]; vts = [(Ve, 16), (Ve, 17)]
                    nk = len(ktoks)
                    pS = pr.next(); P = P_r.next()
                    for j, tok in enumerate(ktoks):
                        self.mmx(pS, pS[:, j * 64:(j + 1) * 64], kT, kT[:, tok:tok + 128], qT, qT[:, q0:q0 + 64], inc=(j == nk - 1))
                    self.act(P, P[:, 0:nk, :], pS, pS[:, 0:nk * 64].rearrange("p (j q) -> p j q", q=64), AF.Exp, scale=0.125)
                    if lat:
                        self.tt("dve", P, P[:, 0:4, :], P, P[:, 0:4, :], tb2, tb2[:, dr0:dr0 + 7:2, :], ALU.mult)
                    pO = psO.next(); pD = psD.next()
                    for j, (Vt, vi) in enumerate(vts):
                        self.mmx(pO, pO[0:64, 0:64], Vt, Vt[:, vi, :], P, P[:, j, :], start=(j == 0), stop=(j == nk - 1), inc=False)
                        self.mmx(pD, pD[0:64, 0:64], self.onesb, self.onesb[:, 0:64], P, P[:, j, :], start=(j == 0), stop=(j == nk - 1),
                                 inc=(j == nk - 1))
                    rd = rd_r.next()
                    S.op("dve", lambda e, rd=rd, pD=pD: e.reciprocal(out=rd[:], in_=pD[0:64, 0:64]), reads=[pD], writes=[rd])
                    self.tt("dve", og, og[:, q0:q0 + 64], pO, pO[0:64, 0:64], rd, rd[:], ALU.mult)
                S.dma("sp", self.OB[2], self.OB[2][h // 2, (h % 2) * 64:(h % 2 + 1) * 64, :], og, og[:], join=True)


class Builder8(Builder7):
    def phase_hg(self, l, lw):
        S = self.S
        NCH = NT // 32
        with S.scope():
            hT = self.load_hT()
            rmask = S.sb("rmask", [128, NT], F32); tril = S.sb("tril", [32, 2, 32], F32)
            S.dma("sp", rmask, rmask[:], self.c_rmask, self.c_rmask[:]); S.dma("sp", tril, tril[:], self.c_tril, self.c_tril[:])
            lbt = S.sb("lbt", [128, 2, 4, 4], F32); ssum = S.sb("ssum", [128, 2, 4], F32)
            low = S.sb("low", [128, 2, 4], F32); oml = S.sb("oml", [128, 2, 4], F32); noml = S.sb("noml", [128, 2, 4], F32)
            S.dma("sp", lbt, lbt[:], self.hg_lbT, self.hg_lbT[:])
            self.act(lbt, lbt[:], lbt, lbt[:], AF.Exp)
            self.tt("dve", ssum, ssum[:], lbt, lbt[:, :, 0, :], lbt, lbt[:, :, 1, :], ALU.add)
            self.tt("dve", ssum, ssum[:], ssum, ssum[:], lbt, lbt[:, :, 2, :], ALU.add)
            self.tt("dve", ssum, ssum[:], ssum, ssum[:], lbt, lbt[:, :, 3, :], ALU.add)
            S.op("dve", lambda e: e.reciprocal(out=ssum[:], in_=ssum[:]), reads=[ssum], writes=[ssum])
            S.op("dve", lambda e: e.memset(low[:], 0.0), writes=[low])
            for i in range(1, l + 1):
                self.tt("dve", low, low[:], low, low[:], lbt, lbt[:, :, i, :], ALU.add)
            self.tt("dve", low, low[:], low, low[:], ssum, ssum[:], ALU.mult)
            self.ts("dve", oml, oml[:], low, low[:], -1.0, 1.0, ALU.mult, ALU.add)
            self.ts("dve", noml, noml[:], oml, oml[:], -1.0, None, ALU.mult)
            ngt = S.sb("ngt", [128, 4], F32)
            S.dma("sp", ngt, ngt[:], self.hg_ngT, self.hg_ngT[l])
            onesf = S.sb("onesf", [128, 128], F32)
            S.op("dve", lambda e: e.memset(onesf[:], 1.0 / 128), writes=[onesf])
            wh = S.sb("wh", [128, 8, 5, 128], BF16)
            qf = S.sb("qf", [128, NT], F32); sg = S.sb("sgate", [128, NT], BF16); osum = S.sb("osum", [128, NT], F32)
            vtok = S.sb("vtok", [32, NCH, 128], BF16)
            qd = [S.sb("qd%d" % d, [128, NT], BF16) for d in range(2)]; kd = [S.sb("kd%d" % d, [128, NT], BF16) for d in range(2)]
            kl = [S.sb("kl%d" % d, [128, NT], F32) for d in range(2)]; dcy = [S.sb("dcy%d" % d, [128, NCH], F32) for d in range(2)]
            kdz = [S.sb("kdz%d" % d, [128, NT], BF16) for d in range(2)]; emid = [S.sb("emid%d" % d, [128, NCH], F32) for d in range(2)]
            mid = S.sb("mid", [128, NCH], F32)
            for d in range(2):
                S.op("pool", lambda e, d=d: e.memset(kdz[d][:], 0.0), writes=[kdz[d]])
            tot = S.sb("tot", [128, NCH], F32)
            Tm = [S.sb("T%d" % i, [128, NT], F32) for i in range(4)]
            St = [S.sb("S%d" % d, [128, 128], F32) for d in range(2)]; Sb = [S.sb("Sb%d" % d, [128, 128], BF16) for d in range(2)]
            Am_r = Ring([S.sb("Am", [32, 32], BF16) for _ in range(4)]); klt_r = Ring([S.sb("klt", [32, 128], BF16) for _ in range(4)])
            og = S.sb("og", [128, NT], BF16)
            pr = self.psr
            v3 = lambda t: t[:].rearrange("p (c i) -> p c i", i=32)
            cols = (C_HQ, C_HF, C_HB, C_HI, C_HG)
            for h in range(4):
                for i, c0 in enumerate(cols):
                    self.load_w(wh, wh[:, :, i, :], "w_in", lw * 1024, 8, c0 + h * 128, 128, join=True)
                T1, T2, T3, T4 = Tm
                for (t0, n) in TBS:
                    p1 = pr.next(); p2 = pr.next(); p3 = pr.next()
                    self.proj_fm(p1, n, wh, lambda k: wh[:, k, 0, :], hT, t0)
                    self.proj_fm(p2, n, wh, lambda k: wh[:, k, 4, :], hT, t0)
                    self.proj_fm(p3, n, wh, lambda k: wh[:, k, 3, :], hT, t0)
                    self.act(qf, qf[:, t0:t0 + n], p1, p1[:, 0:n], AF.Silu)
                    self.act(sg, sg[:, t0:t0 + n], p2, p2[:, 0:n], AF.Silu)
                    S.op("dve", lambda e, p3=p3, t0=t0, n=n: e.tensor_copy(out=T4[:, t0:t0 + n], in_=p3[:, 0:n]), reads=[p3], writes=[T4])
                for c0 in range(0, NCH, 4):
                    p = pr.next()
                    for cc in range(4):
                        c = c0 + cc
                        S.op("pe", lambda e, p=p, cc=cc, c=c: e.transpose(out=p[0:32, cc * 128:(cc + 1) * 128], in_=T4[:, c * 32:(c + 1) * 32],
                                                                          identity=self.ident[:]), reads=[T4, self.ident], writes=[p], inc=(cc == 3))
                    self.act(vtok, vtok[:, c0:c0 + 4, :], p, p[0:32, :].rearrange("p (c d) -> p c d", d=128), AF.Identity)
                for d in range(2):
                    lb_ = low[:, d, h:h + 1]; om_ = oml[:, d, h:h + 1]; nom_ = noml[:, d, h:h + 1]
                    for (t0, n) in TBS:
                        p1 = pr.next()
                        self.proj_fm(p1, n, wh, lambda k, d=d: wh[:, k, 1 + d, :], hT, t0)
                        self.act(T1, T1[:, t0:t0 + n], p1, p1[:, 0:n], AF.Sigmoid)
                    self.ts("dve", T3, T3[:], T1, T1[:], om_, lb_, ALU.mult, ALU.add, rd=[oml, low])
                    self.ts("dve", T2, T2[:], T1, T1[:], nom_, om_, ALU.mult, ALU.add, rd=[oml, noml])
                    self.act(T3, T3[:], T3, T3[:], AF.Ln)
                    S.op("dve", lambda e: e.tensor_tensor_scan(out=T1[:], data0=rmask[:], data1=T3[:], initial=0.0, op0=ALU.mult, op1=ALU.add),
                         reads=[rmask, T3], writes=[T1])
                    S.op("dve", lambda e: e.tensor_copy(out=tot[:], in_=v3(T1)[:, :, 31]), reads=[T1], writes=[tot])
                    self.act(dcy[d], dcy[d][:], tot, tot[:], AF.Exp)
                    self.tt("dve", T4, v3(T4), tot, _bcast_last(tot[:], 32), T1, v3(T1), ALU.subtract)
                    if d == 0:
                        lc, ek = T1, T4
                    else:
                        self.tt("dve", T4, T4[:], T4, T4[:], T3, T3[:], ALU.add)
                        self.tt("pool", T1, T1[:], T1, T1[:], T3, T3[:], ALU.subtract)
                        lc, ek = T4, T1
                    self.act(ek, ek[:], ek, ek[:], AF.Exp)
                    self.tt("dve", kl[d], kl[d][:], T2, T2[:], ek, ek[:], ALU.mult)
                    mi = 15 if d == 0 else 16
                    S.op("dve", lambda e, lc=lc, mi=mi: e.tensor_copy(out=mid[:], in_=v3(lc)[:, :, mi]), reads=[lc], writes=[mid])
                    self.act(emid[d], emid[d][:], mid, mid[:], AF.Exp)
                    self.tt("dve", lc, v3(lc), lc, v3(lc), mid, _bcast_last(mid[:], 32), ALU.subtract)
                    self.act(ek, ek[:], lc, lc[:], AF.Exp)
                    self.tt("dve", qd[d], qd[d][:], qf, qf[:], ek, ek[:], ALU.mult)
                    self.act(ek, ek[:], lc, lc[:], AF.Exp, scale=-1.0)
                    self.tt("dve", kd[d], kd[d][:], T2, T2[:], ek, ek[:], ALU.mult)
                    keep = slice(0, 16) if d == 0 else slice(16, 32)
                    self.tt("pool", kdz[d], v3(kdz[d])[:, :, keep], T2, v3(T2)[:, :, keep], ek, v3(ek)[:, :, keep], ALU.mult)
                S.op("dve", lambda e: e.memset(osum[:], 0.0), writes=[osum])
                for d in range(2):
                    S.op("dve", lambda e, d=d: e.memset(St[d][:], 0.0), writes=[St[d]])
                    S.op("dve", lambda e, d=d: e.memset(Sb[d][:], 0.0), writes=[Sb[d]])
                order = [list(range(64, 72)) + list(range(0, 64)), list(range(71, 63, -1)) + list(range(63, -1, -1))]
                for s in range(NCH):
                    for d in range(2):
                        c = order[d][s]
                        sl = slice(c * 32, (c + 1) * 32)
                        pA = pr.next(); pT = pr.next(); pO = pr.next(); pS = pr.next()
                        h0 = slice(c * 32, c * 32 + 16); h1 = slice(c * 32 + 16, (c + 1) * 32)
                        ka, kb = (kdz[0], kd[0]) if d == 0 else (kd[1], kdz[1])
                        self.mmx(pA, pA[0:32, 0:16], ka, ka[:, sl], qd[d], qd[d][:, h0], inc=False)
                        self.mmx(pA, pA[0:32, 16:32], kb, kb[:, sl], qd[d], qd[d][:, h1], inc=True)
                        Am = Am_r.next()
                        self.tt("dve", Am, Am[:], pA, pA[0:32, 0:32], tril, tril[:, d, :], ALU.mult)
                        S.op("pe", lambda e, pT=pT, d=d, sl=sl: e.transpose(out=pT[0:32, 0:128], in_=kl[d][:, sl], identity=self.ident[:]),
                             reads=[kl[d], self.ident], writes=[pT])
                        klt = klt_r.next()
                        self.act(klt, klt[:], pT, pT[0:32, 0:128], AF.Identity)
                        self.mm(pO, pO[:, 0:32], vtok, vtok[:, c, :], Am, Am[:], start=True, stop=False)
                        self.mm(pO, pO[:, 0:32], Sb[d], Sb[d][:], qd[d], qd[d][:, sl], start=False, stop=True)
                        self.tt("dve", osum, osum[:, sl], osum, osum[:, sl], pO, pO[:, 0:32], ALU.add)
                        self.mm(pS, pS[:, 0:128], klt, klt[:], vtok, vtok[:, c, :])
                        self.stt("dve", St[d], St[d][:], St[d], St[d][:], dcy[d][:, c:c + 1], pS, pS[:, 0:128], ALU.mult, ALU.add, rd=[dcy[d]])
                        cn = order[d][s + 1] if s + 1 < NCH else c
                        self.act(Sb[d], Sb[d][:], St[d], St[d][:], AF.Identity, scale=emid[d][:, cn:cn + 1], rd=[emid[d]])
                for (t0, n) in TBS:
                    self.act(T1, T1[:, t0:t0 + n], osum, osum[:, t0:t0 + n], AF.Square)
                    pss = pr.next()
                    self.mm(pss, pss[:, 0:n], onesf, onesf[:], T1, T1[:, t0:t0 + n])
                    self.rsq(T2, T2[:, t0:t0 + n], pss, pss[:, 0:n], NORM_EPS)
                    self.tt("dve", T3, T3[:, t0:t0 + n], osum, osum[:, t0:t0 + n], T2, T2[:, t0:t0 + n], ALU.mult)
                    self.act(T3, T3[:, t0:t0 + n], T3, T3[:, t0:t0 + n], AF.Identity, scale=ngt[:, h:h + 1], rd=[ngt])
                    self.tt("pool", og, og[:, t0:t0 + n], T3, T3[:, t0:t0 + n], sg, sg[:, t0:t0 + n], ALU.mult)
                S.dma("sp", self.OB[1], self.OB[1][h], og, og[:], join=True)


class BuilderN(Builder8):
    def build_all(self):
        for bi in range(NB):
            self.bi = bi
            self.build_one()
        self.finish()

    def build_one(self):
        self.phase_init()
        for l in self.layers:
            self.phase_mod(l, l)
            self.phase_hT(0, self.XT, self.HT)
            self.phase_ret(l, l)
            self.phase_hg(l, l)
            self.phase_na(l, l)
            self.phase_gqa(l, l)
            self.phase_merge(l, l)
            self.phase_moe(l, l)
            self.phase_ln2(l)
        self.phase_final()


_W_KEYS = ("w_in", "w_mod", "w_br", "w_out", "w_gu", "w_dn")


def kernel(**inputs):
    P = {k: np.asarray(v) for k, v in inputs.items()}
    consts = host_consts(); params = host_params(P); W = host_weights(P)
    nc = bass.Bass("TRN2", target_bir_lowering=False)
    B = BuilderN(nc, full=True)
    B.build_all()
    in_maps = []
    for b in range(NCORES):
        m = {}
        m.update(consts); m.update(params); m.update(host_core_acts(P, b))
        for k in _W_KEYS:
            m[k + "_s"] = W[k]
        in_maps.append({k: np.ascontiguousarray(v, dtype=np.float32) for k, v in m.items()})
    res = run_bass_kernel_spmd(nc, in_maps, core_ids=list(range(NCORES)))
    return np.concatenate([np.asarray(res.results[b]["out"], np.float32) for b in range(NCORES)], axis=0)
```
